# Optimizing a Trainium2 kernel written in Bass

```python
import math
import jax, jax.numpy as jnp
from jax import lax
import numpy as np

D_MODEL = 1024
BATCH = 2
SEQ = 8192
DEPTH = 1

D_RNN = 1024
RNN_BLOCKS = 16
RNN_BW = D_RNN // RNN_BLOCKS
CONV_W = 4
LRU_C = 8.0
N_HEADS = 16
N_KV = 4
HEAD_DIM = 64
GROUP = N_HEADS // N_KV
WINDOW = 128
BLOCK_Q = 128
Q_DIM = N_HEADS * HEAD_DIM
KV_DIM = N_KV * HEAD_DIM
N_EXPERTS = 32
TOP_K = 4
D_FF = 1024
SWIGLU_LIMIT = 7.0
SWIGLU_ALPHA = 1.702
MOE_BLOCK = 128
EPS = 1e-6
SPLITS = (D_RNN, D_RNN, Q_DIM, KV_DIM, KV_DIM, D_MODEL, D_MODEL)
D_IN = sum(SPLITS)

kernel_name = "hybrid_rglru_swa_sink_moe_block"


def rmsnorm(x, g):
    xf = x.astype(jnp.float32)
    y = xf * lax.rsqrt(jnp.mean(xf * xf, axis=-1, keepdims=True) + EPS)
    return (y * g.astype(jnp.float32)).astype(x.dtype)


def causal_depthwise_conv(x, w, b):
    S = x.shape[1]
    xp = jnp.pad(x, ((0, 0), (CONV_W - 1, 0), (0, 0)))
    y = b
    for kk in range(CONV_W):
        y = y + xp[:, kk:kk + S] * w[kk]
    return y


def rg_lru(x, w_a, b_a, w_x, b_x, lam):
    B, S, _ = x.shape
    xb = x.reshape(B, S, RNN_BLOCKS, RNN_BW)
    gate_r = jax.nn.sigmoid(jnp.einsum('bshi,hij->bshj', xb, w_a).reshape(B, S, D_RNN) + b_a)
    gate_i = jax.nn.sigmoid(jnp.einsum('bshi,hij->bshj', xb, w_x).reshape(B, S, D_RNN) + b_x)
    log_a = -LRU_C * gate_r.astype(jnp.float32) * jax.nn.softplus(-lam.astype(jnp.float32))
    a = jnp.exp(log_a)
    mult = jnp.sqrt(-jnp.expm1(2.0 * log_a))
    reset = (jnp.arange(S) == 0)[None, :, None]
    mult = jnp.where(reset, 1.0, mult)
    bterm = (x * gate_i).astype(jnp.float32) * mult

    def combine(left, right):
        a1, b1 = left
        a2, b2 = right
        return a1 * a2, a2 * b1 + b2

    _, h = lax.associative_scan(combine, (a, bterm), axis=1)
    return h.astype(x.dtype)


def alibi_slopes():
    return np.array([2.0 ** (-8.0 * (h + 1) / N_HEADS) for h in range(N_HEADS)], dtype=np.float32)


def swa_sink_attention(q, k, v, sinks):
    B, S = q.shape[0], q.shape[1]
    nb = S // BLOCK_Q
    qb = q.reshape(B, nb, BLOCK_Q, N_KV, GROUP, HEAD_DIM)
    pad = ((0, 0), (BLOCK_Q, 0), (0, 0), (0, 0))
    kb = jnp.pad(k, pad).reshape(B, nb + 1, BLOCK_Q, N_KV, HEAD_DIM)
    vb = jnp.pad(v, pad).reshape(B, nb + 1, BLOCK_Q, N_KV, HEAD_DIM)
    k_band = jnp.concatenate([kb[:, :-1], kb[:, 1:]], axis=2)
    v_band = jnp.concatenate([vb[:, :-1], vb[:, 1:]], axis=2)
    s = jnp.einsum('bnqkgd,bnckd->bnkgqc', qb, k_band,
                   preferred_element_type=jnp.float32) * (HEAD_DIM ** -0.5)
    qi = jnp.arange(BLOCK_Q)[:, None]
    ci = jnp.arange(2 * BLOCK_Q)[None, :]
    dist = qi + BLOCK_Q - ci
    kpos = jnp.arange(nb)[:, None, None] * BLOCK_Q - BLOCK_Q + ci[None]
    valid = (dist >= 0) & (dist < WINDOW) & (kpos >= 0)
    slopes = jnp.asarray(alibi_slopes()).reshape(N_KV, GROUP)[:, :, None, None]
    s = s - slopes * dist.astype(jnp.float32)
    s = jnp.where(valid[None, :, None, None], s, -jnp.inf)
    sink = sinks.astype(jnp.float32).reshape(N_KV, GROUP)[:, :, None, None]
    m = jnp.maximum(jnp.max(s, axis=-1, keepdims=True), sink)
    p = jnp.exp(s - m)
    denom = jnp.sum(p, axis=-1, keepdims=True) + jnp.exp(sink - m)
    o = jnp.einsum('bnkgqc,bnckd->bnqkgd', (p / denom).astype(v.dtype), v_band)
    return o.reshape(B, S, Q_DIM)


def clamped_swiglu(hid):
    x_glu = jnp.minimum(hid[..., ::2], SWIGLU_LIMIT)
    x_lin = jnp.clip(hid[..., 1::2], -SWIGLU_LIMIT, SWIGLU_LIMIT)
    return x_glu * jax.nn.sigmoid(SWIGLU_ALPHA * x_glu) * (x_lin + 1.0)


def moe_ffn(h, router_w, router_b, w1, b1, w2, b2):
    B, S, D = h.shape
    N = B * S
    xt = h.reshape(N, D)
    logits = (xt @ router_w + router_b).astype(jnp.float32)
    top_v, top_e = lax.top_k(logits, TOP_K)
    gates = jax.nn.softmax(top_v, axis=-1)
    A = N * TOP_K
    e_flat = top_e.reshape(A)
    g_flat = gates.reshape(A)
    tok = jnp.arange(A, dtype=jnp.int32) // TOP_K
    order = jnp.argsort(e_flat, stable=True)
    e_s, tok_s, g_s = e_flat[order], tok[order], g_flat[order]
    counts = jnp.bincount(e_flat, length=N_EXPERTS)
    starts = jnp.cumsum(counts) - counts
    padded = ((counts + MOE_BLOCK - 1) // MOE_BLOCK) * MOE_BLOCK
    pend = jnp.cumsum(padded)
    pstart = pend - padded
    dest = pstart[e_s] + jnp.arange(A, dtype=jnp.int32) - starts[e_s]
    P = A + N_EXPERTS * MOE_BLOCK
    nblk = P // MOE_BLOCK
    tok_buf = jnp.full((P,), N, dtype=jnp.int32).at[dest].set(tok_s)
    g_buf = jnp.zeros((P,), jnp.float32).at[dest].set(g_s)
    blk_e = jnp.clip(jnp.searchsorted(pend, jnp.arange(nblk, dtype=jnp.int32) * MOE_BLOCK,
                                      side='right'), 0, N_EXPERTS - 1)
    x_pad = jnp.concatenate([xt, jnp.zeros((1, D), xt.dtype)], axis=0)
    xb = x_pad[tok_buf].reshape(nblk, MOE_BLOCK, D)

    def expert_block(args):
        xblk, e = args
        act = clamped_swiglu(xblk @ w1[e] + b1[e])
        return act @ w2[e] + b2[e]

    yb = lax.map(expert_block, (xb, blk_e)).reshape(P, D)
    yb = yb * g_buf[:, None].astype(yb.dtype)
    out = jax.ops.segment_sum(yb, tok_buf, num_segments=N + 1)[:N]
    return out.reshape(B, S, D)


def setup_inputs(seed: int = 0) -> dict:
    key = jax.random.key(seed)
    ks = jax.random.split(key, 24)
    f32 = jnp.float32
    L = DEPTH

    def nrm(k, shape, scale):
        return jax.random.normal(k, shape, f32) * scale

    def gain(k, shape):
        return 1.0 + 0.05 * jax.random.normal(k, shape, f32)

    u = jax.random.uniform(ks[12], (L, D_RNN), f32, 0.9, 0.999)
    sig = u ** (1.0 / LRU_C)
    rg_lambda = jnp.log(sig) - jnp.log1p(-sig)
    return {
        "x": nrm(ks[0], (BATCH, SEQ, D_MODEL), 1.0),
        "c": nrm(ks[1], (BATCH, D_MODEL), 1.0),
        "w_ada": nrm(ks[2], (L, D_MODEL, 6 * D_MODEL), 0.5 * D_MODEL ** -0.5),
        "b_ada": nrm(ks[3], (L, 6 * D_MODEL), 0.02),
        "norm_pre_mix": gain(ks[4], (L, D_MODEL)),
        "norm_post_mix": gain(ks[5], (L, D_MODEL)),
        "norm_pre_ffn": gain(ks[6], (L, D_MODEL)),
        "norm_post_ffn": gain(ks[7], (L, D_MODEL)),
        "w_in": nrm(ks[8], (L, D_MODEL, D_IN), D_MODEL ** -0.5),
        "b_in": nrm(ks[9], (L, D_IN), 0.02),
        "conv_w": nrm(ks[10], (L, CONV_W, D_RNN), CONV_W ** -0.5),
        "conv_b": nrm(ks[11], (L, D_RNN), 0.02),
        "rg_w_a": nrm(ks[13], (L, RNN_BLOCKS, RNN_BW, RNN_BW), RNN_BW ** -0.5),
        "rg_b_a": nrm(ks[14], (L, D_RNN), 0.02),
        "rg_w_x": nrm(ks[15], (L, RNN_BLOCKS, RNN_BW, RNN_BW), RNN_BW ** -0.5),
        "rg_b_x": nrm(ks[16], (L, D_RNN), 0.02),
        "rg_lambda": rg_lambda,
        "attn_sinks": nrm(ks[17], (L, N_HEADS), 0.5),
        "w_o_rnn": nrm(ks[18], (L, D_RNN, D_MODEL), D_RNN ** -0.5),
        "w_o_attn": nrm(ks[19], (L, Q_DIM, D_MODEL), Q_DIM ** -0.5),
        "w_out": nrm(ks[20], (L, D_MODEL, D_MODEL), D_MODEL ** -0.5),
        "router_w": nrm(ks[21], (L, D_MODEL, N_EXPERTS), D_MODEL ** -0.5),
        "router_b": nrm(ks[22], (L, N_EXPERTS), 0.01),
        "moe_w1": nrm(jax.random.fold_in(ks[23], 0), (L, N_EXPERTS, D_MODEL, 2 * D_FF), D_MODEL ** -0.5),
        "moe_b1": nrm(jax.random.fold_in(ks[23], 1), (L, N_EXPERTS, 2 * D_FF), 0.02),
        "moe_w2": nrm(jax.random.fold_in(ks[23], 2), (L, N_EXPERTS, D_FF, D_MODEL), D_FF ** -0.5),
        "moe_b2": nrm(jax.random.fold_in(ks[23], 3), (L, N_EXPERTS, D_MODEL), 0.02),
    }


def reference(x, c, w_ada, b_ada, norm_pre_mix, norm_post_mix, norm_pre_ffn, norm_post_ffn,
              w_in, b_in, conv_w, conv_b, rg_w_a, rg_b_a, rg_w_x, rg_b_x, rg_lambda,
              attn_sinks, w_o_rnn, w_o_attn, w_out, router_w, router_b,
              moe_w1, moe_b1, moe_w2, moe_b2):
    B, S, _ = x.shape
    split_points = np.cumsum(np.array(SPLITS))[:-1].tolist()
    for layer in range(DEPTH):
        ada = (jax.nn.silu(c) @ w_ada[layer] + b_ada[layer])[:, None, :]
        sh1, sc1, g1, sh2, sc2, g2 = jnp.split(ada, 6, axis=-1)

        h = rmsnorm(x, norm_pre_mix[layer]) * (1.0 + sc1) + sh1
        proj = h @ w_in[layer] + b_in[layer]
        xr, gr, q, k, v, gate_r, gate_a = jnp.split(proj, split_points, axis=-1)
        xr = causal_depthwise_conv(xr, conv_w[layer], conv_b[layer])
        y_rnn = rg_lru(xr, rg_w_a[layer], rg_b_a[layer], rg_w_x[layer], rg_b_x[layer],
                       rg_lambda[layer]) * jax.nn.gelu(gr)
        y_att = swa_sink_attention(q.reshape(B, S, N_HEADS, HEAD_DIM),
                                   k.reshape(B, S, N_KV, HEAD_DIM),
                                   v.reshape(B, S, N_KV, HEAD_DIM), attn_sinks[layer])
        merged = (jax.nn.sigmoid(gate_r) * (y_rnn @ w_o_rnn[layer])
                  + jax.nn.sigmoid(gate_a) * (y_att @ w_o_attn[layer]))
        mix = merged @ w_out[layer]
        x = x + g1 * rmsnorm(mix, norm_post_mix[layer])

        h = rmsnorm(x, norm_pre_ffn[layer]) * (1.0 + sc2) + sh2
        ff = moe_ffn(h, router_w[layer], router_b[layer], moe_w1[layer], moe_b1[layer],
                     moe_w2[layer], moe_b2[layer])
        x = x + g2 * rmsnorm(ff, norm_post_ffn[layer])
    return x
```

```python
import os
import numpy as np
from contextlib import ExitStack
import concourse.bass as bass
import concourse.mybir as mybir
from concourse.bass_utils import run_bass_kernel_spmd

F32, BF16 = mybir.dt.float32, mybir.dt.bfloat16
AF = mybir.ActivationFunctionType
ALU = mybir.AluOpType
AX = mybir.AxisListType

NCORES = 8
D = 1024
T = 2048
NST = 4
KC = 8
NE = 32
EPS = 1e-6
NEG = -30000.0
CAP = 1024
U32 = mybir.dt.uint32
ENGS = ("sync", "scalar", "vector", "gpsimd", "tensor")
SEM_ROT = 3000


class Sched:
    def __init__(self, nc, stack):
        self.nc = nc
        self._stack = stack
        self.ops = {e: [] for e in ENGS}
        self.cur_sem = {}
        self.waited = {e: {} for e in ENGS}
        self.last_w = {}
        self.readers = {}
        self.nsem = 0
        self.dma_sems = {}
        self.pending = {e: [] for e in ENGS}
        self.regs = {}
        self.region = None
        self.nregion = 0
        self._saved_waited = None

    def _new_sem(self, name):
        self.nsem += 1
        return self._stack.enter_context(self.nc.semaphore(f"{name}_{self.nsem}"))

    def region_begin(self, flag_ap, flag_tok):
        self.nregion += 1
        for eng in ENGS:
            assert not self.pending[eng] or eng == "tensor" or True
        self.region = {"id": self.nregion, "flag": flag_ap, "tok": flag_tok, "seen": set()}
        self._saved_waited = {e: dict(d) for e, d in self.waited.items()}

    def region_end(self):
        self.region = None
        self.waited = self._saved_waited
        self._saved_waited = None

    def _eng_completion(self, eng):
        cs = self.cur_sem.get(eng)
        if cs is None or (cs[1] >= SEM_ROT and self.region is None):
            cs = [self._new_sem(f"s_{eng}"), 0]
            self.cur_sem[eng] = cs
        cs[1] += 1
        return (cs[0], cs[1], 1)

    def _dma_completion(self, key):
        ds = self.dma_sems.get(key)
        if ds is None or (ds[1] >= SEM_ROT * 8 and self.region is None):
            ds = [self._new_sem("d"), 0]
            self.dma_sems[key] = ds
        ds[1] += 16
        return (ds[0], ds[1], 16)

    def op(self, eng, fn, reads=(), writes=(), dma=None, sig=True):
        rg = self.region
        if rg is not None and eng not in rg["seen"]:
            rg["seen"].add(eng)
            self.region = None
            flag = rg["flag"]
            self.op(eng, lambda e, eng=eng, flag=flag: e.reg_load(self.regs["flag_" + eng], flag), reads=[rg["tok"]], sig=False)
            self.region = rg
        deps = []
        for t in reads:
            deps.extend(self.last_w.get(t, ()))
        for t in writes:
            deps.extend(self.last_w.get(t, ()))
            deps.extend(self.readers.get(t, ()))
        need = {}
        for (s, v, _) in deps:
            k = id(s)
            if k not in need or need[k][1] < v:
                need[k] = (s, v)
        waits = []
        wd = self.waited[eng]
        for k, (s, v) in need.items():
            if wd.get(k, 0) >= v:
                continue
            wd[k] = v
            waits.append((s, v))
        rid = self.region["id"] if self.region is not None else 0
        if not sig:
            self.ops[eng].append((waits, fn, None, rid))
            self.pending[eng].extend(reads)
            return None
        if dma is not None:
            ds0 = self.dma_sems.get(dma)
            before = (ds0[0], ds0[1]) if ds0 is not None and not (ds0[1] >= SEM_ROT * 8 and self.region is None) else None
        else:
            cs0 = self.cur_sem.get(eng)
            before = (cs0[0], cs0[1]) if cs0 is not None and not (cs0[1] >= SEM_ROT and self.region is None) else None
        comp = self._dma_completion(dma) if dma is not None else self._eng_completion(eng)
        self.ops[eng].append((waits, fn, comp, rid, before))
        if dma is None:
            for t in self.pending[eng]:
                self.readers.setdefault(t, []).append(comp)
            self.pending[eng] = []
        for t in reads:
            self.readers.setdefault(t, []).append(comp)
        for t in writes:
            self.last_w[t] = [comp]
            self.readers[t] = []
        return comp

    def alias(self, new, olds):
        acc = list(self.last_w.get(new, ())) + list(self.readers.get(new, ()))
        for t in olds:
            acc.extend(self.last_w.get(t, ()))
            acc.extend(self.readers.get(t, ()))
        self.last_w[new] = acc
        self.readers[new] = []

    def final_wait(self, eng, keys):
        waits = [(self.dma_sems[k][0], self.dma_sems[k][1]) for k in keys]
        self.ops[eng].append((waits, None, None, 0))

    def emit(self, block):
        def emit_op(e, rec):
            waits, fn, comp = rec[0], rec[1], rec[2]
            for (s_, v) in waits:
                e.wait_ge(s_, v)
            if fn is not None:
                ins = fn(e)
                if comp is not None:
                    ins.then_inc(comp[0], comp[2])

        def mk(engname):
            def body(e):
                if engname == "gpsimd":
                    r = e.alloc_register("bnd")
                    e.reg_mov(r, NE * CAP - 1)
                    self.regs["bnd"] = r
                freg = e.alloc_register("rflag")
                self.regs["flag_" + engname] = freg
                ops = self.ops[engname]
                i = 0
                while i < len(ops):
                    rid = ops[i][3]
                    if rid == 0:
                        emit_op(e, ops[i])
                        i += 1
                        continue
                    j = i
                    while j < len(ops) and ops[j][3] == rid:
                        j += 1
                    run = ops[i:j]
                    with e.If_ne(freg, 0):
                        for rec in run:
                            emit_op(e, rec)
                    with e.Else():
                        tot = {}
                        for rec in run:
                            comp = rec[2]
                            if comp is None:
                                continue
                            k = id(comp[0])
                            if k not in tot:
                                before = rec[4]
                                tot[k] = [comp[0], before[1] if before is not None else 0, 0]
                            tot[k][2] += comp[2]
                        for sem_, base, total in tot.values():
                            if base > 0:
                                e.wait_ge(sem_, base)
                            e.sem_inc(sem_, total)
                    i = j
            return body
        block.sync(mk("sync"))
        block.scalar(mk("scalar"))
        block.vector(mk("vector"))
        block.gpsimd(mk("gpsimd"))
        block.tensor(mk("tensor"))


CH_XR, CH_GR, CH_Q, CH_K, CH_V, CH_GATR, CH_GATA = 0, 8, 16, 24, 26, 28, 36


def build_nc(debug=False, stop=None):
    nc = bass.Bass("TRN2", target_bir_lowering=False)

    def din(name, shape, dt=F32):
        return nc.dram_tensor(name, list(shape), dt, kind="ExternalInput").ap()

    xe = din("xe", [NST * T, D])
    ccol = din("ccol", [128, KC])
    wada = din("wada", [12, 128, KC, 512])
    bada_col = din("bada_col", [128, 48])
    bada_row = din("bada_row", [6, D])
    gam_col = din("gam_col", [128, 4, KC])
    gam_row = din("gam_row", [4, D])
    w_in_h = din("w_in_h", [44, 128, KC, 128])
    b_in_col = din("b_in_col", [128, 44])
    w_ks_h = din("w_ks_h", [2, 128, KC, 128])
    b_ks_col = din("b_ks_col", [128, 2])
    b_v_row = din("b_v_row", [256])
    conv_col = din("conv_col", [128, KC, 5])
    rgw = din("rgw", [128, 2, KC, 128])
    rgb_col = din("rgb_col", [128, 2, KC])
    lam_col = din("lam_col", [128, KC])
    sinks_row = din("sinks_row", [16])
    w_or_h = din("w_or_h", [KC, 128, KC, 128])
    w_oa_h = din("w_oa_h", [KC, 128, KC, 128])
    w_out_h = din("w_out_h", [128, KC, D])
    router_h = din("router_h", [128, KC, NE])
    router_b = din("router_b", [NE])
    NEd = NE if stop is None else 1
    w1_h = din("w1_h", [NEd, 8, 128, 2, KC, 128])
    b1_col = din("b1_col", [128, NE, 16])
    w2_h = din("w2_h", [NEd, 128, KC, D])
    b2_h = din("b2_h", [NE, D])
    abias_h = din("abias_h", [128, 16, 256])
    flags_h = din("flags_h", [128, 16])
    rc_h = din("rc_h", [128, 160])
    out = nc.dram_tensor("out", [T, D], F32, kind="ExternalOutput").ap()
    x1s = nc.dram_tensor("x1s", [T, D], F32).ap()
    xs_d = nc.dram_tensor("xs_d", [NE * CAP, D], BF16).ap()
    ys_d = nc.dram_tensor("ys_d", [NE * CAP, D], F32).ap()
    dbg = {}
    if debug:
        for nm, shp, dt in (("d_yr", [128, KC * T], BF16), ("d_ya", [128, KC * T], BF16),
                            ("d_x1", [T, D], F32), ("d_G", [128, 16 * NE], F32),
                            ("d_idx", [128, 64], U32), ("d_gv", [128, 64], F32), ("d_ada", [128, 32], F32),
                            ("d_g1b", [128, 2 * D], F32)):
            dbg[nm] = nc.dram_tensor(nm, shp, dt, kind="ExternalOutput").ap()

    with ExitStack() as es:
        S = Sched(nc, es)

        def finish(extra=()):
            keys = [k for k in (["outst", "dbg"] + list(extra)) if k in S.dma_sems]
            S.final_wait("sync", keys)
            with nc.Block() as block:
                S.emit(block)
            return nc

        def sbt(name, shape, dt=F32):
            return es.enter_context(nc.sbuf_tensor(name, list(shape), dt))

        ARW = 46720
        AR = sbt("arena", [128, ARW], F32)

        def carve(off, nbytes, dt=F32, pat=None, **kw):
            assert off % 4 == 0 and nbytes % 4 == 0 and off + nbytes <= ARW * 4, (off, nbytes)
            v = AR[:, off // 4:(off + nbytes) // 4]
            if dt != F32:
                v = v.bitcast(dt)
            if pat is not None:
                v = v.rearrange(pat, **kw)
            return v

        PS = [es.enter_context(nc.psum_tensor(f"ps{i}", [128, 512], F32)) for i in range(8)]

        ident = sbt("ident", [128, 128])
        identb = sbt("identb", [128, 128], BF16)
        ones_r = sbt("ones_r", [1, 128])
        flags = sbt("flags", [128, 16])
        ccs = sbt("ccs", [128, KC])
        scs = sbt("scs", [128, KC])
        ada = sbt("ada", [128, 32])
        badac = sbt("badac", [128, 48])
        gamc = sbt("gamc", [128, 4, KC])
        S1 = sbt("S1", [128, KC]); S2 = sbt("S2", [128, KC])
        G1b = sbt("G1b", [128, D]); G2b = sbt("G2b", [128, D])
        binc = sbt("binc", [128, 44])
        binq = sbt("binq", [128, 8])
        bflag = sbt("bflag", [128, NST, KC])
        bks = sbt("bks", [128, 2])
        vb = sbt("vb", [128, 256])
        convc = sbt("convc", [128, KC, 5])
        rgbc = sbt("rgbc", [128, 2, KC])
        lamc = sbt("lamc", [128, KC])
        cL = sbt("cL", [128, KC]); cL2 = sbt("cL2", [128, KC]); spt = sbt("spt", [128, KC]); spe = sbt("spe", [128, KC])
        sinkb = sbt("sinkb", [128, 16])
        hstate = sbt("hstate", [128, KC])
        halo = sbt("halo", [128, KC, 4])
        Gt = sbt("Gt", [128, 16, NE])
        rbb = sbt("rbb", [128, NE])
        routw = sbt("routw", [128, KC, NE])
        b1c = sbt("b1c", [128, NE, 16])
        rcf = sbt("rcf", [128, 160])
        trib = sbt("trib", [128, 128], BF16)
        onesb = sbt("onesb", [128, 128], BF16)
        onesf = sbt("onesf", [128, 128])
        MB = sbt("MB", [128, 16, NE], BF16)
        IDX = sbt("IDX", [128, 16, 4], U32)
        GV = sbt("GV", [128, 16, 4])
        FLG = sbt("FLG", [128, NE], mybir.dt.int32)
        HHALO = sbt("HHALO", [128, KC, 128], BF16)
        stat = sbt("stat", [128, 256])
        statn = [0]

        def newstat(n=16):
            i = statn[0] % 16
            statn[0] += 1
            return i * 16, f"st{i}"

        O_HT, O_YR, O_QT = 0, 32768, 65536
        O_KT, O_V, O_RING, O_R = 98304, 115712, 124416, 140800
        O_SP = 174592
        HT = carve(O_HT, 32768, BF16, "p (k t) -> p k t", k=KC)
        YR = carve(O_YR, 32768, BF16, "p (k t) -> p k t", k=KC)
        QT = carve(O_QT, 32768, BF16, "p (k t) -> p k t", k=KC)
        KT = carve(O_KT, 17408, BF16, "p (k t) -> p k t", k=4)
        VV = carve(O_V, 8704, BF16, "p (n c) -> p n c", n=17)
        RING = [carve(O_RING + 2048 * i, 2048, BF16, "p (k m) -> p k m", k=KC) for i in range(8)]
        WRG = carve(O_SP, 4096, BF16, "p (a k m) -> p a k m", a=2, k=KC)
        XC16 = carve(O_SP + 4096, 4096, BF16)
        JUNK = carve(O_SP + 8192, 4096)
        B = [carve(O_QT + 8192 * i, 8192) for i in range(4)]
        XRB = carve(O_R, 8208)
        B.append(carve(O_R + 8208, 8192))
        XST = [carve(O_R + 16400 + 8192 * i, 8192, F32, "p (j d) -> p j d", j=2) for i in range(2)]

        grow = carve(O_QT, 8192)
        browt = carve(O_QT + 8192, 8192)
        gamr = carve(O_QT + 16384, 8192)

        ring_n = [0]

        def ring_load(src):
            i = ring_n[0] % 8
            ring_n[0] += 1
            S.op("gpsimd", lambda e, i=i, src=src: e.dma_start(out=RING[i], in_=src),
                 writes=[f"ring{i}"], dma=f"ring{i}")
            return RING[i], f"ring{i}"

        def small_load(dst, src, tok):
            S.op("sync", lambda e: e.dma_start(out=dst, in_=src), writes=[tok], dma=tok)

        small_load(flags[:], flags_h, "flags")
        small_load(ccs[:], ccol, "ccs")
        small_load(badac[:], bada_col, "badac")
        small_load(gamc[:], gam_col, "gamc")
        small_load(binc[:], b_in_col, "binc")
        small_load(bks[:], b_ks_col, "bks")
        small_load(vb[:], b_v_row.partition_broadcast(128), "vb")
        small_load(convc[:], conv_col, "convc")
        small_load(rgbc[:], rgb_col, "rgbc")
        small_load(lamc[:], lam_col, "lamc")
        small_load(sinkb[:], sinks_row.partition_broadcast(128), "sinkb")
        small_load(rbb[:], router_b.partition_broadcast(128), "rbb")
        small_load(routw[:], router_h, "routw")
        small_load(b1c[:], b1_col, "b1c")
        small_load(rcf[:], rc_h, "rcf")
        small_load(browt[0:1, 0:D], bada_row[2:3, :], "browt0")
        small_load(browt[0:1, D:2 * D], bada_row[5:6, :], "browt1")
        small_load(gamr[0:1, 0:D], gam_row[1:2, :], "gamr0")
        small_load(gamr[0:1, D:2 * D], gam_row[3:4, :], "gamr1")
        S.op("gpsimd", lambda e: e.dma_start(out=WRG, in_=rgw), writes=["wrg"], dma="wrg")

        S.op("gpsimd", lambda e: e.memset(ident[:], 0.0), writes=["ident"])
        S.op("gpsimd", lambda e: e.affine_select(out=ident[:], in_=ident[:], pattern=[[-1, 128]],
                                                  compare_op=ALU.not_equal, fill=1.0, base=0,
                                                  channel_multiplier=1), reads=["ident"], writes=["ident"])
        S.op("vector", lambda e: e.tensor_copy(out=identb[:], in_=ident[:]), reads=["ident"], writes=["identb"])
        S.op("vector", lambda e: e.memset(ones_r[:], 1.0), writes=["ones_r"])
        S.op("vector", lambda e: e.memset(onesb[:], 1.0), writes=["onesb"])
        S.op("vector", lambda e: e.memset(onesf[:], 1.0), writes=["onesf"])
        S.op("vector", lambda e: e.tensor_copy(out=trib[:], in_=rcf[:, 32:160]), reads=["rcf"], writes=["trib"])
        S.op("vector", lambda e: e.memset(hstate[:], 0.0), writes=["hstate"])
        S.op("vector", lambda e: e.memset(halo[:], 0.0), writes=["halo"])
        S.op("vector", lambda e: e.tensor_scalar(out=binq[:], in0=binc[:, CH_Q:CH_Q + 8], scalar1=0.125, scalar2=None,
                                                 op0=ALU.mult), reads=["binc"], writes=["binq"])
        for st_ in range(NST):
            S.op("vector", lambda e, st_=st_: e.tensor_scalar(out=bflag[:, st_, :], in0=binc[:, CH_XR:CH_XR + 8], scalar1=flags[:, st_:st_ + 1],
                                                              scalar2=None, op0=ALU.mult), reads=["binc", "flags"], writes=["bflag"])
        S.op("vector", lambda e: e.tensor_scalar(out=b1c[:, :, 8:16], in0=b1c[:, :, 8:16], scalar1=1.0, scalar2=None,
                                                 op0=ALU.add), reads=["b1c"], writes=["b1c"])
        S.op("scalar", lambda e: e.activation(out=spe[:], in_=lamc[:], func=AF.Exp, scale=-1.0), reads=["lamc"], writes=["spe"])
        S.op("vector", lambda e: e.tensor_scalar(out=spt[:], in0=spe[:], scalar1=-0.2, scalar2=0.25, op0=ALU.mult, op1=ALU.add),
             reads=["spe"], writes=["spt"])
        for cst in (1.0 / 3.0, 0.5, 1.0):
            S.op("vector", lambda e: e.tensor_tensor(out=spt[:], in0=spt[:], in1=spe[:], op=ALU.mult), reads=["spt", "spe"], writes=["spt"])
            S.op("vector", lambda e, cst=cst: e.tensor_scalar(out=spt[:], in0=spt[:], scalar1=-1.0, scalar2=cst, op0=ALU.mult, op1=ALU.add),
                 reads=["spt"], writes=["spt"])
        S.op("vector", lambda e: e.tensor_tensor(out=spt[:], in0=spt[:], in1=spe[:], op=ALU.mult), reads=["spt", "spe"], writes=["spt"])
        S.op("vector", lambda e: e.tensor_scalar(out=cL[:], in0=spt[:], scalar1=-8.0, scalar2=None, op0=ALU.mult), reads=["spt"], writes=["cL"])
        S.op("vector", lambda e: e.tensor_scalar(out=cL2[:], in0=spt[:], scalar1=-16.0, scalar2=None, op0=ALU.mult), reads=["spt"], writes=["cL2"])

        S.op("scalar", lambda e: e.activation(out=scs[:], in_=ccs[:], func=AF.Silu), reads=["ccs"], writes=["scs"])
        WA = [carve(O_YR + 8192 * i, 8192, BF16, "p (k n) -> p k n", k=KC) for i in range(3)]
        scs16 = sbt("scs16", [128, KC], BF16)
        S.op("vector", lambda e: e.tensor_copy(out=scs16[:], in_=scs[:]), reads=["scs"], writes=["scs16"])
        col_pieces = {0: 0, 1: 4, 2: 8, 3: 12, 6: 16, 7: 20, 8: 24, 9: 28}
        row_pieces = {4: 0, 5: 512, 10: 1024, 11: 1536}
        for pc in range(12):
            wa = WA[pc % 3]
            tk = f"wa{pc % 3}"
            for hk in range(2):
                S.op("gpsimd", lambda e, wa=wa, pc=pc, hk=hk: e.dma_start(out=wa[:, 4 * hk:4 * hk + 4, :], in_=wada[pc][:, 4 * hk:4 * hk + 4, :]),
                     writes=[tk], dma=tk)
            if pc in col_pieces:
                base = col_pieces[pc]
                for sub in range(4):
                    for k in range(KC):
                        S.op("tensor", lambda e, wa=wa, sub=sub, k=k: e.matmul(
                            PS[0][:, sub:sub + 1], lhsT=wa[:, k, sub * 128:(sub + 1) * 128], rhs=scs16[:, k:k + 1],
                            start=(k == 0), stop=(k == KC - 1)), reads=[tk, "scs16"], writes=["ps0"], sig=(k == KC - 1))
                S.op("vector", lambda e, base=base, pc=pc: e.tensor_tensor(
                    out=ada[:, base:base + 4], in0=PS[0][:, 0:4], in1=badac[:, pc * 4:pc * 4 + 4], op=ALU.add),
                    reads=["ps0", "badac"], writes=["ada"])
            else:
                ro = row_pieces[pc]
                for k in range(KC):
                    S.op("tensor", lambda e, wa=wa, k=k: e.matmul(
                        PS[1][0:1, :], lhsT=scs16[:, k:k + 1], rhs=wa[:, k, :], start=(k == 0), stop=(k == KC - 1)),
                        reads=[tk, "scs16"], writes=["ps1"], sig=(k == KC - 1))
                S.op("vector", lambda e, ro=ro: e.tensor_tensor(out=grow[0:1, ro:ro + 512], in0=PS[1][0:1, :],
                                                                in1=browt[0:1, ro:ro + 512], op=ALU.add),
                     reads=["ps1", "browt0", "browt1"], writes=["grow"])
                S.op("vector", lambda e, ro=ro: e.tensor_tensor(out=grow[0:1, ro:ro + 512], in0=grow[0:1, ro:ro + 512],
                                                                in1=gamr[0:1, ro:ro + 512], op=ALU.mult),
                     reads=["grow", "gamr0", "gamr1"], writes=["grow"])
                S.op("tensor", lambda e, ro=ro: e.matmul(PS[2][:, :], lhsT=ones_r[0:1, :], rhs=grow[0:1, ro:ro + 512],
                                                         start=True, stop=True), reads=["grow", "ones_r"], writes=["ps2"])
                dstb = G1b if ro < 1024 else G2b
                S.op("vector", lambda e, ro=ro, dstb=dstb: e.tensor_copy(out=dstb[:, (ro % 1024):(ro % 1024) + 512], in_=PS[2][:, :]),
                     reads=["ps2"], writes=["G1b" if ro < 1024 else "G2b"])
        S.op("vector", lambda e: e.scalar_tensor_tensor(out=S1[:], in0=ada[:, 8:16], scalar=1.0, in1=gamc[:, 0, :], op0=ALU.add, op1=ALU.mult),
             reads=["ada", "gamc"], writes=["S1"])
        S.op("vector", lambda e: e.scalar_tensor_tensor(out=S2[:], in0=ada[:, 24:32], scalar=1.0, in1=gamc[:, 2, :], op0=ALU.add, op1=ALU.mult),
             reads=["ada", "gamc"], writes=["S2"])
        S.alias("YR", ["wa0", "wa1", "wa2"])
        S.alias("ga0", ["grow"]); S.alias("ga1", ["grow"]); S.alias("gi0", ["browt0", "browt1"]); S.alias("gi1", ["browt0", "browt1"]); S.alias("ta0", ["gamr0", "gamr1"]); S.alias("ta1", ["gamr0", "gamr1"])
        if debug:
            S.op("sync", lambda e: e.dma_start(out=dbg["d_ada"], in_=ada[:]), reads=["ada"], dma="dbg")
            S.op("sync", lambda e: e.dma_start(out=dbg["d_g1b"][:, 0:D], in_=G1b[:]), reads=["G1b"], dma="dbg")
            S.op("sync", lambda e: e.dma_start(out=dbg["d_g1b"][:, D:2 * D], in_=G2b[:]), reads=["G2b"], dma="dbg")

        if stop == "p0":
            return finish()

        def norm_to_T(src_rows, row0, ntile2, scale_col, shift_col, dstT, dst_tok, col0, dst_f32=None):
            xs = XST[ntile2 % 2]
            tk = f"xst{ntile2 % 2}"
            S.op("sync", lambda e: e.dma_start(out=xs, in_=src_rows[row0:row0 + 256, :].rearrange("(j p) d -> p j d", p=128)),
                 writes=[tk], dma=tk)
            so, stk = newstat()
            for j in range(2):
                S.op("vector", lambda e, j=j: e.scalar_tensor_tensor(out=JUNK, in0=xs[:, j, :], scalar=1.0, in1=xs[:, j, :], op0=ALU.mult, op1=ALU.mult,
                                                                     accum_out=stat[:, so + j:so + j + 1]),
                     reads=[tk], writes=["junk", stk])
            S.op("scalar", lambda e: e.activation(out=stat[:, so + 2:so + 4], in_=stat[:, so:so + 2], func=AF.Sqrt, scale=1.0 / D, bias=EPS),
                 reads=[stk], writes=[stk])
            S.op("vector", lambda e: e.reciprocal(out=stat[:, so + 2:so + 4], in_=stat[:, so + 2:so + 4]), reads=[stk], writes=[stk])
            for j in range(2):
                S.op("vector", lambda e, j=j: e.tensor_scalar(out=xs[:, j, :], in0=xs[:, j, :], scalar1=stat[:, so + 2 + j:so + 3 + j],
                                                               scalar2=None, op0=ALU.mult),
                     reads=[tk, stk], writes=[tk])
            for half in range(4):
                pb = PS[4 + half]
                pt = f"ps{4 + half}"
                for kk in range(2):
                    k = half * 2 + kk
                    for j in range(2):
                        S.op("tensor", lambda e, k=k, j=j, kk=kk, pb=pb: e.transpose(
                            out=pb[:, (kk * 2 + j) * 128:(kk * 2 + j + 1) * 128], in_=xs[:, j, k * 128:(k + 1) * 128], identity=ident[:]),
                            reads=[tk, "ident"], writes=[pt], sig=(kk == 1 and j == 1))
                for kk in range(2):
                    k = half * 2 + kk
                    S.op("scalar", lambda e, k=k, kk=kk, pb=pb: e.activation(
                        out=dstT[:, k, col0:col0 + 256], in_=pb[:, kk * 256:(kk + 1) * 256], func=AF.Identity,
                        bias=shift_col[:, k:k + 1], scale=scale_col[:, k:k + 1]),
                        reads=[pt, "ada", "S1", "S2"], writes=[dst_tok])

        def proj_T(chunk_src, rhsT, rhs_tok, ncols, col0, evac):
            w, wt = ring_load(chunk_src)
            for i in range((ncols + 511) // 512):
                n = min(512, ncols - i * 512)
                pb = PS[i % 4]
                pt = f"ps{i % 4}"
                for k in range(KC):
                    S.op("tensor", lambda e, k=k, pb=pb, n=n, i=i: e.matmul(
                        pb[:, 0:n], lhsT=w[:, k, :], rhs=rhsT[:, k, col0 + i * 512:col0 + i * 512 + n],
                        start=(k == 0), stop=(k == KC - 1)), reads=[wt, rhs_tok], writes=[pt], sig=(k == KC - 1))
                evac(i, pb, pt, i * 512, n)

        XRBs = [XRB, carve(O_KT, 8208)]
        XCs = [B[4], carve(O_KT + 8208, 8192)]
        XC16s = [XC16, carve(O_KT + 16400, 4096, BF16)]
        GA, GI, TA_, TM = B[0], B[1], B[2], B[3]
        ntile2 = [0]

        def rnn_norm(st):
            for g2 in range(T // 256):
                norm_to_T(xe, st * T + g2 * 256, ntile2[0], S1, ada[:, 0:8], HT, "HT", g2 * 256)
                ntile2[0] += 1
            if st == NST - 2:
                S.op("gpsimd", lambda e: e.tensor_copy(out=HHALO[:], in_=HT[:, :, T - 128:T]), reads=["HT"], writes=["hhalo"])

        def rnn_A(st, c):
            sx = (st * KC + c) % 2
            xrb, xc, xc16 = XRBs[sx], XCs[sx], XC16s[sx]
            xrbt, xct, xc16t = f"xrb{sx}", f"xc{sx}", f"xc16{sx}"
            S.op("vector", lambda e: e.tensor_copy(out=xrb[:, 0:3], in_=halo[:, c, 0:3]), reads=["halo"], writes=[xrbt])

            def ev_xr(i, pb, pt, c0, n):
                if i % 2 == 0:
                    S.op("scalar", lambda e: e.activation(out=xrb[:, 3 + c0:3 + c0 + n], in_=pb[:, 0:n], func=AF.Identity,
                                                          bias=bflag[:, st, c:c + 1], scale=flags[:, st:st + 1]),
                         reads=[pt, "bflag", "flags"], writes=[xrbt])
                else:
                    S.op("vector", lambda e: e.tensor_scalar(out=xrb[:, 3 + c0:3 + c0 + n], in0=pb[:, 0:n], scalar1=binc[:, c:c + 1],
                                                             scalar2=flags[:, st:st + 1], op0=ALU.add, op1=ALU.mult),
                         reads=[pt, "binc", "flags"], writes=[xrbt])
            proj_T(w_in_h[CH_XR + c], HT, "HT", T, 0, ev_xr)
            S.op("vector", lambda e: e.tensor_copy(out=halo[:, c, 0:3], in_=xrb[:, T:T + 3]), reads=[xrbt], writes=["halo"])
            S.op("vector", lambda e: e.tensor_scalar(out=xc, in0=xrb[:, 0:T], scalar1=convc[:, c, 0:1], scalar2=convc[:, c, 4:5],
                                                     op0=ALU.mult, op1=ALU.add), reads=[xrbt, "convc"], writes=[xct])
            for kk in range(1, 4):
                S.op("vector", lambda e, kk=kk: e.scalar_tensor_tensor(out=xc, in0=xrb[:, kk:kk + T], scalar=convc[:, c, kk:kk + 1],
                                                                       in1=xc, op0=ALU.mult, op1=ALU.add),
                     reads=[xrbt, "convc", xct], writes=[xct])
            S.op("scalar", lambda e: e.copy(out=xc16, in_=xc), reads=[xct], writes=[xc16t])

        def rnn_B(st, c):
            own = (st == NST - 1)
            sx = (st * KC + c) % 2
            xc, xc16 = XCs[sx], XC16s[sx]
            xct, xc16t = f"xc{sx}", f"xc16{sx}"
            HH = xc
            TH = T // 2
            for h in range(2):
                cs = slice(h * TH, (h + 1) * TH)
                gat, git, tat, tmt = f"ga{h}", f"gi{h}", f"ta{h}", f"tm{h}"
                for i2 in range(2):
                    i = 2 * h + i2
                    for a in range(2):
                        pb = PS[4 + 2 * i2 + a]
                        pt = f"ps{4 + 2 * i2 + a}"
                        S.op("tensor", lambda e, a=a, i=i, pb=pb: e.matmul(pb[:, :], lhsT=WRG[:, a, c, :], rhs=xc16[:, i * 512:(i + 1) * 512],
                                                                           start=True, stop=True), reads=["wrg", xc16t], writes=[pt])
                        dst = GA if a == 0 else GI
                        S.op("scalar", lambda e, a=a, i=i, pb=pb, dst=dst: e.activation(
                            out=dst[:, i * 512:(i + 1) * 512], in_=pb[:, :], func=AF.Sigmoid, bias=rgbc[:, a, c:c + 1], scale=1.0),
                            reads=[pt, "rgbc"], writes=[gat if a == 0 else git])
                S.op("scalar", lambda e, cs=cs: e.activation(out=TA_[:, cs], in_=GA[:, cs], func=AF.Exp, scale=cL[:, c:c + 1]), reads=[gat, "cL"], writes=[tat])
                S.op("scalar", lambda e, cs=cs: e.activation(out=TM[:, cs], in_=GA[:, cs], func=AF.Exp, scale=cL2[:, c:c + 1]), reads=[gat, "cL2"], writes=[tmt])
                S.op("vector", lambda e, cs=cs: e.tensor_scalar(out=TM[:, cs], in0=TM[:, cs], scalar1=1.0, scalar2=-1.0, op0=ALU.min, op1=ALU.mult),
                     reads=[tmt], writes=[tmt])
                S.op("scalar", lambda e, cs=cs: e.activation(out=TM[:, cs], in_=TM[:, cs], func=AF.Sqrt, scale=1.0, bias=1.0), reads=[tmt], writes=[tmt])
                if h == 0:
                    S.op("vector", lambda e: e.tensor_tensor(out=TM[:, 0:1], in0=TM[:, 0:1], in1=flags[:, 4 + st:5 + st], op=ALU.max),
                         reads=[tmt, "flags"], writes=[tmt])
                S.op("vector", lambda e, cs=cs: e.scalar_tensor_tensor(out=GI[:, cs], in0=xc[:, cs], scalar=flags[:, st:st + 1], in1=GI[:, cs],
                                                                      op0=ALU.mult, op1=ALU.mult), reads=[xct, git, "flags"], writes=[git])
                S.op("vector", lambda e, cs=cs: e.tensor_tensor(out=GI[:, cs], in0=GI[:, cs], in1=TM[:, cs], op=ALU.mult), reads=[git, tmt], writes=[git])
                init = hstate[:, c:c + 1] if h == 0 else HH[:, TH - 1:TH]
                S.op("vector", lambda e, cs=cs, init=init: e.tensor_tensor_scan(out=HH[:, cs], data0=TA_[:, cs], data1=GI[:, cs], initial=init,
                                                                                op0=ALU.mult, op1=ALU.add),
                     reads=[tat, git, "hstate", xct], writes=[xct])
            S.op("vector", lambda e: e.tensor_copy(out=hstate[:, c:c + 1], in_=HH[:, T - 1:T]), reads=[xct], writes=["hstate"])
            if own:
                XG, X2 = B[0], B[1]
                gaA, giA = ["ga0", "ga1"], ["gi0", "gi1"]

                def ev_gr(i, pb, pt, c0, n):
                    S.op("scalar", lambda e: e.activation(out=XG[:, c0:c0 + n], in_=pb[:, 0:n], func=AF.Identity,
                                                          bias=binc[:, CH_GR + c:CH_GR + c + 1], scale=1.0),
                         reads=[pt, "binc"], writes=gaA)
                proj_T(w_in_h[CH_GR + c], HT, "HT", T, 0, ev_gr)
                S.op("vector", lambda e: e.tensor_tensor(out=X2, in0=XG, in1=XG, op=ALU.mult), reads=gaA, writes=giA)
                S.op("vector", lambda e: e.tensor_scalar(out=X2, in0=X2, scalar1=0.044715, scalar2=1.0, op0=ALU.mult, op1=ALU.add),
                     reads=giA, writes=giA)
                S.op("vector", lambda e: e.tensor_tensor(out=X2, in0=X2, in1=XG, op=ALU.mult), reads=giA + gaA, writes=giA)
                S.op("scalar", lambda e: e.activation(out=X2, in_=X2, func=AF.Sigmoid, scale=1.5957691216057308), reads=giA, writes=giA)
                S.op("vector", lambda e: e.tensor_tensor(out=X2, in0=X2, in1=XG, op=ALU.mult), reads=giA + gaA, writes=giA)
                S.op("vector", lambda e: e.tensor_tensor(out=YR[:, c, :], in0=HH, in1=X2, op=ALU.mult), reads=[xct] + giA, writes=["YR"])

        seq = [(st, c) for st in range(NST) for c in range(KC)]
        rnn_norm(0)
        rnn_A(*seq[0])
        for n in range(len(seq)):
            if n + 1 < len(seq):
                st1, c1 = seq[n + 1]
                if c1 == 0:
                    rnn_norm(st1)
                rnn_A(st1, c1)
            rnn_B(*seq[n])
        if debug:
            S.op("sync", lambda e: e.dma_start(out=dbg["d_yr"], in_=YR.rearrange("p k t -> p (k t)")), reads=["YR"], dma="dbg")

        if stop == "p1":
            return finish()

        S.alias("QT", ["ga0", "ga1", "gi0", "gi1", "ta0", "ta1", "tm0", "tm1", "xc0"])
        S.alias("KT", ["xrb1", "xc1", "xc161"]); S.alias("VV", ["xrb1", "xc1", "xc161"])
        for kc in range(4):
            def ev_kh(i, pb, pt, c0, n, kc=kc):
                bcol = binc[:, CH_K + kc:CH_K + kc + 1] if kc < 2 else bks[:, kc - 2:kc - 1]
                S.op("scalar", lambda e: e.activation(out=KT[:, kc, 0:128], in_=pb[:, 0:128], func=AF.Identity, bias=bcol, scale=1.0),
                     reads=[pt, "binc", "bks"], writes=["KT"])
            proj_T(w_in_h[CH_K + kc] if kc < 2 else w_ks_h[kc - 2], HHALO, "hhalo", 128, 0, ev_kh)
        for vc in range(2):
            w_, wt_ = ring_load(w_in_h[CH_V + vc])
            for k in range(KC):
                S.op("tensor", lambda e, k=k, w_=w_, vc=vc: e.matmul(PS[0][:, vc * 128:(vc + 1) * 128], lhsT=HHALO[:, k, :],
                                                                   rhs=w_[:, k, :], start=(k == 0), stop=(k == KC - 1)),
                     reads=[wt_, "hhalo"], writes=["ps0"], sig=(k == KC - 1))
        S.op("vector", lambda e: e.tensor_tensor(out=VV[:, 0, :], in0=PS[0][:, 0:256], in1=vb[:], op=ALU.add),
             reads=["ps0", "vb"], writes=["VV"])
        for qc in range(8):
            def ev_q(i, pb, pt, c0, n, qc=qc):
                S.op("scalar", lambda e: e.activation(out=QT[:, qc, c0:c0 + n], in_=pb[:, 0:n], func=AF.Identity,
                                                      bias=binq[:, qc:qc + 1], scale=0.125), reads=[pt, "binq"], writes=["QT"])
            proj_T(w_in_h[CH_Q + qc], HT, "HT", T, 0, ev_q)
        for kc in range(4):
            def ev_k(i, pb, pt, c0, n, kc=kc):
                bcol = binc[:, CH_K + kc:CH_K + kc + 1] if kc < 2 else bks[:, kc - 2:kc - 1]
                S.op("scalar", lambda e: e.activation(out=KT[:, kc, 128 + c0:128 + c0 + n], in_=pb[:, 0:n], func=AF.Identity, bias=bcol, scale=1.0),
                     reads=[pt, "binc", "bks"], writes=["KT"])
            proj_T(w_in_h[CH_K + kc] if kc < 2 else w_ks_h[kc - 2], HT, "HT", T, 0, ev_k)
        wv = [ring_load(w_in_h[CH_V + vc]) for vc in range(2)]
        for tt in range(16):
            pb = PS[tt % 4]; pt = f"ps{tt % 4}"
            for vc in range(2):
                for k in range(KC):
                    S.op("tensor", lambda e, k=k, vc=vc, tt=tt, pb=pb: e.matmul(pb[:, vc * 128:(vc + 1) * 128], lhsT=HT[:, k, tt * 128:(tt + 1) * 128],
                                                                             rhs=wv[vc][0][:, k, :], start=(k == 0), stop=(k == KC - 1)),
                         reads=[wv[vc][1], "HT"], writes=[pt], sig=(k == KC - 1))
            S.op("vector", lambda e, tt=tt, pb=pb: e.tensor_tensor(out=VV[:, 1 + tt, :], in0=pb[:, 0:256], in1=vb[:], op=ALU.add),
                 reads=[pt, "vb"], writes=["VV"])

        if stop == "p2":
            return finish()

        S.alias("attR", ["xrb0", "xc0", "xst0", "xst1"])
        SS = [carve(O_R + 4096 * i, 4096, F32, "p (h c) -> p h c", h=4) for i in range(2)]
        PN = [carve(O_R + 8192 + 2048 * i, 2048, BF16, "p (h c) -> p h c", h=4) for i in range(2)]
        PTB = [carve(O_R + 12288 + 2048 * i, 2048, BF16, "p (b c) -> p b c", b=2) for i in range(2)]
        ABI = carve(O_R + 16384, 16384, F32, "p (h c) -> p h c", h=16)
        S.op("sync", lambda e: e.dma_start(out=ABI, in_=abias_h), writes=["attR"], dma="abi")
        S.alias("abi", ["attR"]); S.alias("ss0", ["attR"]); S.alias("ss1", ["attR"]); S.alias("pn0", ["attR"]); S.alias("pn1", ["attR"])
        S.alias("ptb0", ["attR"]); S.alias("ptb1", ["attR"])
        def att_stage1(it, qb, g):
            par = it % 2
            ss, pn = SS[par], PN[par]
            sst, pnt = f"ss{par}", f"pn{par}"
            pS = (PS[0], PS[1]) if par == 0 else (PS[2], PS[3])
            pSt = ("ps0", "ps1") if par == 0 else ("ps2", "ps3")
            for hh in (0, 2, 1, 3):
                h = 4 * g + hh
                ch, hp = h // 2, h % 2
                po = 64 * hp
                kch = (g // 2) if (g % 2) == hp else 2 + (g // 2)
                pb = pS[hp]
                S.op("tensor", lambda e, ch=ch, po=po, qb=qb, kch=kch, hh=hh, pb=pb: e.matmul(
                    pb[:, (hh // 2) * 256:(hh // 2) * 256 + 256], lhsT=QT[po:po + 64, ch, qb * 128:(qb + 1) * 128],
                    rhs=KT[po:po + 64, kch, qb * 128:qb * 128 + 256], start=True, stop=True),
                    reads=["QT", "KT"], writes=[pSt[hp]], sig=(hh // 2 == 1))
            for half in range(2):
                S.op("vector", lambda e, half=half, g=g, ss=ss, pS=pS: e.tensor_tensor(
                    out=ss[:, 2 * half:2 * half + 2, :], in0=pS[half][:, :].rearrange("p (h c) -> p h c", h=2),
                    in1=ABI[:, 4 * g + half:4 * g + half + 3:2, :], op=ALU.add),
                    reads=[pSt[half], "abi"], writes=[sst])
            if qb == 0:
                S.op("vector", lambda e, ss=ss: e.tensor_scalar(out=ss[:, :, 0:128], in0=ss[:, :, 0:128], scalar1=flags[:, 8:9], scalar2=None,
                                                                op0=ALU.add), reads=[sst, "flags"], writes=[sst])
            so, stk = newstat()
            mx, nmx, rs, es_ = stat[:, so:so + 4], stat[:, so + 4:so + 8], stat[:, so + 8:so + 12], stat[:, so + 12:so + 16]
            S.op("vector", lambda e, ss=ss, mx=mx: e.tensor_reduce(out=mx, in_=ss, axis=AX.X, op=ALU.max), reads=[sst], writes=[stk])
            S.op("vector", lambda e, mx=mx, g=g: e.tensor_tensor(out=mx, in0=mx, in1=sinkb[:, 4 * g:4 * g + 4], op=ALU.max),
                 reads=[stk, "sinkb"], writes=[stk])
            S.op("vector", lambda e, mx=mx, nmx=nmx: e.tensor_scalar(out=nmx, in0=mx, scalar1=-1.0, scalar2=None, op0=ALU.mult),
                 reads=[stk], writes=[stk])
            att_ctx[it] = (so, stk)

        def att_stage1b(it, qb, g):
            par = it % 2
            ss, pn = SS[par], PN[par]
            sst, pnt = f"ss{par}", f"pn{par}"
            so, stk = att_ctx.pop(it)
            mx, nmx, rs, es_ = stat[:, so:so + 4], stat[:, so + 4:so + 8], stat[:, so + 8:so + 12], stat[:, so + 12:so + 16]
            for hh in range(4):
                S.op("scalar", lambda e, hh=hh, ss=ss, nmx=nmx, rs=rs: e.activation(
                    out=ss[:, hh, :], in_=ss[:, hh, :], func=AF.Exp, bias=nmx[:, hh:hh + 1], scale=1.0, accum_out=rs[:, hh:hh + 1]),
                    reads=[sst, stk], writes=[sst, stk])
            S.op("vector", lambda e, mx=mx, g=g, es_=es_: e.tensor_tensor(out=es_, in0=sinkb[:, 4 * g:4 * g + 4], in1=mx, op=ALU.subtract),
                 reads=[stk, "sinkb"], writes=[stk])
            S.op("scalar", lambda e, es_=es_: e.activation(out=es_, in_=es_, func=AF.Exp), reads=[stk], writes=[stk])
            S.op("vector", lambda e, rs=rs, es_=es_: e.tensor_tensor(out=rs, in0=rs, in1=es_, op=ALU.add), reads=[stk], writes=[stk])
            S.op("vector", lambda e, rs=rs: e.reciprocal(out=rs, in_=rs), reads=[stk], writes=[stk])
            for pos in range(4):
                S.op("vector", lambda e, pos=pos, ss=ss, pn=pn, rs=rs: e.tensor_scalar(
                    out=pn[:, pos, :], in0=ss[:, pos, :], scalar1=rs[:, pos:pos + 1], scalar2=None, op0=ALU.mult),
                    reads=[sst, stk], writes=[pnt])

        def att_stage2(it, qb, g):
            par = it % 2
            pn, ptb = PN[par], PTB[par]
            pnt, ptbt = f"pn{par}", f"ptb{par}"
            pT = PS[4 + par]
            pTt = f"ps{4 + par}"
            pTb = pT[:, 0:512].bitcast(BF16).rearrange("p (b c) -> p b c", b=2)
            for pos in range(4):
                for kb in range(2):
                    S.op("tensor", lambda e, pos=pos, kb=kb, pn=pn, pTb=pTb: e.transpose(
                        out=pTb[:, kb, pos * 128:(pos + 1) * 128], in_=pn[:, pos, kb * 128:(kb + 1) * 128], identity=identb[:]),
                        reads=[pnt, "identb"], writes=[pTt], sig=(pos == 3 and kb == 1))
            S.op("scalar", lambda e, ptb=ptb, pTb=pTb: e.copy(out=ptb, in_=pTb), reads=[pTt], writes=[ptbt])
            pO = PS[6 + par]
            pOt = f"ps{6 + par}"
            for eo in range(2):
                for kb in range(2):
                    S.op("tensor", lambda e, eo=eo, kb=kb, qb=qb, g=g, ptb=ptb, pO=pO: e.matmul(
                        pO[64 * eo:64 * eo + 64, 0:256], lhsT=VV[:, qb + kb, 64 * g:64 * g + 64], rhs=ptb[:, kb, 256 * eo:256 * eo + 256],
                        start=(kb == 0), stop=(kb == 1)), reads=["VV", ptbt], writes=[pOt], sig=(eo == 1 and kb == 1))
            S.op("vector", lambda e, qb=qb, g=g, pO=pO: e.tensor_copy(
                out=QT[:, 2 * g:2 * g + 2, qb * 128:(qb + 1) * 128], in_=pO[:, 0:256].rearrange("p (c t) -> p c t", c=2)),
                reads=[pOt], writes=["yaW"])

        att_ctx = {}
        iters = [(qb, g) for qb in range(16) for g in range(4)]
        for it in range(len(iters) + 2):
            if it < len(iters):
                att_stage1(it, *iters[it])
            if 1 <= it <= len(iters):
                att_stage1b(it - 1, *iters[it - 1])
            if it >= 2:
                att_stage2(it - 2, *iters[it - 2])
        S.alias("QTy", ["QT", "yaW"])
        if debug:
            S.op("sync", lambda e: e.dma_start(out=dbg["d_ya"], in_=QT.rearrange("p k t -> p (k t)")), reads=["QTy"], dma="dbg")

        if stop == "p3":
            return finish()

        S.alias("p4R", ["abi", "ss0", "ss1", "pn0", "pn1", "ptb0", "ptb1"])
        S.alias("p4K", ["KT", "VV"])
        MG = [carve(O_R + 8192 * i, 8192, BF16, "p (k t) -> p k t", k=KC) for i in range(2)]
        XT4 = carve(O_R + 16384, 4096)
        MIXT = carve(O_R + 20480, 4096)
        H2F = carve(O_R + 24576, 4096, F32, "p (k t) -> p k t", k=KC)
        SG = [carve(O_R + 28672 + 2048 * i, 2048) for i in range(2)]
        WOUT = carve(O_KT, 16384, BF16, "p (k n) -> p k n", k=KC)
        for q4 in range(4):
            S.op("gpsimd", lambda e, q4=q4: e.dma_start(out=WOUT[:, 2 * q4:2 * q4 + 2, :], in_=w_out_h[:, 2 * q4:2 * q4 + 2, :]),
                 writes=["p4K"], dma="wout")
        S.alias("wout", ["p4K"])
        for nm in ("mg0", "mg1", "xt4", "mixt", "h2f", "sg0", "sg1"):
            S.alias(nm, ["p4R"])
        S2B = carve(O_V, 4096)
        SH2B = carve(O_V + 4096, 4096)
        H2TOK = [carve(O_SP + 2048 * i, 2048, BF16) for i in range(2)]
        S.alias("h2t0", ["wrg"]); S.alias("h2t1", ["wrg"])
        S.alias("s2b", ["KT", "VV"])
        for which, dstb in ((0, S2B), (1, SH2B)):
            for k in range(KC):
                src = S2[:, k:k + 1] if which == 0 else ada[:, 16 + k:17 + k]
                S.op("vector", lambda e, src=src: e.tensor_scalar(out=SG[0][:, 0:128], in0=ident[:], scalar1=src, scalar2=None, op0=ALU.mult),
                     reads=["ident", "S2", "ada", "sg0"], writes=["sg0"])
                S.op("tensor", lambda e: e.matmul(PS[5][:, 0:128], lhsT=onesf[:], rhs=SG[0][:, 0:128], start=True, stop=True),
                     reads=["sg0", "onesf"], writes=["ps5"])
                S.op("vector", lambda e, k=k, dstb=dstb: e.tensor_copy(out=dstb[:, k * 128:(k + 1) * 128], in_=PS[5][:, 0:128]),
                     reads=["ps5"], writes=["s2b"])
        def p4_merge(tt):
            mg = MG[tt % 2]; mgt = f"mg{tt % 2}"
            c0 = tt * 512
            for f in range(KC):
                wr, wrt = ring_load(w_in_h[CH_GATR + f])
                wa_, wat = ring_load(w_in_h[CH_GATA + f])
                wor, wort = ring_load(w_or_h[f])
                woa, woat = ring_load(w_oa_h[f])
                specs = ((wr, wrt, HT, "HT"), (wa_, wat, HT, "HT"), (wor, wort, YR, "YR"), (woa, woat, QT, "QTy"))
                for j, (w, wt, rT, rtok) in enumerate(specs):
                    for k in range(KC):
                        S.op("tensor", lambda e, j=j, k=k, w=w, rT=rT, c0=c0: e.matmul(PS[j][:, :], lhsT=w[:, k, :], rhs=rT[:, k, c0:c0 + 512],
                                                                            start=(k == 0), stop=(k == KC - 1)),
                             reads=[wt, rtok], writes=[f"ps{j}"], sig=(k == KC - 1))
                for j in range(2):
                    bci = (CH_GATR if j == 0 else CH_GATA) + f
                    S.op("scalar", lambda e, j=j, bci=bci: e.activation(out=SG[j], in_=PS[j][:, :], func=AF.Sigmoid, bias=binc[:, bci:bci + 1], scale=1.0),
                         reads=[f"ps{j}", "binc"], writes=[f"sg{j}"])
                S.op("vector", lambda e: e.tensor_tensor(out=SG[0], in0=SG[0], in1=PS[2][:, :], op=ALU.mult), reads=["sg0", "ps2"], writes=["sg0"])
                S.op("vector", lambda e: e.tensor_tensor(out=SG[1], in0=SG[1], in1=PS[3][:, :], op=ALU.mult), reads=["sg1", "ps3"], writes=["sg1"])
                S.op("vector", lambda e, f=f, mg=mg: e.tensor_tensor(out=mg[:, f, :], in0=SG[0], in1=SG[1], op=ALU.add),
                     reads=["sg0", "sg1"], writes=[mgt])
        XT4s = [XT4, carve(O_SP + 4096, 4096)]
        S.alias("xt40", ["xt4"]); S.alias("xt41", ["xc160"])

        def p4_main(tile):
            tt, t4 = tile // 4, tile % 4
            mg = MG[tt % 2]; mgt = f"mg{tt % 2}"
            XT4 = XT4s[tile % 2]; xt4t = f"xt4{tile % 2}"
            if True:
                r0 = tile * 128
                S.op("sync", lambda e, r0=r0: e.dma_start(out=XT4, in_=xe[(NST - 1) * T + r0:(NST - 1) * T + r0 + 128, :]), writes=[xt4t], dma=xt4t)
                for hf in range(2):
                    for k in range(KC):
                        S.op("tensor", lambda e, hf=hf, k=k, t4=t4, mg=mg: e.matmul(PS[4 + hf][:, :], lhsT=mg[:, k, t4 * 128:(t4 + 1) * 128],
                                                                                rhs=WOUT[:, k, hf * 512:(hf + 1) * 512], start=(k == 0), stop=(k == KC - 1)),
                             reads=[mgt, "wout"], writes=[f"ps{4 + hf}"], sig=(k == KC - 1))
                so, stk = newstat()
                for hf in range(2):
                    S.op("scalar", lambda e, hf=hf, so=so: e.activation(out=JUNK[:, 0:512], in_=PS[4 + hf][:, :], func=AF.Square,
                                                                        accum_out=stat[:, so + hf:so + hf + 1]), reads=[f"ps{4 + hf}"], writes=["junk", stk])
                S.op("vector", lambda e, so=so: e.tensor_tensor(out=stat[:, so + 2:so + 3], in0=stat[:, so:so + 1], in1=stat[:, so + 1:so + 2], op=ALU.add),
                     reads=[stk], writes=[stk])
                S.op("scalar", lambda e, so=so: e.activation(out=stat[:, so + 3:so + 4], in_=stat[:, so + 2:so + 3], func=AF.Sqrt, scale=1.0 / D, bias=EPS),
                     reads=[stk], writes=[stk])
                S.op("vector", lambda e, so=so: e.reciprocal(out=stat[:, so + 3:so + 4], in_=stat[:, so + 3:so + 4]), reads=[stk], writes=[stk])
                for hf in range(2):
                    S.op("vector", lambda e, hf=hf, so=so: e.scalar_tensor_tensor(out=MIXT[:, hf * 512:(hf + 1) * 512], in0=PS[4 + hf][:, :],
                                                                                  scalar=stat[:, so + 3:so + 4], in1=G1b[:, hf * 512:(hf + 1) * 512],
                                                                                  op0=ALU.mult, op1=ALU.mult),
                         reads=[f"ps{4 + hf}", stk, "G1b"], writes=["mixt"])
                S.op("vector", lambda e: e.tensor_tensor(out=MIXT, in0=MIXT, in1=XT4, op=ALU.add), reads=["mixt", xt4t], writes=["mixt"])
                S.op("sync", lambda e, r0=r0: e.dma_start(out=x1s[r0:r0 + 128, :], in_=MIXT), reads=["mixt"], writes=["x1sd"], dma="x1s")
                if debug:
                    S.op("sync", lambda e, r0=r0: e.dma_start(out=dbg["d_x1"][r0:r0 + 128, :], in_=MIXT), reads=["mixt"], dma="dbg")
                S.op("vector", lambda e, so=so: e.scalar_tensor_tensor(out=JUNK, in0=MIXT, scalar=1.0, in1=MIXT, op0=ALU.mult, op1=ALU.mult,
                                                                       accum_out=stat[:, so + 4:so + 5]),
                     reads=["mixt"], writes=["junk", stk])
                S.op("scalar", lambda e, so=so: e.activation(out=stat[:, so + 5:so + 6], in_=stat[:, so + 4:so + 5], func=AF.Sqrt, scale=1.0 / D, bias=EPS),
                     reads=[stk], writes=[stk])
                S.op("vector", lambda e, so=so: e.reciprocal(out=stat[:, so + 5:so + 6], in_=stat[:, so + 5:so + 6]), reads=[stk], writes=[stk])
                S.op("vector", lambda e, so=so: e.tensor_scalar(out=XT4, in0=MIXT, scalar1=stat[:, so + 5:so + 6], scalar2=None, op0=ALU.mult),
                     reads=["mixt", stk, xt4t], writes=[xt4t])
                for hf in range(2):
                    for kk in range(4):
                        k = hf * 4 + kk
                        S.op("tensor", lambda e, k=k, kk=kk, hf=hf: e.transpose(out=PS[6 + hf][:, kk * 128:(kk + 1) * 128], in_=XT4[:, k * 128:(k + 1) * 128],
                                                                                identity=ident[:]), reads=[xt4t, "ident"], writes=[f"ps{6 + hf}"], sig=(kk == 3))
                    for kk in range(4):
                        k = hf * 4 + kk
                        S.op("vector", lambda e, k=k, kk=kk, hf=hf: e.tensor_scalar(out=H2F[:, k, :], in0=PS[6 + hf][:, kk * 128:(kk + 1) * 128],
                                                                                   scalar1=S2[:, k:k + 1], scalar2=ada[:, 16 + k:17 + k],
                                                                                   op0=ALU.mult, op1=ALU.add),
                             reads=[f"ps{6 + hf}", "S2", "ada"], writes=["h2f"])
                h2t = H2TOK[tile % 2]; h2tt = f"h2t{tile % 2}"
                S.op("vector", lambda e: e.tensor_tensor(out=JUNK, in0=XT4, in1=S2B, op=ALU.mult), reads=[xt4t, "s2b", "junk"], writes=["junk"])
                S.op("vector", lambda e, h2t=h2t: e.tensor_tensor(out=h2t, in0=JUNK, in1=SH2B, op=ALU.add), reads=["junk", "s2b"], writes=[h2tt])
                for k in range(KC):
                    S.op("tensor", lambda e, k=k: e.matmul(PS[7][:, 0:NE], lhsT=H2F[:, k, :], rhs=routw[:, k, :], start=(k == 0), stop=(k == KC - 1)),
                         reads=["h2f", "routw"], writes=["ps7"], sig=(k == KC - 1))
                go, gtk = newstat()
                lg = Gt[:, tile, :]
                S.op("vector", lambda e, lg=lg: e.tensor_tensor(out=lg, in0=PS[7][:, 0:NE], in1=rbb[:], op=ALU.add), reads=["ps7", "rbb"], writes=["Gt"])
                S.op("vector", lambda e, lg=lg, go=go: e.max(out=stat[:, go:go + 8], in_=lg), reads=["Gt"], writes=[gtk])
                S.op("vector", lambda e, go=go: e.tensor_scalar(out=stat[:, go + 8:go + 9], in0=stat[:, go:go + 1], scalar1=-1.0, scalar2=None, op0=ALU.mult),
                     reads=[gtk], writes=[gtk])
                S.op("vector", lambda e, lg=lg, go=go: e.tensor_scalar(out=SG[0][:, 0:NE], in0=lg, scalar1=stat[:, go + 3:go + 4], scalar2=None, op0=ALU.is_ge),
                     reads=["Gt", gtk], writes=["sg0"])
                S.op("scalar", lambda e, lg=lg, go=go: e.activation(out=lg, in_=lg, func=AF.Exp, bias=stat[:, go + 8:go + 9], scale=1.0),
                     reads=["Gt", gtk], writes=["Gt"])
                S.op("vector", lambda e, lg=lg: e.tensor_tensor(out=lg, in0=lg, in1=SG[0][:, 0:NE], op=ALU.mult), reads=["Gt", "sg0"], writes=["Gt"])
                S.op("vector", lambda e, lg=lg, go=go: e.tensor_reduce(out=stat[:, go + 9:go + 10], in_=lg, axis=AX.X, op=ALU.add), reads=["Gt"], writes=[gtk])
                S.op("vector", lambda e, go=go: e.reciprocal(out=stat[:, go + 9:go + 10], in_=stat[:, go + 9:go + 10]), reads=[gtk], writes=[gtk])
                S.op("vector", lambda e, lg=lg, go=go: e.tensor_scalar(out=lg, in0=lg, scalar1=stat[:, go + 9:go + 10], scalar2=None, op0=ALU.mult),
                     reads=["Gt", gtk], writes=["Gt"])

        def p4_route(tile):
            lg = Gt[:, tile, :]
            h2t = H2TOK[tile % 2]; h2tt = f"h2t{tile % 2}"
            S.op("vector", lambda e, lg=lg, tile=tile: e.tensor_scalar(out=MB[:, tile, :], in0=lg, scalar1=0.0, scalar2=None, op0=ALU.is_gt),
                 reads=["Gt"], writes=["MB"])
            for ip in range(tile):
                S.op("tensor", lambda e, ip=ip: e.matmul(PS[7][:, 32:64], lhsT=onesb[:], rhs=MB[:, ip, :], start=(ip == 0), stop=False),
                     reads=["MB", "onesb"], writes=["ps7"], sig=False)
            S.op("tensor", lambda e, tile=tile: e.matmul(PS[7][:, 32:64], lhsT=trib[:], rhs=MB[:, tile, :], start=(tile == 0), stop=True),
                 reads=["MB", "trib"], writes=["ps7"])
            ro, rtk = newstat()
            SLF, SEL = SG[1][:, 0:NE], SG[1][:, 64:64 + NE]
            S.op("vector", lambda e: e.tensor_scalar(out=SEL, in0=PS[7][:, 32:64], scalar1=CAP - 0.5, scalar2=1.0e9, op0=ALU.is_ge, op1=ALU.mult),
                 reads=["ps7", "sg1"], writes=["sg1"])
            S.op("vector", lambda e: e.tensor_tensor(out=SLF, in0=PS[7][:, 32:64], in1=rcf[:, 0:NE], op=ALU.add), reads=["ps7", "rcf", "sg1"], writes=["sg1"])
            S.op("vector", lambda e: e.tensor_tensor(out=SLF, in0=SLF, in1=SEL, op=ALU.add), reads=["sg1"], writes=["sg1"])
            S.op("vector", lambda e, lg=lg, ro=ro: e.max(out=stat[:, ro:ro + 8], in_=lg), reads=["Gt"], writes=[rtk])
            for kk in range(4):
                S.op("vector", lambda e, lg=lg, ro=ro, kk=kk: e.tensor_scalar(out=SEL, in0=lg, scalar1=stat[:, ro + kk:ro + kk + 1], scalar2=None, op0=ALU.is_equal),
                     reads=["Gt", rtk, "sg1"], writes=["sg1"])
                S.op("vector", lambda e: e.tensor_tensor(out=SEL, in0=SEL, in1=SLF, op=ALU.mult), reads=["sg1"], writes=["sg1"])
                S.op("vector", lambda e, ro=ro, kk=kk: e.tensor_reduce(out=stat[:, ro + 8 + kk:ro + 9 + kk], in_=SEL, axis=AX.X, op=ALU.add),
                     reads=["sg1"], writes=[rtk])
            S.op("vector", lambda e, ro=ro, tile=tile: e.tensor_copy(out=IDX[:, tile, :], in_=stat[:, ro + 8:ro + 12]), reads=[rtk], writes=["IDX"])
            S.op("vector", lambda e, ro=ro: e.tensor_scalar(out=stat[:, ro + 12:ro + 16], in0=stat[:, ro + 8:ro + 12], scalar1=NE * CAP - 0.5, scalar2=None, op0=ALU.is_lt),
                 reads=[rtk], writes=[rtk])
            S.op("vector", lambda e, ro=ro, tile=tile: e.tensor_tensor(out=GV[:, tile, :], in0=stat[:, ro:ro + 4], in1=stat[:, ro + 12:ro + 16], op=ALU.mult),
                 reads=[rtk], writes=["GV"])
            for kk in range(4):
                S.op("gpsimd", lambda e, tile=tile, kk=kk, h2t=h2t: e.indirect_dma_start(
                    out=xs_d, out_offset=bass.IndirectOffsetOnAxis(ap=IDX[:, tile, kk:kk + 1], axis=0), in_=h2t, in_offset=None,
                    bounds_check=S.regs["bnd"], oob_is_err=False), reads=["IDX", h2tt], writes=[f"xs{tile}_{kk}"], dma=f"scat{kk}")
        p4_merge(0)
        for tile in range(16):
            if tile % 4 == 0 and tile // 4 + 1 < 4:
                p4_merge(tile // 4 + 1)
            p4_main(tile)
            if tile >= 1:
                p4_route(tile - 1)
        p4_route(15)
        for ip in range(16):
            S.op("tensor", lambda e, ip=ip: e.matmul(PS[7][:, 64:96], lhsT=onesb[:], rhs=MB[:, ip, :], start=(ip == 0), stop=(ip == 15)),
                 reads=["MB", "onesb"], writes=["ps7"], sig=(ip == 15))
        FORCE = os.environ.get("KFORCE")
        S.op("vector", lambda e: e.tensor_scalar(out=FLG[:], in0=PS[7][:, 64:96], scalar1=(-1.0 if FORCE else 512.0), scalar2=None, op0=ALU.is_gt),
             reads=["ps7"], writes=["FLG"])
        xs_tokens = [f"xs{tile}_{kk}" for tile in range(16) for kk in range(4)]
        if debug:
            S.op("sync", lambda e: e.dma_start(out=dbg["d_G"], in_=Gt.rearrange("p a b -> p (a b)")), reads=["Gt"], dma="dbg")
            S.op("sync", lambda e: e.dma_start(out=dbg["d_idx"], in_=IDX.rearrange("p a b -> p (a b)")), reads=["IDX"], dma="dbg")
            S.op("sync", lambda e: e.dma_start(out=dbg["d_gv"], in_=GV.rearrange("p a b -> p (a b)")), reads=["GV"], dma="dbg")

        if stop == "p4":
            return finish(["x1s"])

        allold = ["HT", "YR", "QTy", "wout", "mg0", "mg1", "xt4", "mixt", "h2f", "sg0", "sg1", "junk", "wrg", "xc160", "s2b", "h2t0", "h2t1"] \
            + [f"ring{i}" for i in range(8)]
        NBL = CAP // 128
        XE = [carve(16384 * i, 16384, BF16, "p (b d) -> p b d", b=NBL) for i in range(2)]
        XET = [carve(32768 + 16384 * i, 16384, BF16, "p (k t) -> p k t", k=KC) for i in range(2)]
        ACTT = carve(65536, 16384, BF16, "p (k t) -> p k t", k=KC)
        W2B = [carve(81920 + 16384 * i, 16384, BF16, "p (k n) -> p k n", k=KC) for i in range(2)]
        W1R = [carve(114688 + 4096 * i, 4096, BF16, "p (a k m) -> p a k m", a=2, k=KC) for i in range(8)]
        TMP = [[carve(147456 + 8192 * s_ + 2048 * j, 2048) for j in range(4)] for s_ in range(2)]
        YS = [carve(163840 + 4096 * i, 4096) for i in range(2)]
        W1X = [carve(172032 + 4096 * i, 4096, BF16, "p (a k m) -> p a k m", a=2, k=KC) for i in range(2)]
        names5 = ["xe0", "xe1", "xet0", "xet1", "actt", "w2b0", "w2b1", "ys0", "ys1", "w1x0", "w1x1"] + [f"w1r{i}" for i in range(8)] \
            + [f"tmp{s_}{j}" for s_ in range(2) for j in range(4)]
        for nm in names5:
            S.alias(nm, allold)

        def w1_load(ex_, c_):
            S.op("gpsimd", lambda e: e.dma_start(out=W1R[c_], in_=w1_h[ex_, c_]), writes=[f"w1r{c_}"], dma=f"w1r{c_}")

        def xe_load(ex_):
            S.op("sync", lambda e, ex_=ex_: e.dma_start(out=XE[ex_ % 2], in_=xs_d[ex_ * CAP:(ex_ + 1) * CAP, :].rearrange("(b p) d -> p b d", p=128)),
                 reads=xs_tokens, writes=[f"xe{ex_ % 2}"], dma=f"xe{ex_ % 2}")

        def w2_load(ex_):
            for q4 in range(4):
                S.op("gpsimd", lambda e, ex_=ex_, q4=q4: e.dma_start(out=W2B[ex_ % 2][:, 2 * q4:2 * q4 + 2, :], in_=w2_h[ex_, :, 2 * q4:2 * q4 + 2, :]),
                     writes=[f"w2b{ex_ % 2}"], dma=f"w2b{ex_ % 2}")

        cnt5 = {"it": 0, "ys": 0}

        def moe_T(ex, half):
            xeb, xetb = XE[ex % 2], XET[ex % 2]
            xetk, xettk = f"xe{ex % 2}", f"xet{ex % 2}"
            for k in range(KC):
                pbk = PS[6 + (k % 2)]
                pbt = f"ps{6 + (k % 2)}"
                pv = pbk[:, 0:256].bitcast(BF16)
                for b4 in range(4):
                    b_ = half * 4 + b4
                    S.op("tensor", lambda e, k=k, b_=b_, b4=b4, pv=pv: e.transpose(out=pv[:, b4 * 128:(b4 + 1) * 128], in_=xeb[:, b_, k * 128:(k + 1) * 128],
                                                                                 identity=identb[:]), reads=[xetk, "identb"], writes=[pbt], sig=(b4 == 3))
                if k % 2 == 0:
                    S.op("scalar", lambda e, k=k, pv=pv: e.copy(out=xetb[:, k, half * 512:(half + 1) * 512], in_=pv), reads=[pbt], writes=[xettk])
                else:
                    S.op("vector", lambda e, k=k, pv=pv: e.tensor_copy(out=xetb[:, k, half * 512:(half + 1) * 512], in_=pv), reads=[pbt], writes=[xettk])

        def moe_H(ex, tt):
            xetb = XET[ex % 2]
            xettk = f"xet{ex % 2}"
            if tt == 1:
                for c in range(2):
                    S.op("gpsimd", lambda e, c=c: e.dma_start(out=W1X[c], in_=w1_h[ex, c]), writes=[f"w1x{c}"], dma=f"w1x{c}")
            for c in range(8):
                if tt == 0:
                    w1 = W1R[c]; w1t = f"w1r{c}"
                else:
                    w1 = W1X[c % 2]; w1t = f"w1x{c % 2}"
                sset = cnt5["it"] % 2
                cnt5["it"] += 1
                tg, tsg, tu, tgs = TMP[sset]
                for a in range(2):
                    pb = PS[2 * sset + a]
                    for k in range(KC):
                        S.op("tensor", lambda e, a=a, k=k, w1=w1, pb=pb: e.matmul(pb[:, :], lhsT=w1[:, a, k, :], rhs=xetb[:, k, tt * 512:(tt + 1) * 512],
                                                                               start=(k == 0), stop=(k == KC - 1)),
                             reads=[w1t, xettk], writes=[f"ps{2 * sset + a}"], sig=(k == KC - 1))
                pg, pl = PS[2 * sset], PS[2 * sset + 1]
                S.op("vector", lambda e, c=c, tg=tg, pg=pg: e.tensor_scalar(out=tg, in0=pg[:, :], scalar1=b1c[:, ex, c:c + 1], scalar2=7.0,
                                                                           op0=ALU.add, op1=ALU.min), reads=[f"ps{2 * sset}", "b1c"], writes=[f"tmp{sset}0"])
                S.op("scalar", lambda e, tg=tg, tsg=tsg: e.activation(out=tsg, in_=tg, func=AF.Sigmoid, scale=1.702), reads=[f"tmp{sset}0"], writes=[f"tmp{sset}1"])
                S.op("vector", lambda e, c=c, tu=tu, pl=pl: e.tensor_scalar(out=tu, in0=pl[:, :], scalar1=b1c[:, ex, 8 + c:9 + c], scalar2=8.0,
                                                                           op0=ALU.add, op1=ALU.min), reads=[f"ps{2 * sset + 1}", "b1c"], writes=[f"tmp{sset}2"])
                S.op("vector", lambda e, tu=tu, tg=tg, tgs=tgs: e.scalar_tensor_tensor(out=tgs, in0=tu, scalar=-6.0, in1=tg, op0=ALU.max, op1=ALU.mult),
                     reads=[f"tmp{sset}2", f"tmp{sset}0"], writes=[f"tmp{sset}3"])
                S.op("vector", lambda e, c=c, tsg=tsg, tgs=tgs: e.tensor_tensor(out=ACTT[:, c, tt * 512:(tt + 1) * 512], in0=tgs, in1=tsg, op=ALU.mult),
                     reads=[f"tmp{sset}3", f"tmp{sset}1"], writes=[f"actt{tt}"])
                if tt == 1 and c + 2 < 8:
                    S.op("gpsimd", lambda e, c=c: e.dma_start(out=W1X[c % 2], in_=w1_h[ex, c + 2]), writes=[f"w1x{c % 2}"], dma=f"w1x{c % 2}")
                if tt == 0 and ex + 1 < NE:
                    w1_load(ex + 1, c)

        def moe_Y(ex, half):
            w2 = W2B[ex % 2]; w2t = f"w2b{ex % 2}"
            for b4 in range(4):
                b_ = half * 4 + b4
                ys = YS[cnt5["ys"] % 2]; yst = f"ys{cnt5['ys'] % 2}"
                ysk = f"ysst{cnt5['ys'] % 2}"
                cnt5["ys"] += 1
                for hf in range(2):
                    pi = 4 + hf
                    for c in range(8):
                        S.op("tensor", lambda e, c=c, b_=b_, hf=hf, pi=pi: e.matmul(PS[pi][:, :], lhsT=ACTT[:, c, b_ * 128:(b_ + 1) * 128],
                                                                                  rhs=w2[:, c, hf * 512:(hf + 1) * 512], start=(c == 0), stop=(c == 7)),
                             reads=[f"actt{half}", w2t], writes=[f"ps{pi}"], sig=(c == 7))
                    S.op("scalar", lambda e, hf=hf, pi=pi, ys=ys: e.copy(out=ys[:, hf * 512:(hf + 1) * 512], in_=PS[pi][:, :]), reads=[f"ps{pi}"], writes=[yst])
                r0 = ex * CAP + b_ * 128
                S.op("sync", lambda e, r0=r0, ys=ys: e.dma_start(out=ys_d[r0:r0 + 128, :], in_=ys), reads=[yst], writes=[f"ysd{ex}_{b_}"], dma=ysk)

        S.alias("actt0", ["actt"]); S.alias("actt1", ["actt"])
        xe_load(0)
        w2_load(0)
        for c in range(8):
            w1_load(0, c)
        for ex in range(NE):
            if ex + 1 < NE:
                xe_load(ex + 1)
                w2_load(ex + 1)
            if ex == 0:
                moe_T(ex, 0)
            moe_H(ex, 0)
            S.region_begin(FLG[0:1, ex:ex + 1], "FLG")
            moe_T(ex, 1)
            moe_H(ex, 1)
            moe_Y(ex, 1)
            S.region_end()
            if ex + 1 < NE:
                moe_T(ex + 1, 0)
            moe_Y(ex, 0)
        ys_tokens = [f"ysd{ex}_{b_}" for ex in range(NE) for b_ in range(NBL)]
        names5 = names5 + ["actt0", "actt1"]

        S.alias("fin", names5)
        ACC6 = [carve(4096 * i, 4096) for i in range(2)]
        XO = [carve(8192 + 4096 * i, 4096) for i in range(2)]
        JK = carve(16384, 4096)
        B2S = [carve(20480 + 2048 * i, 2048) for i in range(2)]
        GTT = carve(24576, 2048)
        NYG = 4
        YG = [[carve(28672 + 16384 * s_ + 4096 * j, 4096) for j in range(4)] for s_ in range(NYG)]
        n6 = ["acc0", "acc1", "xo0", "xo1", "jk", "b2s0", "b2s1", "gtt"] + [f"yg{s_}{j}" for s_ in range(NYG) for j in range(4)]
        for nm in n6:
            S.alias(nm, ["fin"])
        for s_ in range(NYG):
            for j in range(4):
                S.op("gpsimd", lambda e, s_=s_, j=j: e.memset(YG[s_][j], 0.0), writes=[f"yg{s_}{j}"])
        for hf in range(2):
            S.op("sync", lambda e, hf=hf: e.dma_start(out=B2S[hf][0:NE, :], in_=b2_h[:, hf * 512:(hf + 1) * 512]), writes=[f"b2s{hf}"], dma=f"b2s{hf}")
        for tile in range(16):
            s6 = tile % 2
            sg6 = tile % NYG
            acc = ACC6[s6]; acct = f"acc{s6}"
            xo = XO[s6]; xot = f"xo{s6}"
            r0 = tile * 128
            S.op("sync", lambda e, r0=r0, xo=xo: e.dma_start(out=xo, in_=x1s[r0:r0 + 128, :]), reads=["x1sd"], writes=[xot], dma=xot)
            for kk in range(4):
                S.op("gpsimd", lambda e, tile=tile, kk=kk, sg6=sg6: e.indirect_dma_start(
                    out=YG[sg6][kk], out_offset=None, in_=ys_d, in_offset=bass.IndirectOffsetOnAxis(ap=IDX[:, tile, kk:kk + 1], axis=0),
                    bounds_check=S.regs["bnd"], oob_is_err=False), reads=ys_tokens + ["IDX"], writes=[f"yg{sg6}{kk}"], dma=f"yg{sg6}{kk}")
            S.op("tensor", lambda e, tile=tile: e.transpose(out=PS[7][0:NE, 0:128], in_=Gt[:, tile, :], identity=ident[:]),
                 reads=["Gt", "ident"], writes=["ps7"])
            S.op("vector", lambda e: e.tensor_copy(out=GTT[0:NE, 0:128], in_=PS[7][0:NE, 0:128]), reads=["ps7"], writes=["gtt"])
            for hf in range(2):
                S.op("tensor", lambda e, hf=hf: e.matmul(PS[4 + hf][:, :], lhsT=GTT[0:NE, 0:128], rhs=B2S[hf][0:NE, :], start=True, stop=True),
                     reads=[f"b2s{hf}", "gtt"], writes=[f"ps{4 + hf}"])
                S.op("vector", lambda e, hf=hf, acc=acc: e.tensor_copy(out=acc[:, hf * 512:(hf + 1) * 512], in_=PS[4 + hf][:, :]),
                     reads=[f"ps{4 + hf}"], writes=[acct])
            for kk in range(4):
                S.op("vector", lambda e, tile=tile, kk=kk, sg6=sg6, acc=acc: e.scalar_tensor_tensor(out=acc, in0=YG[sg6][kk], scalar=GV[:, tile, kk:kk + 1], in1=acc,
                                                                                            op0=ALU.mult, op1=ALU.add),
                     reads=[f"yg{sg6}{kk}", "GV", acct], writes=[acct])
            so, stk = newstat()
            S.op("scalar", lambda e, acc=acc, so=so: e.activation(out=JK, in_=acc, func=AF.Square, accum_out=stat[:, so:so + 1]),
                 reads=[acct], writes=["jk", stk])
            S.op("scalar", lambda e, so=so: e.activation(out=stat[:, so + 1:so + 2], in_=stat[:, so:so + 1], func=AF.Sqrt, scale=1.0 / D, bias=EPS),
                 reads=[stk], writes=[stk])
            S.op("vector", lambda e, so=so: e.reciprocal(out=stat[:, so + 1:so + 2], in_=stat[:, so + 1:so + 2]), reads=[stk], writes=[stk])
            S.op("vector", lambda e, acc=acc, so=so: e.scalar_tensor_tensor(out=acc, in0=acc, scalar=stat[:, so + 1:so + 2], in1=G2b[:],
                                                                          op0=ALU.mult, op1=ALU.mult), reads=[acct, stk, "G2b"], writes=[acct])
            S.op("vector", lambda e, acc=acc, xo=xo: e.tensor_tensor(out=xo, in0=xo, in1=acc, op=ALU.add), reads=[acct, xot], writes=[xot])
            S.op("sync", lambda e, r0=r0, xo=xo: e.dma_start(out=out[r0:r0 + 128, :], in_=xo), reads=[xot], dma="outst")
        return finish()


def _alibi_bias():
    slopes = np.array([2.0 ** (-8.0 * (h + 1) / 16) for h in range(16)], dtype=np.float32)
    qi = np.arange(128)[:, None]
    ci = np.arange(256)[None, :]
    dist = qi + 128 - ci
    valid = (dist >= 0) & (dist < 128)
    b = np.where(valid[:, None, :], -slopes[None, :, None] * dist[:, None, :].astype(np.float32), np.float32(NEG))
    return np.ascontiguousarray(b.astype(np.float32))


def _col(v, k=KC):
    return np.ascontiguousarray(np.asarray(v, np.float32).reshape(k, 128).T)


def prepare_inputs(x, c, w_ada, b_ada, norm_pre_mix, norm_post_mix, norm_pre_ffn, norm_post_ffn,
                   w_in, b_in, conv_w, conv_b, rg_w_a, rg_b_a, rg_w_x, rg_b_x, rg_lambda,
                   attn_sinks, w_o_rnn, w_o_attn, w_out, router_w, router_b,
                   moe_w1, moe_b1, moe_w2, moe_b2):
    f = lambda a: np.asarray(a, np.float32)
    x, c = f(x), f(c)
    L = 0
    shared = {}
    shared["wada"] = np.ascontiguousarray(f(w_ada)[L].reshape(KC, 128, 12, 512).transpose(2, 1, 0, 3))
    shared["bada_col"] = _col(f(b_ada)[L], 48)
    shared["bada_row"] = np.ascontiguousarray(f(b_ada)[L].reshape(6, D))
    gam = np.stack([f(norm_pre_mix)[L], f(norm_post_mix)[L], f(norm_pre_ffn)[L], f(norm_post_ffn)[L]])
    shared["gam_col"] = np.ascontiguousarray(gam.reshape(4, KC, 128).transpose(2, 0, 1))
    shared["gam_row"] = np.ascontiguousarray(gam)
    shared["w_in_h"] = np.ascontiguousarray(f(w_in)[L].reshape(KC, 128, 44, 128).transpose(2, 1, 0, 3))
    shared["b_in_col"] = _col(f(b_in)[L], 44)
    wk = f(w_in)[L][:, 3072:3328].reshape(KC, 128, 2, 2, 64)[:, :, :, ::-1, :].reshape(KC, 128, 2, 128)
    shared["w_ks_h"] = np.ascontiguousarray(wk.transpose(2, 1, 0, 3))
    bk = f(b_in)[L][3072:3328].reshape(2, 2, 64)[:, ::-1, :].reshape(2, 128)
    shared["b_ks_col"] = np.ascontiguousarray(bk.T)
    shared["b_v_row"] = np.ascontiguousarray(f(b_in)[L][3328:3584])
    cw = np.concatenate([f(conv_w)[L], f(conv_b)[L][None, :]], axis=0)
    shared["conv_col"] = np.ascontiguousarray(cw.reshape(5, KC, 128).transpose(2, 1, 0))
    rg = np.zeros((128, 2, KC, 128), np.float32)
    for a, wsrc in enumerate((f(rg_w_a)[L], f(rg_w_x)[L])):
        for cc in range(KC):
            rg[0:64, a, cc, 0:64] = wsrc[2 * cc]
            rg[64:128, a, cc, 64:128] = wsrc[2 * cc + 1]
    shared["rgw"] = rg
    shared["rgb_col"] = np.ascontiguousarray(np.stack([_col(f(rg_b_a)[L]), _col(f(rg_b_x)[L])], axis=1))
    shared["lam_col"] = _col(f(rg_lambda)[L])
    shared["sinks_row"] = np.ascontiguousarray(f(attn_sinks)[L].reshape(4, 4)[:, [0, 2, 1, 3]].reshape(16))
    shared["w_or_h"] = np.ascontiguousarray(f(w_o_rnn)[L].reshape(KC, 128, KC, 128).transpose(2, 1, 0, 3))
    shared["w_oa_h"] = np.ascontiguousarray(f(w_o_attn)[L].reshape(KC, 128, KC, 128).transpose(2, 1, 0, 3))
    shared["w_out_h"] = np.ascontiguousarray(f(w_out)[L].reshape(KC, 128, D).transpose(1, 0, 2))
    shared["router_h"] = np.ascontiguousarray(f(router_w)[L].reshape(KC, 128, NE).transpose(1, 0, 2))
    shared["router_b"] = np.ascontiguousarray(f(router_b)[L])
    shared["w1_h"] = np.ascontiguousarray(f(moe_w1)[L].reshape(NE, KC, 128, 8, 128, 2).transpose(0, 3, 2, 5, 1, 4))
    b1 = f(moe_b1)[L].reshape(NE, 8, 128, 2)
    shared["b1_col"] = np.ascontiguousarray(b1.transpose(2, 0, 3, 1).reshape(128, NE, 16))
    shared["w2_h"] = np.ascontiguousarray(f(moe_w2)[L].reshape(NE, KC, 128, D).transpose(0, 2, 1, 3))
    shared["b2_h"] = np.ascontiguousarray(f(moe_b2)[L])
    shared["abias_h"] = _alibi_bias()
    in_maps = []
    for r in range(NCORES):
        b, j = r // 4, r % 4
        m = dict(shared)
        xe = np.zeros((NST * T, D), np.float32)
        n_real = (j + 1) * T
        xe[NST * T - n_real:] = x[b, :n_real]
        m["xe"] = xe
        m["ccol"] = _col(c[b])
        fl = np.zeros((128, 16), np.float32)
        for st in range(NST):
            valid = 1.0 if st >= NST - 1 - j else 0.0
            first = 1.0 if st == NST - 1 - j else 0.0
            fl[:, st] = valid
            fl[:, 4 + st] = first
        fl[:, 8] = 0.0 if j > 0 else NEG
        m["flags_h"] = fl
        rc = np.zeros((128, 160), np.float32)
        rc[:, 0:32] = (np.arange(32, dtype=np.float32) * CAP)[None, :]
        rc[:, 32:160] = (np.arange(128)[:, None] < np.arange(128)[None, :]).astype(np.float32)
        m["rc_h"] = rc
        in_maps.append(m)
    return in_maps


_NC_CACHE = {}


def kernel(**inputs):
    debug = bool(os.environ.get("KDEBUG"))
    stop = os.environ.get("KSTOP") or None
    in_maps = prepare_inputs(**inputs)
    if stop is not None:
        for m in in_maps:
            m["w1_h"] = m["w1_h"][:1]
            m["w2_h"] = m["w2_h"][:1]
    if (debug, stop) not in _NC_CACHE:
        _NC_CACHE[(debug, stop)] = build_nc(debug, stop)
    nc = _NC_CACHE[(debug, stop)]
    res = run_bass_kernel_spmd(nc, in_maps, core_ids=list(range(NCORES)))
    outs = [np.asarray(r["out"], np.float32) for r in res.results]
    full = np.stack([np.concatenate(outs[0:4], axis=0), np.concatenate(outs[4:8], axis=0)], axis=0)
    if debug:
        kernel.last_results = res.results
    return full.astype(np.float32)
```

```python
import os
import numpy as np
from contextlib import ExitStack
import concourse.bass as bass
import concourse.mybir as mybir
from concourse.bass_utils import run_bass_kernel_spmd

F32, BF16 = mybir.dt.float32, mybir.dt.bfloat16
AF = mybir.ActivationFunctionType
ALU = mybir.AluOpType
AX = mybir.AxisListType

NCORES = 8
D = 1024
T = 2048
NST = 4
KC = 8
NE = 32
EPS = 1e-6
NEG = -30000.0
CAP = 1024
U32 = mybir.dt.uint32
ENGS = ("sync", "scalar", "vector", "gpsimd", "tensor")
SEM_ROT = 3000


class Sched:
    def __init__(self, nc, stack):
        self.nc = nc
        self._stack = stack
        self.ops = {e: [] for e in ENGS}
        self.cur_sem = {}
        self.waited = {e: {} for e in ENGS}
        self.last_w = {}
        self.readers = {}
        self.nsem = 0
        self.dma_sems = {}
        self.pending = {e: [] for e in ENGS}
        self.regs = {}
        self.region = None
        self.nregion = 0
        self._saved_waited = None

    def _new_sem(self, name):
        self.nsem += 1
        return self._stack.enter_context(self.nc.semaphore(f"{name}_{self.nsem}"))

    def region_begin(self, flag_ap, flag_tok):
        self.nregion += 1
        for eng in ENGS:
            assert not self.pending[eng] or eng == "tensor" or True
        self.region = {"id": self.nregion, "flag": flag_ap, "tok": flag_tok, "seen": set()}
        self._saved_waited = {e: dict(d) for e, d in self.waited.items()}

    def region_end(self):
        self.region = None
        self.waited = self._saved_waited
        self._saved_waited = None

    def _eng_completion(self, eng):
        cs = self.cur_sem.get(eng)
        if cs is None or (cs[1] >= SEM_ROT and self.region is None):
            cs = [self._new_sem(f"s_{eng}"), 0]
            self.cur_sem[eng] = cs
        cs[1] += 1
        return (cs[0], cs[1], 1)

    def _dma_completion(self, key):
        ds = self.dma_sems.get(key)
        if ds is None or (ds[1] >= SEM_ROT * 8 and self.region is None):
            ds = [self._new_sem("d"), 0]
            self.dma_sems[key] = ds
        ds[1] += 16
        return (ds[0], ds[1], 16)

    def op(self, eng, fn, reads=(), writes=(), dma=None, sig=True):
        rg = self.region
        if rg is not None and eng not in rg["seen"]:
            rg["seen"].add(eng)
            self.region = None
            flag = rg["flag"]
            self.op(eng, lambda e, eng=eng, flag=flag: e.reg_load(self.regs["flag_" + eng], flag), reads=[rg["tok"]], sig=False)
            self.region = rg
        deps = []
        for t in reads:
            deps.extend(self.last_w.get(t, ()))
        for t in writes:
            deps.extend(self.last_w.get(t, ()))
            deps.extend(self.readers.get(t, ()))
        need = {}
        for (s, v, _) in deps:
            k = id(s)
            if k not in need or need[k][1] < v:
                need[k] = (s, v)
        waits = []
        wd = self.waited[eng]
        for k, (s, v) in need.items():
            if wd.get(k, 0) >= v:
                continue
            wd[k] = v
            waits.append((s, v))
        rid = self.region["id"] if self.region is not None else 0
        if not sig:
            self.ops[eng].append((waits, fn, None, rid))
            self.pending[eng].extend(reads)
            return None
        if dma is not None:
            ds0 = self.dma_sems.get(dma)
            before = (ds0[0], ds0[1]) if ds0 is not None and not (ds0[1] >= SEM_ROT * 8 and self.region is None) else None
        else:
            cs0 = self.cur_sem.get(eng)
            before = (cs0[0], cs0[1]) if cs0 is not None and not (cs0[1] >= SEM_ROT and self.region is None) else None
        comp = self._dma_completion(dma) if dma is not None else self._eng_completion(eng)
        self.ops[eng].append((waits, fn, comp, rid, before))
        if dma is None:
            for t in self.pending[eng]:
                self.readers.setdefault(t, []).append(comp)
            self.pending[eng] = []
        for t in reads:
            self.readers.setdefault(t, []).append(comp)
        for t in writes:
            self.last_w[t] = [comp]
            self.readers[t] = []
        return comp

    def alias(self, new, olds):
        acc = list(self.last_w.get(new, ())) + list(self.readers.get(new, ()))
        for t in olds:
            acc.extend(self.last_w.get(t, ()))
            acc.extend(self.readers.get(t, ()))
        self.last_w[new] = acc
        self.readers[new] = []

    def final_wait(self, eng, keys):
        waits = [(self.dma_sems[k][0], self.dma_sems[k][1]) for k in keys]
        self.ops[eng].append((waits, None, None, 0))

    def emit(self, block):
        def emit_op(e, rec):
            waits, fn, comp = rec[0], rec[1], rec[2]
            for (s_, v) in waits:
                e.wait_ge(s_, v)
            if fn is not None:
                ins = fn(e)
                if comp is not None:
                    ins.then_inc(comp[0], comp[2])

        def mk(engname):
            def body(e):
                if engname == "gpsimd":
                    r = e.alloc_register("bnd")
                    e.reg_mov(r, NE * CAP - 1)
                    self.regs["bnd"] = r
                freg = e.alloc_register("rflag")
                self.regs["flag_" + engname] = freg
                ops = self.ops[engname]
                i = 0
                while i < len(ops):
                    rid = ops[i][3]
                    if rid == 0:
                        emit_op(e, ops[i])
                        i += 1
                        continue
                    j = i
                    while j < len(ops) and ops[j][3] == rid:
                        j += 1
                    run = ops[i:j]
                    with e.If_ne(freg, 0):
                        for rec in run:
                            emit_op(e, rec)
                    with e.Else():
                        tot = {}
                        for rec in run:
                            comp = rec[2]
                            if comp is None:
                                continue
                            k = id(comp[0])
                            if k not in tot:
                                before = rec[4]
                                tot[k] = [comp[0], before[1] if before is not None else 0, 0]
                            tot[k][2] += comp[2]
                        for sem_, base, total in tot.values():
                            if base > 0:
                                e.wait_ge(sem_, base)
                            e.sem_inc(sem_, total)
                    i = j
            return body
        block.sync(mk("sync"))
        block.scalar(mk("scalar"))
        block.vector(mk("vector"))
        block.gpsimd(mk("gpsimd"))
        block.tensor(mk("tensor"))


CH_XR, CH_GR, CH_Q, CH_K, CH_V, CH_GATR, CH_GATA = 0, 8, 16, 24, 26, 28, 36


def build_nc(debug=False, stop=None):
    nc = bass.Bass("TRN2", target_bir_lowering=False)

    def din(name, shape, dt=F32):
        return nc.dram_tensor(name, list(shape), dt, kind="ExternalInput").ap()

    xe = din("xe", [NST * T, D])
    ccol = din("ccol", [128, KC])
    wada = din("wada", [12, 128, KC, 512])
    bada_col = din("bada_col", [128, 48])
    bada_row = din("bada_row", [6, D])
    gam_col = din("gam_col", [128, 4, KC])
    gam_row = din("gam_row", [4, D])
    w_in_h = din("w_in_h", [44, 128, KC, 128])
    b_in_col = din("b_in_col", [128, 44])
    w_ks_h = din("w_ks_h", [2, 128, KC, 128])
    b_ks_col = din("b_ks_col", [128, 2])
    b_v_row = din("b_v_row", [256])
    conv_col = din("conv_col", [128, KC, 5])
    rgw = din("rgw", [128, 2, KC, 128])
    rgb_col = din("rgb_col", [128, 2, KC])
    lam_col = din("lam_col", [128, KC])
    sinks_row = din("sinks_row", [16])
    w_or_h = din("w_or_h", [KC, 128, KC, 128])
    w_oa_h = din("w_oa_h", [KC, 128, KC, 128])
    w_out_h = din("w_out_h", [128, KC, D])
    router_h = din("router_h", [128, KC, NE])
    router_b = din("router_b", [NE])
    NEd = NE if stop is None else 1
    w1_h = din("w1_h", [NEd, 8, 128, 2, KC, 128])
    b1_col = din("b1_col", [128, NE, 16])
    w2_h = din("w2_h", [NEd, 128, KC, D])
    b2_h = din("b2_h", [NE, D])
    abias_h = din("abias_h", [128, 16, 256])
    flags_h = din("flags_h", [128, 16])
    rc_h = din("rc_h", [128, 160])
    out = nc.dram_tensor("out", [T, D], F32, kind="ExternalOutput").ap()
    x1s = nc.dram_tensor("x1s", [T, D], F32).ap()
    xs_d = nc.dram_tensor("xs_d", [NE * CAP, D], BF16).ap()
    ys_d = nc.dram_tensor("ys_d", [NE * CAP, D], F32).ap()
    dbg = {}
    if debug:
        for nm, shp, dt in (("d_yr", [128, KC * T], BF16), ("d_ya", [128, KC * T], BF16),
                            ("d_x1", [T, D], F32), ("d_G", [128, 16 * NE], F32),
                            ("d_idx", [128, 64], U32), ("d_gv", [128, 64], F32), ("d_ada", [128, 32], F32),
                            ("d_g1b", [128, 2 * D], F32)):
            dbg[nm] = nc.dram_tensor(nm, shp, dt, kind="ExternalOutput").ap()

    with ExitStack() as es:
        S = Sched(nc, es)

        def finish(extra=()):
            keys = [k for k in (["outst", "dbg"] + list(extra)) if k in S.dma_sems]
            S.final_wait("sync", keys)
            with nc.Block() as block:
                S.emit(block)
            return nc

        def sbt(name, shape, dt=F32):
            return es.enter_context(nc.sbuf_tensor(name, list(shape), dt))

        ARW = 46720
        AR = sbt("arena", [128, ARW], F32)

        def carve(off, nbytes, dt=F32, pat=None, **kw):
            assert off % 4 == 0 and nbytes % 4 == 0 and off + nbytes <= ARW * 4, (off, nbytes)
            v = AR[:, off // 4:(off + nbytes) // 4]
            if dt != F32:
                v = v.bitcast(dt)
            if pat is not None:
                v = v.rearrange(pat, **kw)
            return v

        PS = [es.enter_context(nc.psum_tensor(f"ps{i}", [128, 512], F32)) for i in range(8)]

        ident = sbt("ident", [128, 128])
        identb = sbt("identb", [128, 128], BF16)
        ones_r = sbt("ones_r", [1, 128])
        flags = sbt("flags", [128, 16])
        ccs = sbt("ccs", [128, KC])
        scs = sbt("scs", [128, KC])
        ada = sbt("ada", [128, 32])
        badac = sbt("badac", [128, 48])
        gamc = sbt("gamc", [128, 4, KC])
        S1 = sbt("S1", [128, KC]); S2 = sbt("S2", [128, KC])
        G1b = sbt("G1b", [128, D]); G2b = sbt("G2b", [128, D])
        binc = sbt("binc", [128, 44])
        binq = sbt("binq", [128, 8])
        bflag = sbt("bflag", [128, NST, KC])
        bks = sbt("bks", [128, 2])
        vb = sbt("vb", [128, 256])
        convc = sbt("convc", [128, KC, 5])
        rgbc = sbt("rgbc", [128, 2, KC])
        lamc = sbt("lamc", [128, KC])
        cL = sbt("cL", [128, KC]); cL2 = sbt("cL2", [128, KC]); spt = sbt("spt", [128, KC]); spe = sbt("spe", [128, KC])
        sinkb = sbt("sinkb", [128, 16])
        hstate = sbt("hstate", [128, KC])
        halo = sbt("halo", [128, KC, 4])
        Gt = sbt("Gt", [128, 16, NE])
        rbb = sbt("rbb", [128, NE])
        routw = sbt("routw", [128, KC, NE])
        b1c = sbt("b1c", [128, NE, 16])
        rcf = sbt("rcf", [128, 160])
        trib = sbt("trib", [128, 128], BF16)
        onesb = sbt("onesb", [128, 128], BF16)
        onesf = sbt("onesf", [128, 128])
        MB = sbt("MB", [128, 16, NE], BF16)
        IDX = sbt("IDX", [128, 16, 4], U32)
        GV = sbt("GV", [128, 16, 4])
        FLG = sbt("FLG", [128, NE], mybir.dt.int32)
        HHALO = sbt("HHALO", [128, KC, 128], BF16)
        stat = sbt("stat", [128, 256])
        statn = [0]

        def newstat(n=16):
            i = statn[0] % 16
            statn[0] += 1
            return i * 16, f"st{i}"

        O_HT, O_YR, O_QT = 0, 32768, 65536
        O_KT, O_V, O_RING, O_R = 98304, 115712, 124416, 140800
        O_SP = 174592
        HT = carve(O_HT, 32768, BF16, "p (k t) -> p k t", k=KC)
        YR = carve(O_YR, 32768, BF16, "p (k t) -> p k t", k=KC)
        QT = carve(O_QT, 32768, BF16, "p (k t) -> p k t", k=KC)
        KT = carve(O_KT, 17408, BF16, "p (k t) -> p k t", k=4)
        VV = carve(O_V, 8704, BF16, "p (n c) -> p n c", n=17)
        RING = [carve(O_RING + 2048 * i, 2048, BF16, "p (k m) -> p k m", k=KC) for i in range(8)]
        WRG = carve(O_SP, 4096, BF16, "p (a k m) -> p a k m", a=2, k=KC)
        XC16 = carve(O_SP + 4096, 4096, BF16)
        JUNK = carve(O_SP + 8192, 4096)
        B = [carve(O_QT + 8192 * i, 8192) for i in range(4)]
        XRB = carve(O_R, 8208)
        B.append(carve(O_R + 8208, 8192))
        XST = [carve(O_R + 16400 + 8192 * i, 8192, F32, "p (j d) -> p j d", j=2) for i in range(2)]

        grow = carve(O_QT, 8192)
        browt = carve(O_QT + 8192, 8192)
        gamr = carve(O_QT + 16384, 8192)

        ring_n = [0]

        def ring_load(src):
            i = ring_n[0] % 8
            ring_n[0] += 1
            S.op("gpsimd", lambda e, i=i, src=src: e.dma_start(out=RING[i], in_=src),
                 writes=[f"ring{i}"], dma=f"ring{i}")
            return RING[i], f"ring{i}"

        def small_load(dst, src, tok):
            S.op("sync", lambda e: e.dma_start(out=dst, in_=src), writes=[tok], dma=tok)

        small_load(flags[:], flags_h, "flags")
        small_load(ccs[:], ccol, "ccs")
        small_load(badac[:], bada_col, "badac")
        small_load(gamc[:], gam_col, "gamc")
        small_load(binc[:], b_in_col, "binc")
        small_load(bks[:], b_ks_col, "bks")
        small_load(vb[:], b_v_row.partition_broadcast(128), "vb")
        small_load(convc[:], conv_col, "convc")
        small_load(rgbc[:], rgb_col, "rgbc")
        small_load(lamc[:], lam_col, "lamc")
        small_load(sinkb[:], sinks_row.partition_broadcast(128), "sinkb")
        small_load(rbb[:], router_b.partition_broadcast(128), "rbb")
        small_load(routw[:], router_h, "routw")
        small_load(b1c[:], b1_col, "b1c")
        small_load(rcf[:], rc_h, "rcf")
        small_load(browt[0:1, 0:D], bada_row[2:3, :], "browt0")
        small_load(browt[0:1, D:2 * D], bada_row[5:6, :], "browt1")
        small_load(gamr[0:1, 0:D], gam_row[1:2, :], "gamr0")
        small_load(gamr[0:1, D:2 * D], gam_row[3:4, :], "gamr1")
        S.op("gpsimd", lambda e: e.dma_start(out=WRG, in_=rgw), writes=["wrg"], dma="wrg")

        S.op("gpsimd", lambda e: e.memset(ident[:], 0.0), writes=["ident"])
        S.op("gpsimd", lambda e: e.affine_select(out=ident[:], in_=ident[:], pattern=[[-1, 128]],
                                                  compare_op=ALU.not_equal, fill=1.0, base=0,
                                                  channel_multiplier=1), reads=["ident"], writes=["ident"])
        S.op("vector", lambda e: e.tensor_copy(out=identb[:], in_=ident[:]), reads=["ident"], writes=["identb"])
        S.op("vector", lambda e: e.memset(ones_r[:], 1.0), writes=["ones_r"])
        S.op("vector", lambda e: e.memset(onesb[:], 1.0), writes=["onesb"])
        S.op("vector", lambda e: e.memset(onesf[:], 1.0), writes=["onesf"])
        S.op("vector", lambda e: e.tensor_copy(out=trib[:], in_=rcf[:, 32:160]), reads=["rcf"], writes=["trib"])
        S.op("vector", lambda e: e.memset(hstate[:], 0.0), writes=["hstate"])
        S.op("vector", lambda e: e.memset(halo[:], 0.0), writes=["halo"])
        S.op("vector", lambda e: e.tensor_scalar(out=binq[:], in0=binc[:, CH_Q:CH_Q + 8], scalar1=0.125, scalar2=None,
                                                 op0=ALU.mult), reads=["binc"], writes=["binq"])
        for st_ in range(NST):
            S.op("vector", lambda e, st_=st_: e.tensor_scalar(out=bflag[:, st_, :], in0=binc[:, CH_XR:CH_XR + 8], scalar1=flags[:, st_:st_ + 1],
                                                              scalar2=None, op0=ALU.mult), reads=["binc", "flags"], writes=["bflag"])
        S.op("vector", lambda e: e.tensor_scalar(out=b1c[:, :, 8:16], in0=b1c[:, :, 8:16], scalar1=1.0, scalar2=None,
                                                 op0=ALU.add), reads=["b1c"], writes=["b1c"])
        S.op("scalar", lambda e: e.activation(out=spe[:], in_=lamc[:], func=AF.Exp, scale=-1.0), reads=["lamc"], writes=["spe"])
        S.op("vector", lambda e: e.tensor_scalar(out=spt[:], in0=spe[:], scalar1=-0.2, scalar2=0.25, op0=ALU.mult, op1=ALU.add),
             reads=["spe"], writes=["spt"])
        for cst in (1.0 / 3.0, 0.5, 1.0):
            S.op("vector", lambda e: e.tensor_tensor(out=spt[:], in0=spt[:], in1=spe[:], op=ALU.mult), reads=["spt", "spe"], writes=["spt"])
            S.op("vector", lambda e, cst=cst: e.tensor_scalar(out=spt[:], in0=spt[:], scalar1=-1.0, scalar2=cst, op0=ALU.mult, op1=ALU.add),
                 reads=["spt"], writes=["spt"])
        S.op("vector", lambda e: e.tensor_tensor(out=spt[:], in0=spt[:], in1=spe[:], op=ALU.mult), reads=["spt", "spe"], writes=["spt"])
        S.op("vector", lambda e: e.tensor_scalar(out=cL[:], in0=spt[:], scalar1=-8.0, scalar2=None, op0=ALU.mult), reads=["spt"], writes=["cL"])
        S.op("vector", lambda e: e.tensor_scalar(out=cL2[:], in0=spt[:], scalar1=-16.0, scalar2=None, op0=ALU.mult), reads=["spt"], writes=["cL2"])

        S.op("scalar", lambda e: e.activation(out=scs[:], in_=ccs[:], func=AF.Silu), reads=["ccs"], writes=["scs"])
        WA = [carve(O_YR + 8192 * i, 8192, BF16, "p (k n) -> p k n", k=KC) for i in range(3)]
        scs16 = sbt("scs16", [128, KC], BF16)
        S.op("vector", lambda e: e.tensor_copy(out=scs16[:], in_=scs[:]), reads=["scs"], writes=["scs16"])
        col_pieces = {0: 0, 1: 4, 2: 8, 3: 12, 6: 16, 7: 20, 8: 24, 9: 28}
        row_pieces = {4: 0, 5: 512, 10: 1024, 11: 1536}
        for pc in range(12):
            wa = WA[pc % 3]
            tk = f"wa{pc % 3}"
            for hk in range(2):
                S.op("gpsimd", lambda e, wa=wa, pc=pc, hk=hk: e.dma_start(out=wa[:, 4 * hk:4 * hk + 4, :], in_=wada[pc][:, 4 * hk:4 * hk + 4, :]),
                     writes=[tk], dma=tk)
            if pc in col_pieces:
                base = col_pieces[pc]
                for sub in range(4):
                    for k in range(KC):
                        S.op("tensor", lambda e, wa=wa, sub=sub, k=k: e.matmul(
                            PS[0][:, sub:sub + 1], lhsT=wa[:, k, sub * 128:(sub + 1) * 128], rhs=scs16[:, k:k + 1],
                            start=(k == 0), stop=(k == KC - 1)), reads=[tk, "scs16"], writes=["ps0"], sig=(k == KC - 1))
                S.op("vector", lambda e, base=base, pc=pc: e.tensor_tensor(
                    out=ada[:, base:base + 4], in0=PS[0][:, 0:4], in1=badac[:, pc * 4:pc * 4 + 4], op=ALU.add),
                    reads=["ps0", "badac"], writes=["ada"])
            else:
                ro = row_pieces[pc]
                for k in range(KC):
                    S.op("tensor", lambda e, wa=wa, k=k: e.matmul(
                        PS[1][0:1, :], lhsT=scs16[:, k:k + 1], rhs=wa[:, k, :], start=(k == 0), stop=(k == KC - 1)),
                        reads=[tk, "scs16"], writes=["ps1"], sig=(k == KC - 1))
                S.op("vector", lambda e, ro=ro: e.tensor_tensor(out=grow[0:1, ro:ro + 512], in0=PS[1][0:1, :],
                                                                in1=browt[0:1, ro:ro + 512], op=ALU.add),
                     reads=["ps1", "browt0", "browt1"], writes=["grow"])
                S.op("vector", lambda e, ro=ro: e.tensor_tensor(out=grow[0:1, ro:ro + 512], in0=grow[0:1, ro:ro + 512],
                                                                in1=gamr[0:1, ro:ro + 512], op=ALU.mult),
                     reads=["grow", "gamr0", "gamr1"], writes=["grow"])
                S.op("tensor", lambda e, ro=ro: e.matmul(PS[2][:, :], lhsT=ones_r[0:1, :], rhs=grow[0:1, ro:ro + 512],
                                                         start=True, stop=True), reads=["grow", "ones_r"], writes=["ps2"])
                dstb = G1b if ro < 1024 else G2b
                S.op("vector", lambda e, ro=ro, dstb=dstb: e.tensor_copy(out=dstb[:, (ro % 1024):(ro % 1024) + 512], in_=PS[2][:, :]),
                     reads=["ps2"], writes=["G1b" if ro < 1024 else "G2b"])
        S.op("vector", lambda e: e.scalar_tensor_tensor(out=S1[:], in0=ada[:, 8:16], scalar=1.0, in1=gamc[:, 0, :], op0=ALU.add, op1=ALU.mult),
             reads=["ada", "gamc"], writes=["S1"])
        S.op("vector", lambda e: e.scalar_tensor_tensor(out=S2[:], in0=ada[:, 24:32], scalar=1.0, in1=gamc[:, 2, :], op0=ALU.add, op1=ALU.mult),
             reads=["ada", "gamc"], writes=["S2"])
        S.alias("YR", ["wa0", "wa1", "wa2"])
        S.alias("ga0", ["grow"]); S.alias("ga1", ["grow"]); S.alias("gi0", ["browt0", "browt1"]); S.alias("gi1", ["browt0", "browt1"]); S.alias("ta0", ["gamr0", "gamr1"]); S.alias("ta1", ["gamr0", "gamr1"])
        if debug:
            S.op("sync", lambda e: e.dma_start(out=dbg["d_ada"], in_=ada[:]), reads=["ada"], dma="dbg")
            S.op("sync", lambda e: e.dma_start(out=dbg["d_g1b"][:, 0:D], in_=G1b[:]), reads=["G1b"], dma="dbg")
            S.op("sync", lambda e: e.dma_start(out=dbg["d_g1b"][:, D:2 * D], in_=G2b[:]), reads=["G2b"], dma="dbg")

        if stop == "p0":
            return finish()

        def norm_to_T(src_rows, row0, ntile2, scale_col, shift_col, dstT, dst_tok, col0, dst_f32=None):
            xs = XST[ntile2 % 2]
            tk = f"xst{ntile2 % 2}"
            S.op("sync", lambda e: e.dma_start(out=xs, in_=src_rows[row0:row0 + 256, :].rearrange("(j p) d -> p j d", p=128)),
                 writes=[tk], dma=tk)
            so, stk = newstat()
            for j in range(2):
                S.op("vector", lambda e, j=j: e.scalar_tensor_tensor(out=JUNK, in0=xs[:, j, :], scalar=1.0, in1=xs[:, j, :], op0=ALU.mult, op1=ALU.mult,
                                                                     accum_out=stat[:, so + j:so + j + 1]),
                     reads=[tk], writes=["junk", stk])
            S.op("scalar", lambda e: e.activation(out=stat[:, so + 2:so + 4], in_=stat[:, so:so + 2], func=AF.Sqrt, scale=1.0 / D, bias=EPS),
                 reads=[stk], writes=[stk])
            S.op("vector", lambda e: e.reciprocal(out=stat[:, so + 2:so + 4], in_=stat[:, so + 2:so + 4]), reads=[stk], writes=[stk])
            for j in range(2):
                S.op("vector", lambda e, j=j: e.tensor_scalar(out=xs[:, j, :], in0=xs[:, j, :], scalar1=stat[:, so + 2 + j:so + 3 + j],
                                                               scalar2=None, op0=ALU.mult),
                     reads=[tk, stk], writes=[tk])
            for half in range(4):
                pb = PS[4 + half]
                pt = f"ps{4 + half}"
                for kk in range(2):
                    k = half * 2 + kk
                    for j in range(2):
                        S.op("tensor", lambda e, k=k, j=j, kk=kk, pb=pb: e.transpose(
                            out=pb[:, (kk * 2 + j) * 128:(kk * 2 + j + 1) * 128], in_=xs[:, j, k * 128:(k + 1) * 128], identity=ident[:]),
                            reads=[tk, "ident"], writes=[pt], sig=(kk == 1 and j == 1))
                for kk in range(2):
                    k = half * 2 + kk
                    S.op("scalar", lambda e, k=k, kk=kk, pb=pb: e.activation(
                        out=dstT[:, k, col0:col0 + 256], in_=pb[:, kk * 256:(kk + 1) * 256], func=AF.Identity,
                        bias=shift_col[:, k:k + 1], scale=scale_col[:, k:k + 1]),
                        reads=[pt, "ada", "S1", "S2"], writes=[dst_tok])

        def proj_T(chunk_src, rhsT, rhs_tok, ncols, col0, evac):
            w, wt = ring_load(chunk_src)
            for i in range((ncols + 511) // 512):
                n = min(512, ncols - i * 512)
                pb = PS[i % 4]
                pt = f"ps{i % 4}"
                for k in range(KC):
                    S.op("tensor", lambda e, k=k, pb=pb, n=n, i=i: e.matmul(
                        pb[:, 0:n], lhsT=w[:, k, :], rhs=rhsT[:, k, col0 + i * 512:col0 + i * 512 + n],
                        start=(k == 0), stop=(k == KC - 1)), reads=[wt, rhs_tok], writes=[pt], sig=(k == KC - 1))
                evac(i, pb, pt, i * 512, n)

        XRBs = [XRB, carve(O_KT, 8208)]
        XCs = [B[4], carve(O_KT + 8208, 8192)]
        XC16s = [XC16, carve(O_KT + 16400, 4096, BF16)]
        GA, GI, TA_, TM = B[0], B[1], B[2], B[3]
        ntile2 = [0]

        def rnn_norm(st):
            for g2 in range(T // 256):
                norm_to_T(xe, st * T + g2 * 256, ntile2[0], S1, ada[:, 0:8], HT, "HT", g2 * 256)
                ntile2[0] += 1
            if st == NST - 2:
                S.op("gpsimd", lambda e: e.tensor_copy(out=HHALO[:], in_=HT[:, :, T - 128:T]), reads=["HT"], writes=["hhalo"])

        def rnn_A(st, c):
            sx = (st * KC + c) % 2
            xrb, xc, xc16 = XRBs[sx], XCs[sx], XC16s[sx]
            xrbt, xct, xc16t = f"xrb{sx}", f"xc{sx}", f"xc16{sx}"
            S.op("vector", lambda e: e.tensor_copy(out=xrb[:, 0:3], in_=halo[:, c, 0:3]), reads=["halo"], writes=[xrbt])

            def ev_xr(i, pb, pt, c0, n):
                if i % 2 == 0:
                    S.op("scalar", lambda e: e.activation(out=xrb[:, 3 + c0:3 + c0 + n], in_=pb[:, 0:n], func=AF.Identity,
                                                          bias=bflag[:, st, c:c + 1], scale=flags[:, st:st + 1]),
                         reads=[pt, "bflag", "flags"], writes=[xrbt])
                else:
                    S.op("vector", lambda e: e.tensor_scalar(out=xrb[:, 3 + c0:3 + c0 + n], in0=pb[:, 0:n], scalar1=binc[:, c:c + 1],
                                                             scalar2=flags[:, st:st + 1], op0=ALU.add, op1=ALU.mult),
                         reads=[pt, "binc", "flags"], writes=[xrbt])
            proj_T(w_in_h[CH_XR + c], HT, "HT", T, 0, ev_xr)
            S.op("vector", lambda e: e.tensor_copy(out=halo[:, c, 0:3], in_=xrb[:, T:T + 3]), reads=[xrbt], writes=["halo"])
            S.op("vector", lambda e: e.tensor_scalar(out=xc, in0=xrb[:, 0:T], scalar1=convc[:, c, 0:1], scalar2=convc[:, c, 4:5],
                                                     op0=ALU.mult, op1=ALU.add), reads=[xrbt, "convc"], writes=[xct])
            for kk in range(1, 4):
                S.op("vector", lambda e, kk=kk: e.scalar_tensor_tensor(out=xc, in0=xrb[:, kk:kk + T], scalar=convc[:, c, kk:kk + 1],
                                                                       in1=xc, op0=ALU.mult, op1=ALU.add),
                     reads=[xrbt, "convc", xct], writes=[xct])
            S.op("vector", lambda e: e.tensor_copy(out=xc16, in_=xc), reads=[xct], writes=[xc16t])

        def rnn_B(st, c):
            own = (st == NST - 1)
            sx = (st * KC + c) % 2
            xc, xc16 = XCs[sx], XC16s[sx]
            xct, xc16t = f"xc{sx}", f"xc16{sx}"
            HH = xc
            TH = T // 2
            for h in range(2):
                cs = slice(h * TH, (h + 1) * TH)
                gat, git, tat, tmt = f"ga{h}", f"gi{h}", f"ta{h}", f"tm{h}"
                for i2 in range(2):
                    i = 2 * h + i2
                    for a in range(2):
                        pb = PS[4 + 2 * i2 + a]
                        pt = f"ps{4 + 2 * i2 + a}"
                        S.op("tensor", lambda e, a=a, i=i, pb=pb: e.matmul(pb[:, :], lhsT=WRG[:, a, c, :], rhs=xc16[:, i * 512:(i + 1) * 512],
                                                                           start=True, stop=True), reads=["wrg", xc16t], writes=[pt])
                        dst = GA if a == 0 else GI
                        S.op("scalar", lambda e, a=a, i=i, pb=pb, dst=dst: e.activation(
                            out=dst[:, i * 512:(i + 1) * 512], in_=pb[:, :], func=AF.Sigmoid, bias=rgbc[:, a, c:c + 1], scale=1.0),
                            reads=[pt, "rgbc"], writes=[gat if a == 0 else git])
                S.op("scalar", lambda e, cs=cs: e.activation(out=TA_[:, cs], in_=GA[:, cs], func=AF.Exp, scale=cL[:, c:c + 1]), reads=[gat, "cL"], writes=[tat])
                S.op("scalar", lambda e, cs=cs: e.activation(out=TM[:, cs], in_=GA[:, cs], func=AF.Exp, scale=cL2[:, c:c + 1]), reads=[gat, "cL2"], writes=[tmt])
                S.op("vector", lambda e, cs=cs: e.tensor_scalar(out=TM[:, cs], in0=TM[:, cs], scalar1=1.0, scalar2=-1.0, op0=ALU.min, op1=ALU.mult),
                     reads=[tmt], writes=[tmt])
                S.op("scalar", lambda e, cs=cs: e.activation(out=TM[:, cs], in_=TM[:, cs], func=AF.Sqrt, scale=1.0, bias=1.0), reads=[tmt], writes=[tmt])
                if h == 0:
                    S.op("vector", lambda e: e.tensor_tensor(out=TM[:, 0:1], in0=TM[:, 0:1], in1=flags[:, 4 + st:5 + st], op=ALU.max),
                         reads=[tmt, "flags"], writes=[tmt])
                S.op("vector", lambda e, cs=cs: e.scalar_tensor_tensor(out=GI[:, cs], in0=xc[:, cs], scalar=flags[:, st:st + 1], in1=GI[:, cs],
                                                                      op0=ALU.mult, op1=ALU.mult), reads=[xct, git, "flags"], writes=[git])
                S.op("vector", lambda e, cs=cs: e.tensor_tensor(out=GI[:, cs], in0=GI[:, cs], in1=TM[:, cs], op=ALU.mult), reads=[git, tmt], writes=[git])
                init = hstate[:, c:c + 1] if h == 0 else HH[:, TH - 1:TH]
                S.op("vector", lambda e, cs=cs, init=init: e.tensor_tensor_scan(out=HH[:, cs], data0=TA_[:, cs], data1=GI[:, cs], initial=init,
                                                                                op0=ALU.mult, op1=ALU.add),
                     reads=[tat, git, "hstate", xct], writes=[xct])
            S.op("vector", lambda e: e.tensor_copy(out=hstate[:, c:c + 1], in_=HH[:, T - 1:T]), reads=[xct], writes=["hstate"])
            if own:
                XG, X2 = B[0], B[1]
                gaA, giA = ["ga0", "ga1"], ["gi0", "gi1"]

                def ev_gr(i, pb, pt, c0, n):
                    S.op("scalar", lambda e: e.activation(out=XG[:, c0:c0 + n], in_=pb[:, 0:n], func=AF.Identity,
                                                          bias=binc[:, CH_GR + c:CH_GR + c + 1], scale=1.0),
                         reads=[pt, "binc"], writes=gaA)
                proj_T(w_in_h[CH_GR + c], HT, "HT", T, 0, ev_gr)
                S.op("vector", lambda e: e.tensor_tensor(out=X2, in0=XG, in1=XG, op=ALU.mult), reads=gaA, writes=giA)
                S.op("vector", lambda e: e.tensor_scalar(out=X2, in0=X2, scalar1=0.044715, scalar2=1.0, op0=ALU.mult, op1=ALU.add),
                     reads=giA, writes=giA)
                S.op("vector", lambda e: e.tensor_tensor(out=X2, in0=X2, in1=XG, op=ALU.mult), reads=giA + gaA, writes=giA)
                S.op("scalar", lambda e: e.activation(out=X2, in_=X2, func=AF.Sigmoid, scale=1.5957691216057308), reads=giA, writes=giA)
                S.op("vector", lambda e: e.tensor_tensor(out=X2, in0=X2, in1=XG, op=ALU.mult), reads=giA + gaA, writes=giA)
                S.op("vector", lambda e: e.tensor_tensor(out=YR[:, c, :], in0=HH, in1=X2, op=ALU.mult), reads=[xct] + giA, writes=["YR"])

        seq = [(st, c) for st in range(NST) for c in range(KC)]
        rnn_norm(0)
        rnn_A(*seq[0])
        for n in range(len(seq)):
            if n + 1 < len(seq):
                st1, c1 = seq[n + 1]
                if c1 == 0:
                    rnn_norm(st1)
                rnn_A(st1, c1)
            rnn_B(*seq[n])
        if debug:
            S.op("sync", lambda e: e.dma_start(out=dbg["d_yr"], in_=YR.rearrange("p k t -> p (k t)")), reads=["YR"], dma="dbg")

        if stop == "p1":
            return finish()

        S.alias("QT", ["ga0", "ga1", "gi0", "gi1", "ta0", "ta1", "tm0", "tm1", "xc0"])
        S.alias("KT", ["xrb1", "xc1", "xc161"]); S.alias("VV", ["xrb1", "xc1", "xc161"])
        for kc in range(4):
            def ev_kh(i, pb, pt, c0, n, kc=kc):
                bcol = binc[:, CH_K + kc:CH_K + kc + 1] if kc < 2 else bks[:, kc - 2:kc - 1]
                S.op("scalar", lambda e: e.activation(out=KT[:, kc, 0:128], in_=pb[:, 0:128], func=AF.Identity, bias=bcol, scale=1.0),
                     reads=[pt, "binc", "bks"], writes=["KT"])
            proj_T(w_in_h[CH_K + kc] if kc < 2 else w_ks_h[kc - 2], HHALO, "hhalo", 128, 0, ev_kh)
        for vc in range(2):
            w_, wt_ = ring_load(w_in_h[CH_V + vc])
            for k in range(KC):
                S.op("tensor", lambda e, k=k, w_=w_, vc=vc: e.matmul(PS[0][:, vc * 128:(vc + 1) * 128], lhsT=HHALO[:, k, :],
                                                                   rhs=w_[:, k, :], start=(k == 0), stop=(k == KC - 1)),
                     reads=[wt_, "hhalo"], writes=["ps0"], sig=(k == KC - 1))
        S.op("vector", lambda e: e.tensor_tensor(out=VV[:, 0, :], in0=PS[0][:, 0:256], in1=vb[:], op=ALU.add),
             reads=["ps0", "vb"], writes=["VV"])
        for qc in range(8):
            def ev_q(i, pb, pt, c0, n, qc=qc):
                S.op("scalar", lambda e: e.activation(out=QT[:, qc, c0:c0 + n], in_=pb[:, 0:n], func=AF.Identity,
                                                      bias=binq[:, qc:qc + 1], scale=0.125), reads=[pt, "binq"], writes=["QT"])
            proj_T(w_in_h[CH_Q + qc], HT, "HT", T, 0, ev_q)
        for kc in range(4):
            def ev_k(i, pb, pt, c0, n, kc=kc):
                bcol = binc[:, CH_K + kc:CH_K + kc + 1] if kc < 2 else bks[:, kc - 2:kc - 1]
                S.op("scalar", lambda e: e.activation(out=KT[:, kc, 128 + c0:128 + c0 + n], in_=pb[:, 0:n], func=AF.Identity, bias=bcol, scale=1.0),
                     reads=[pt, "binc", "bks"], writes=["KT"])
            proj_T(w_in_h[CH_K + kc] if kc < 2 else w_ks_h[kc - 2], HT, "HT", T, 0, ev_k)
        wv = [ring_load(w_in_h[CH_V + vc]) for vc in range(2)]
        for tt in range(16):
            pb = PS[tt % 4]; pt = f"ps{tt % 4}"
            for vc in range(2):
                for k in range(KC):
                    S.op("tensor", lambda e, k=k, vc=vc, tt=tt, pb=pb: e.matmul(pb[:, vc * 128:(vc + 1) * 128], lhsT=HT[:, k, tt * 128:(tt + 1) * 128],
                                                                             rhs=wv[vc][0][:, k, :], start=(k == 0), stop=(k == KC - 1)),
                         reads=[wv[vc][1], "HT"], writes=[pt], sig=(k == KC - 1))
            S.op("vector", lambda e, tt=tt, pb=pb: e.tensor_tensor(out=VV[:, 1 + tt, :], in0=pb[:, 0:256], in1=vb[:], op=ALU.add),
                 reads=[pt, "vb"], writes=["VV"])

        if stop == "p2":
            return finish()

        S.alias("attR", ["xrb0", "xc0", "xst0", "xst1"])
        SS = [carve(O_R + 4096 * i, 4096, F32, "p (h c) -> p h c", h=4) for i in range(2)]
        PN = [carve(O_R + 8192 + 2048 * i, 2048, BF16, "p (h c) -> p h c", h=4) for i in range(2)]
        PTB = [carve(O_R + 12288 + 2048 * i, 2048, BF16, "p (b c) -> p b c", b=2) for i in range(2)]
        ABI = carve(O_R + 16384, 16384, F32, "p (h c) -> p h c", h=16)
        S.op("sync", lambda e: e.dma_start(out=ABI, in_=abias_h), writes=["attR"], dma="abi")
        S.alias("abi", ["attR"]); S.alias("ss0", ["attR"]); S.alias("ss1", ["attR"]); S.alias("pn0", ["attR"]); S.alias("pn1", ["attR"])
        S.alias("ptb0", ["attR"]); S.alias("ptb1", ["attR"])
        def att_stage1(it, qb, g):
            par = it % 2
            ss, pn = SS[par], PN[par]
            sst, pnt = f"ss{par}", f"pn{par}"
            pS = (PS[0], PS[1]) if par == 0 else (PS[2], PS[3])
            pSt = ("ps0", "ps1") if par == 0 else ("ps2", "ps3")
            for hh in (0, 2, 1, 3):
                h = 4 * g + hh
                ch, hp = h // 2, h % 2
                po = 64 * hp
                kch = (g // 2) if (g % 2) == hp else 2 + (g // 2)
                pb = pS[hp]
                S.op("tensor", lambda e, ch=ch, po=po, qb=qb, kch=kch, hh=hh, pb=pb: e.matmul(
                    pb[:, (hh // 2) * 256:(hh // 2) * 256 + 256], lhsT=QT[po:po + 64, ch, qb * 128:(qb + 1) * 128],
                    rhs=KT[po:po + 64, kch, qb * 128:qb * 128 + 256], start=True, stop=True),
                    reads=["QT", "KT"], writes=[pSt[hp]], sig=(hh // 2 == 1))
            for half in range(2):
                S.op("vector", lambda e, half=half, g=g, ss=ss, pS=pS: e.tensor_tensor(
                    out=ss[:, 2 * half:2 * half + 2, :], in0=pS[half][:, :].rearrange("p (h c) -> p h c", h=2),
                    in1=ABI[:, 4 * g + half:4 * g + half + 3:2, :], op=ALU.add),
                    reads=[pSt[half], "abi"], writes=[sst])
            if qb == 0:
                S.op("vector", lambda e, ss=ss: e.tensor_scalar(out=ss[:, :, 0:128], in0=ss[:, :, 0:128], scalar1=flags[:, 8:9], scalar2=None,
                                                                op0=ALU.add), reads=[sst, "flags"], writes=[sst])
            so, stk = newstat()
            mx, nmx, rs, es_ = stat[:, so:so + 4], stat[:, so + 4:so + 8], stat[:, so + 8:so + 12], stat[:, so + 12:so + 16]
            S.op("vector", lambda e, ss=ss, mx=mx: e.tensor_reduce(out=mx, in_=ss, axis=AX.X, op=ALU.max), reads=[sst], writes=[stk])
            S.op("vector", lambda e, mx=mx, g=g: e.tensor_tensor(out=mx, in0=mx, in1=sinkb[:, 4 * g:4 * g + 4], op=ALU.max),
                 reads=[stk, "sinkb"], writes=[stk])
            S.op("vector", lambda e, mx=mx, nmx=nmx: e.tensor_scalar(out=nmx, in0=mx, scalar1=-1.0, scalar2=None, op0=ALU.mult),
                 reads=[stk], writes=[stk])
            att_ctx[it] = (so, stk)

        def att_stage1b(it, qb, g):
            par = it % 2
            ss, pn = SS[par], PN[par]
            sst, pnt = f"ss{par}", f"pn{par}"
            so, stk = att_ctx.pop(it)
            mx, nmx, rs, es_ = stat[:, so:so + 4], stat[:, so + 4:so + 8], stat[:, so + 8:so + 12], stat[:, so + 12:so + 16]
            for hh in range(4):
                S.op("scalar", lambda e, hh=hh, ss=ss, nmx=nmx, rs=rs: e.activation(
                    out=ss[:, hh, :], in_=ss[:, hh, :], func=AF.Exp, bias=nmx[:, hh:hh + 1], scale=1.0, accum_out=rs[:, hh:hh + 1]),
                    reads=[sst, stk], writes=[sst, stk])
            S.op("vector", lambda e, mx=mx, g=g, es_=es_: e.tensor_tensor(out=es_, in0=sinkb[:, 4 * g:4 * g + 4], in1=mx, op=ALU.subtract),
                 reads=[stk, "sinkb"], writes=[stk])
            S.op("scalar", lambda e, es_=es_: e.activation(out=es_, in_=es_, func=AF.Exp), reads=[stk], writes=[stk])
            S.op("vector", lambda e, rs=rs, es_=es_: e.tensor_tensor(out=rs, in0=rs, in1=es_, op=ALU.add), reads=[stk], writes=[stk])
            S.op("vector", lambda e, rs=rs: e.reciprocal(out=rs, in_=rs), reads=[stk], writes=[stk])
            for pos in range(4):
                S.op("vector", lambda e, pos=pos, ss=ss, pn=pn, rs=rs: e.tensor_scalar(
                    out=pn[:, pos, :], in0=ss[:, pos, :], scalar1=rs[:, pos:pos + 1], scalar2=None, op0=ALU.mult),
                    reads=[sst, stk], writes=[pnt])

        def att_stage2(it, qb, g):
            par = it % 2
            pn, ptb = PN[par], PTB[par]
            pnt, ptbt = f"pn{par}", f"ptb{par}"
            pT = PS[4 + par]
            pTt = f"ps{4 + par}"
            pTb = pT[:, 0:512].bitcast(BF16).rearrange("p (b c) -> p b c", b=2)
            for pos in range(4):
                for kb in range(2):
                    S.op("tensor", lambda e, pos=pos, kb=kb, pn=pn, pTb=pTb: e.transpose(
                        out=pTb[:, kb, pos * 128:(pos + 1) * 128], in_=pn[:, pos, kb * 128:(kb + 1) * 128], identity=identb[:]),
                        reads=[pnt, "identb"], writes=[pTt], sig=(pos == 3 and kb == 1))
            S.op("scalar", lambda e, ptb=ptb, pTb=pTb: e.copy(out=ptb, in_=pTb), reads=[pTt], writes=[ptbt])
            pO = PS[6 + par]
            pOt = f"ps{6 + par}"
            for eo in range(2):
                for kb in range(2):
                    S.op("tensor", lambda e, eo=eo, kb=kb, qb=qb, g=g, ptb=ptb, pO=pO: e.matmul(
                        pO[64 * eo:64 * eo + 64, 0:256], lhsT=VV[:, qb + kb, 64 * g:64 * g + 64], rhs=ptb[:, kb, 256 * eo:256 * eo + 256],
                        start=(kb == 0), stop=(kb == 1)), reads=["VV", ptbt], writes=[pOt], sig=(eo == 1 and kb == 1))
            S.op("vector", lambda e, qb=qb, g=g, pO=pO: e.tensor_copy(
                out=QT[:, 2 * g:2 * g + 2, qb * 128:(qb + 1) * 128], in_=pO[:, 0:256].rearrange("p (c t) -> p c t", c=2)),
                reads=[pOt], writes=["yaW"])

        att_ctx = {}
        iters = [(qb, g) for qb in range(16) for g in range(4)]
        for it in range(len(iters) + 2):
            if it < len(iters):
                att_stage1(it, *iters[it])
            if 1 <= it <= len(iters):
                att_stage1b(it - 1, *iters[it - 1])
            if it >= 2:
                att_stage2(it - 2, *iters[it - 2])
        S.alias("QTy", ["QT", "yaW"])
        if debug:
            S.op("sync", lambda e: e.dma_start(out=dbg["d_ya"], in_=QT.rearrange("p k t -> p (k t)")), reads=["QTy"], dma="dbg")

        if stop == "p3":
            return finish()

        S.alias("p4R", ["abi", "ss0", "ss1", "pn0", "pn1", "ptb0", "ptb1"])
        S.alias("p4K", ["KT", "VV"])
        MG = [carve(O_R + 8192 * i, 8192, BF16, "p (k t) -> p k t", k=KC) for i in range(2)]
        XT4 = carve(O_R + 16384, 4096)
        MIXT = carve(O_R + 20480, 4096)
        H2F = carve(O_R + 24576, 4096, F32, "p (k t) -> p k t", k=KC)
        SG = [carve(O_R + 28672 + 2048 * i, 2048) for i in range(2)]
        WOUT = carve(O_KT, 16384, BF16, "p (k n) -> p k n", k=KC)
        for q4 in range(4):
            S.op("gpsimd", lambda e, q4=q4: e.dma_start(out=WOUT[:, 2 * q4:2 * q4 + 2, :], in_=w_out_h[:, 2 * q4:2 * q4 + 2, :]),
                 writes=["p4K"], dma="wout")
        S.alias("wout", ["p4K"])
        for nm in ("mg0", "mg1", "xt4", "mixt", "h2f", "sg0", "sg1"):
            S.alias(nm, ["p4R"])
        S2B = carve(O_V, 4096)
        SH2B = carve(O_V + 4096, 4096)
        H2TOK = [carve(O_SP + 2048 * i, 2048, BF16) for i in range(2)]
        S.alias("h2t0", ["wrg"]); S.alias("h2t1", ["wrg"])
        S.alias("s2b", ["KT", "VV"])
        for which, dstb in ((0, S2B), (1, SH2B)):
            for k in range(KC):
                src = S2[:, k:k + 1] if which == 0 else ada[:, 16 + k:17 + k]
                S.op("vector", lambda e, src=src: e.tensor_scalar(out=SG[0][:, 0:128], in0=ident[:], scalar1=src, scalar2=None, op0=ALU.mult),
                     reads=["ident", "S2", "ada", "sg0"], writes=["sg0"])
                S.op("tensor", lambda e: e.matmul(PS[5][:, 0:128], lhsT=onesf[:], rhs=SG[0][:, 0:128], start=True, stop=True),
                     reads=["sg0", "onesf"], writes=["ps5"])
                S.op("vector", lambda e, k=k, dstb=dstb: e.tensor_copy(out=dstb[:, k * 128:(k + 1) * 128], in_=PS[5][:, 0:128]),
                     reads=["ps5"], writes=["s2b"])
        def p4_merge(tt):
            mg = MG[tt % 2]; mgt = f"mg{tt % 2}"
            c0 = tt * 512
            for f in range(KC):
                wr, wrt = ring_load(w_in_h[CH_GATR + f])
                wa_, wat = ring_load(w_in_h[CH_GATA + f])
                wor, wort = ring_load(w_or_h[f])
                woa, woat = ring_load(w_oa_h[f])
                specs = ((wr, wrt, HT, "HT"), (wa_, wat, HT, "HT"), (wor, wort, YR, "YR"), (woa, woat, QT, "QTy"))
                for j, (w, wt, rT, rtok) in enumerate(specs):
                    for k in range(KC):
                        S.op("tensor", lambda e, j=j, k=k, w=w, rT=rT, c0=c0: e.matmul(PS[j][:, :], lhsT=w[:, k, :], rhs=rT[:, k, c0:c0 + 512],
                                                                            start=(k == 0), stop=(k == KC - 1)),
                             reads=[wt, rtok], writes=[f"ps{j}"], sig=(k == KC - 1))
                for j in range(2):
                    bci = (CH_GATR if j == 0 else CH_GATA) + f
                    S.op("scalar", lambda e, j=j, bci=bci: e.activation(out=SG[j], in_=PS[j][:, :], func=AF.Sigmoid, bias=binc[:, bci:bci + 1], scale=1.0),
                         reads=[f"ps{j}", "binc"], writes=[f"sg{j}"])
                S.op("vector", lambda e: e.tensor_tensor(out=SG[0], in0=SG[0], in1=PS[2][:, :], op=ALU.mult), reads=["sg0", "ps2"], writes=["sg0"])
                S.op("vector", lambda e: e.tensor_tensor(out=SG[1], in0=SG[1], in1=PS[3][:, :], op=ALU.mult), reads=["sg1", "ps3"], writes=["sg1"])
                S.op("vector", lambda e, f=f, mg=mg: e.tensor_tensor(out=mg[:, f, :], in0=SG[0], in1=SG[1], op=ALU.add),
                     reads=["sg0", "sg1"], writes=[mgt])
        XT4s = [XT4, carve(O_SP + 4096, 4096)]
        S.alias("xt40", ["xt4"]); S.alias("xt41", ["xc160"])

        def p4_main(tile):
            tt, t4 = tile // 4, tile % 4
            mg = MG[tt % 2]; mgt = f"mg{tt % 2}"
            XT4 = XT4s[tile % 2]; xt4t = f"xt4{tile % 2}"
            if True:
                r0 = tile * 128
                S.op("sync", lambda e, r0=r0: e.dma_start(out=XT4, in_=xe[(NST - 1) * T + r0:(NST - 1) * T + r0 + 128, :]), writes=[xt4t], dma=xt4t)
                for hf in range(2):
                    for k in range(KC):
                        S.op("tensor", lambda e, hf=hf, k=k, t4=t4, mg=mg: e.matmul(PS[4 + hf][:, :], lhsT=mg[:, k, t4 * 128:(t4 + 1) * 128],
                                                                                rhs=WOUT[:, k, hf * 512:(hf + 1) * 512], start=(k == 0), stop=(k == KC - 1)),
                             reads=[mgt, "wout"], writes=[f"ps{4 + hf}"], sig=(k == KC - 1))
                so, stk = newstat()
                for hf in range(2):
                    S.op("scalar", lambda e, hf=hf, so=so: e.activation(out=JUNK[:, 0:512], in_=PS[4 + hf][:, :], func=AF.Square,
                                                                        accum_out=stat[:, so + hf:so + hf + 1]), reads=[f"ps{4 + hf}"], writes=["junk", stk])
                S.op("vector", lambda e, so=so: e.tensor_tensor(out=stat[:, so + 2:so + 3], in0=stat[:, so:so + 1], in1=stat[:, so + 1:so + 2], op=ALU.add),
                     reads=[stk], writes=[stk])
                S.op("scalar", lambda e, so=so: e.activation(out=stat[:, so + 3:so + 4], in_=stat[:, so + 2:so + 3], func=AF.Sqrt, scale=1.0 / D, bias=EPS),
                     reads=[stk], writes=[stk])
                S.op("vector", lambda e, so=so: e.reciprocal(out=stat[:, so + 3:so + 4], in_=stat[:, so + 3:so + 4]), reads=[stk], writes=[stk])
                for hf in range(2):
                    S.op("vector", lambda e, hf=hf, so=so: e.scalar_tensor_tensor(out=MIXT[:, hf * 512:(hf + 1) * 512], in0=PS[4 + hf][:, :],
                                                                                  scalar=stat[:, so + 3:so + 4], in1=G1b[:, hf * 512:(hf + 1) * 512],
                                                                                  op0=ALU.mult, op1=ALU.mult),
                         reads=[f"ps{4 + hf}", stk, "G1b"], writes=["mixt"])
                S.op("vector", lambda e: e.tensor_tensor(out=MIXT, in0=MIXT, in1=XT4, op=ALU.add), reads=["mixt", xt4t], writes=["mixt"])
                S.op("sync", lambda e, r0=r0: e.dma_start(out=x1s[r0:r0 + 128, :], in_=MIXT), reads=["mixt"], writes=["x1sd"], dma="x1s")
                if debug:
                    S.op("sync", lambda e, r0=r0: e.dma_start(out=dbg["d_x1"][r0:r0 + 128, :], in_=MIXT), reads=["mixt"], dma="dbg")
                S.op("scalar", lambda e, so=so: e.activation(out=JUNK, in_=MIXT, func=AF.Square, accum_out=stat[:, so + 4:so + 5]),
                     reads=["mixt"], writes=["junk", stk])
                S.op("scalar", lambda e, so=so: e.activation(out=stat[:, so + 5:so + 6], in_=stat[:, so + 4:so + 5], func=AF.Sqrt, scale=1.0 / D, bias=EPS),
                     reads=[stk], writes=[stk])
                S.op("vector", lambda e, so=so: e.reciprocal(out=stat[:, so + 5:so + 6], in_=stat[:, so + 5:so + 6]), reads=[stk], writes=[stk])
                S.op("vector", lambda e, so=so: e.tensor_scalar(out=XT4, in0=MIXT, scalar1=stat[:, so + 5:so + 6], scalar2=None, op0=ALU.mult),
                     reads=["mixt", stk, xt4t], writes=[xt4t])
                for hf in range(2):
                    for kk in range(4):
                        k = hf * 4 + kk
                        S.op("tensor", lambda e, k=k, kk=kk, hf=hf: e.transpose(out=PS[6 + hf][:, kk * 128:(kk + 1) * 128], in_=XT4[:, k * 128:(k + 1) * 128],
                                                                                identity=ident[:]), reads=[xt4t, "ident"], writes=[f"ps{6 + hf}"], sig=(kk == 3))
                    for kk in range(4):
                        k = hf * 4 + kk
                        S.op("vector", lambda e, k=k, kk=kk, hf=hf: e.tensor_scalar(out=H2F[:, k, :], in0=PS[6 + hf][:, kk * 128:(kk + 1) * 128],
                                                                                   scalar1=S2[:, k:k + 1], scalar2=ada[:, 16 + k:17 + k],
                                                                                   op0=ALU.mult, op1=ALU.add),
                             reads=[f"ps{6 + hf}", "S2", "ada"], writes=["h2f"])
                h2t = H2TOK[tile % 2]; h2tt = f"h2t{tile % 2}"
                S.op("vector", lambda e: e.tensor_tensor(out=JUNK, in0=XT4, in1=S2B, op=ALU.mult), reads=[xt4t, "s2b", "junk"], writes=["junk"])
                S.op("vector", lambda e, h2t=h2t: e.tensor_tensor(out=h2t, in0=JUNK, in1=SH2B, op=ALU.add), reads=["junk", "s2b"], writes=[h2tt])
                for k in range(KC):
                    S.op("tensor", lambda e, k=k: e.matmul(PS[7][:, 0:NE], lhsT=H2F[:, k, :], rhs=routw[:, k, :], start=(k == 0), stop=(k == KC - 1)),
                         reads=["h2f", "routw"], writes=["ps7"], sig=(k == KC - 1))
                go, gtk = newstat()
                lg = Gt[:, tile, :]
                S.op("vector", lambda e, lg=lg: e.tensor_tensor(out=lg, in0=PS[7][:, 0:NE], in1=rbb[:], op=ALU.add), reads=["ps7", "rbb"], writes=["Gt"])
                S.op("vector", lambda e, lg=lg, go=go: e.max(out=stat[:, go:go + 8], in_=lg), reads=["Gt"], writes=[gtk])
                S.op("vector", lambda e, go=go: e.tensor_scalar(out=stat[:, go + 8:go + 9], in0=stat[:, go:go + 1], scalar1=-1.0, scalar2=None, op0=ALU.mult),
                     reads=[gtk], writes=[gtk])
                S.op("vector", lambda e, lg=lg, go=go: e.tensor_scalar(out=SG[0][:, 0:NE], in0=lg, scalar1=stat[:, go + 3:go + 4], scalar2=None, op0=ALU.is_ge),
                     reads=["Gt", gtk], writes=["sg0"])
                S.op("scalar", lambda e, lg=lg, go=go: e.activation(out=lg, in_=lg, func=AF.Exp, bias=stat[:, go + 8:go + 9], scale=1.0),
                     reads=["Gt", gtk], writes=["Gt"])
                S.op("vector", lambda e, lg=lg: e.tensor_tensor(out=lg, in0=lg, in1=SG[0][:, 0:NE], op=ALU.mult), reads=["Gt", "sg0"], writes=["Gt"])
                S.op("vector", lambda e, lg=lg, go=go: e.tensor_reduce(out=stat[:, go + 9:go + 10], in_=lg, axis=AX.X, op=ALU.add), reads=["Gt"], writes=[gtk])
                S.op("vector", lambda e, go=go: e.reciprocal(out=stat[:, go + 9:go + 10], in_=stat[:, go + 9:go + 10]), reads=[gtk], writes=[gtk])
                S.op("vector", lambda e, lg=lg, go=go: e.tensor_scalar(out=lg, in0=lg, scalar1=stat[:, go + 9:go + 10], scalar2=None, op0=ALU.mult),
                     reads=["Gt", gtk], writes=["Gt"])

        def p4_route(tile):
            lg = Gt[:, tile, :]
            h2t = H2TOK[tile % 2]; h2tt = f"h2t{tile % 2}"
            S.op("vector", lambda e, lg=lg, tile=tile: e.tensor_scalar(out=MB[:, tile, :], in0=lg, scalar1=0.0, scalar2=None, op0=ALU.is_gt),
                 reads=["Gt"], writes=["MB"])
            for ip in range(tile):
                S.op("tensor", lambda e, ip=ip: e.matmul(PS[7][:, 32:64], lhsT=onesb[:], rhs=MB[:, ip, :], start=(ip == 0), stop=False),
                     reads=["MB", "onesb"], writes=["ps7"], sig=False)
            S.op("tensor", lambda e, tile=tile: e.matmul(PS[7][:, 32:64], lhsT=trib[:], rhs=MB[:, tile, :], start=(tile == 0), stop=True),
                 reads=["MB", "trib"], writes=["ps7"])
            ro, rtk = newstat()
            SLF, SEL = SG[1][:, 0:NE], SG[1][:, 64:64 + NE]
            S.op("vector", lambda e: e.tensor_scalar(out=SEL, in0=PS[7][:, 32:64], scalar1=CAP - 0.5, scalar2=1.0e9, op0=ALU.is_ge, op1=ALU.mult),
                 reads=["ps7", "sg1"], writes=["sg1"])
            S.op("vector", lambda e: e.tensor_tensor(out=SLF, in0=PS[7][:, 32:64], in1=rcf[:, 0:NE], op=ALU.add), reads=["ps7", "rcf", "sg1"], writes=["sg1"])
            S.op("vector", lambda e: e.tensor_tensor(out=SLF, in0=SLF, in1=SEL, op=ALU.add), reads=["sg1"], writes=["sg1"])
            S.op("vector", lambda e, lg=lg, ro=ro: e.max(out=stat[:, ro:ro + 8], in_=lg), reads=["Gt"], writes=[rtk])
            for kk in range(4):
                S.op("vector", lambda e, lg=lg, ro=ro, kk=kk: e.tensor_scalar(out=SEL, in0=lg, scalar1=stat[:, ro + kk:ro + kk + 1], scalar2=None, op0=ALU.is_equal),
                     reads=["Gt", rtk, "sg1"], writes=["sg1"])
                S.op("vector", lambda e: e.tensor_tensor(out=SEL, in0=SEL, in1=SLF, op=ALU.mult), reads=["sg1"], writes=["sg1"])
                S.op("vector", lambda e, ro=ro, kk=kk: e.tensor_reduce(out=stat[:, ro + 8 + kk:ro + 9 + kk], in_=SEL, axis=AX.X, op=ALU.add),
                     reads=["sg1"], writes=[rtk])
            S.op("vector", lambda e, ro=ro, tile=tile: e.tensor_copy(out=IDX[:, tile, :], in_=stat[:, ro + 8:ro + 12]), reads=[rtk], writes=["IDX"])
            S.op("vector", lambda e, ro=ro: e.tensor_scalar(out=stat[:, ro + 12:ro + 16], in0=stat[:, ro + 8:ro + 12], scalar1=NE * CAP - 0.5, scalar2=None, op0=ALU.is_lt),
                 reads=[rtk], writes=[rtk])
            S.op("vector", lambda e, ro=ro, tile=tile: e.tensor_tensor(out=GV[:, tile, :], in0=stat[:, ro:ro + 4], in1=stat[:, ro + 12:ro + 16], op=ALU.mult),
                 reads=[rtk], writes=["GV"])
            for kk in range(4):
                S.op("gpsimd", lambda e, tile=tile, kk=kk, h2t=h2t: e.indirect_dma_start(
                    out=xs_d, out_offset=bass.IndirectOffsetOnAxis(ap=IDX[:, tile, kk:kk + 1], axis=0), in_=h2t, in_offset=None,
                    bounds_check=S.regs["bnd"], oob_is_err=False), reads=["IDX", h2tt], writes=[f"xs{tile}_{kk}"], dma=f"scat{kk}")
        p4_merge(0)
        for tile in range(16):
            if tile % 4 == 0 and tile // 4 + 1 < 4:
                p4_merge(tile // 4 + 1)
            p4_main(tile)
            if tile >= 1:
                p4_route(tile - 1)
        p4_route(15)
        for ip in range(16):
            S.op("tensor", lambda e, ip=ip: e.matmul(PS[7][:, 64:96], lhsT=onesb[:], rhs=MB[:, ip, :], start=(ip == 0), stop=(ip == 15)),
                 reads=["MB", "onesb"], writes=["ps7"], sig=(ip == 15))
        FORCE = os.environ.get("KFORCE")
        S.op("vector", lambda e: e.tensor_scalar(out=FLG[:], in0=PS[7][:, 64:96], scalar1=(-1.0 if FORCE else 512.0), scalar2=None, op0=ALU.is_gt),
             reads=["ps7"], writes=["FLG"])
        xs_tokens = [f"xs{tile}_{kk}" for tile in range(16) for kk in range(4)]
        if debug:
            S.op("sync", lambda e: e.dma_start(out=dbg["d_G"], in_=Gt.rearrange("p a b -> p (a b)")), reads=["Gt"], dma="dbg")
            S.op("sync", lambda e: e.dma_start(out=dbg["d_idx"], in_=IDX.rearrange("p a b -> p (a b)")), reads=["IDX"], dma="dbg")
            S.op("sync", lambda e: e.dma_start(out=dbg["d_gv"], in_=GV.rearrange("p a b -> p (a b)")), reads=["GV"], dma="dbg")

        if stop == "p4":
            return finish(["x1s"])

        allold = ["HT", "YR", "QTy", "wout", "mg0", "mg1", "xt4", "mixt", "h2f", "sg0", "sg1", "junk", "wrg", "xc160", "s2b", "h2t0", "h2t1"] \
            + [f"ring{i}" for i in range(8)]
        NBL = CAP // 128
        XE = [carve(16384 * i, 16384, BF16, "p (b d) -> p b d", b=NBL) for i in range(2)]
        XET = [carve(32768 + 16384 * i, 16384, BF16, "p (k t) -> p k t", k=KC) for i in range(2)]
        ACTT = carve(65536, 16384, BF16, "p (k t) -> p k t", k=KC)
        W2B = [carve(81920 + 16384 * i, 16384, BF16, "p (k n) -> p k n", k=KC) for i in range(2)]
        W1R = [carve(114688 + 4096 * i, 4096, BF16, "p (a k m) -> p a k m", a=2, k=KC) for i in range(8)]
        TMP = [[carve(147456 + 8192 * s_ + 2048 * j, 2048) for j in range(4)] for s_ in range(2)]
        YS = [carve(163840 + 4096 * i, 4096) for i in range(2)]
        W1X = [carve(172032 + 4096 * i, 4096, BF16, "p (a k m) -> p a k m", a=2, k=KC) for i in range(2)]
        names5 = ["xe0", "xe1", "xeh0", "xeh1", "xet0", "xet1", "actt", "w2b0", "w2b1", "ys0", "ys1", "w1x0", "w1x1"] + [f"w1r{i}" for i in range(8)] \
            + [f"tmp{s_}{j}" for s_ in range(2) for j in range(4)]
        for nm in names5:
            S.alias(nm, allold)

        def w1_load(ex_, c_):
            S.op("gpsimd", lambda e: e.dma_start(out=W1R[c_], in_=w1_h[ex_, c_]), writes=[f"w1r{c_}"], dma=f"w1r{c_}")

        def xe_load(ex_):
            S.op("sync", lambda e, ex_=ex_: e.dma_start(out=XE[ex_ % 2][:, 0:4, :],
                                                        in_=xs_d[ex_ * CAP:ex_ * CAP + 512, :].rearrange("(b p) d -> p b d", p=128)),
                 reads=xs_tokens, writes=[f"xe{ex_ % 2}"], dma=f"xe{ex_ % 2}")

        def xe_load_hi(ex_):
            S.op("sync", lambda e, ex_=ex_: e.dma_start(out=XE[ex_ % 2][:, 4:8, :],
                                                        in_=xs_d[ex_ * CAP + 512:(ex_ + 1) * CAP, :].rearrange("(b p) d -> p b d", p=128)),
                 reads=xs_tokens, writes=[f"xeh{ex_ % 2}"], dma=f"xeh{ex_ % 2}")

        def w2_load(ex_):
            for q4 in range(4):
                S.op("gpsimd", lambda e, ex_=ex_, q4=q4: e.dma_start(out=W2B[ex_ % 2][:, 2 * q4:2 * q4 + 2, :], in_=w2_h[ex_, :, 2 * q4:2 * q4 + 2, :]),
                     writes=[f"w2b{ex_ % 2}"], dma=f"w2b{ex_ % 2}")

        cnt5 = {"it": 0, "ys": 0}

        def moe_T(ex, half):
            xeb, xetb = XE[ex % 2], XET[ex % 2]
            xetk, xettk = (f"xe{ex % 2}" if half == 0 else f"xeh{ex % 2}"), f"xet{ex % 2}"
            for k in range(KC):
                pbk = PS[6 + (k % 2)]
                pbt = f"ps{6 + (k % 2)}"
                pv = pbk[:, 0:256].bitcast(BF16)
                for b4 in range(4):
                    b_ = half * 4 + b4
                    S.op("tensor", lambda e, k=k, b_=b_, b4=b4, pv=pv: e.transpose(out=pv[:, b4 * 128:(b4 + 1) * 128], in_=xeb[:, b_, k * 128:(k + 1) * 128],
                                                                                 identity=identb[:]), reads=[xetk, "identb"], writes=[pbt], sig=(b4 == 3))
                if k % 2 == 0:
                    S.op("scalar", lambda e, k=k, pv=pv: e.copy(out=xetb[:, k, half * 512:(half + 1) * 512], in_=pv), reads=[pbt], writes=[xettk])
                else:
                    S.op("vector", lambda e, k=k, pv=pv: e.tensor_copy(out=xetb[:, k, half * 512:(half + 1) * 512], in_=pv), reads=[pbt], writes=[xettk])

        def moe_H(ex, tt):
            xetb = XET[ex % 2]
            xettk = f"xet{ex % 2}"
            if tt == 1:
                for c in range(2):
                    S.op("gpsimd", lambda e, c=c: e.dma_start(out=W1X[c], in_=w1_h[ex, c]), writes=[f"w1x{c}"], dma=f"w1x{c}")
            for c in range(8):
                if tt == 0:
                    w1 = W1R[c]; w1t = f"w1r{c}"
                else:
                    w1 = W1X[c % 2]; w1t = f"w1x{c % 2}"
                sset = cnt5["it"] % 2
                cnt5["it"] += 1
                tg, tsg, tu, tgs = TMP[sset]
                for a in range(2):
                    pb = PS[2 * sset + a]
                    for k in range(KC):
                        S.op("tensor", lambda e, a=a, k=k, w1=w1, pb=pb: e.matmul(pb[:, :], lhsT=w1[:, a, k, :], rhs=xetb[:, k, tt * 512:(tt + 1) * 512],
                                                                               start=(k == 0), stop=(k == KC - 1)),
                             reads=[w1t, xettk], writes=[f"ps{2 * sset + a}"], sig=(k == KC - 1))
                pg, pl = PS[2 * sset], PS[2 * sset + 1]
                S.op("vector", lambda e, c=c, tg=tg, pg=pg: e.tensor_scalar(out=tg, in0=pg[:, :], scalar1=b1c[:, ex, c:c + 1], scalar2=7.0,
                                                                           op0=ALU.add, op1=ALU.min), reads=[f"ps{2 * sset}", "b1c"], writes=[f"tmp{sset}0"])
                S.op("scalar", lambda e, tg=tg, tsg=tsg: e.activation(out=tsg, in_=tg, func=AF.Sigmoid, scale=1.702), reads=[f"tmp{sset}0"], writes=[f"tmp{sset}1"])
                S.op("vector", lambda e, c=c, tu=tu, pl=pl: e.tensor_scalar(out=tu, in0=pl[:, :], scalar1=b1c[:, ex, 8 + c:9 + c], scalar2=8.0,
                                                                           op0=ALU.add, op1=ALU.min), reads=[f"ps{2 * sset + 1}", "b1c"], writes=[f"tmp{sset}2"])
                S.op("vector", lambda e, tu=tu, tg=tg, tgs=tgs: e.scalar_tensor_tensor(out=tgs, in0=tu, scalar=-6.0, in1=tg, op0=ALU.max, op1=ALU.mult),
                     reads=[f"tmp{sset}2", f"tmp{sset}0"], writes=[f"tmp{sset}3"])
                S.op("vector", lambda e, c=c, tsg=tsg, tgs=tgs: e.tensor_tensor(out=ACTT[:, c, tt * 512:(tt + 1) * 512], in0=tgs, in1=tsg, op=ALU.mult),
                     reads=[f"tmp{sset}3", f"tmp{sset}1"], writes=[f"actt{tt}"])
                if tt == 1 and c + 2 < 8:
                    S.op("gpsimd", lambda e, c=c: e.dma_start(out=W1X[c % 2], in_=w1_h[ex, c + 2]), writes=[f"w1x{c % 2}"], dma=f"w1x{c % 2}")
                if tt == 0 and ex + 1 < NE:
                    w1_load(ex + 1, c)

        def moe_Y(ex, half):
            w2 = W2B[ex % 2]; w2t = f"w2b{ex % 2}"
            for b4 in range(4):
                b_ = half * 4 + b4
                ys = YS[cnt5["ys"] % 2]; yst = f"ys{cnt5['ys'] % 2}"
                ysk = f"ysst{cnt5['ys'] % 2}"
                cnt5["ys"] += 1
                for hf in range(2):
                    pi = 4 + hf
                    for c in range(8):
                        S.op("tensor", lambda e, c=c, b_=b_, hf=hf, pi=pi: e.matmul(PS[pi][:, :], lhsT=ACTT[:, c, b_ * 128:(b_ + 1) * 128],
                                                                                  rhs=w2[:, c, hf * 512:(hf + 1) * 512], start=(c == 0), stop=(c == 7)),
                             reads=[f"actt{half}", w2t], writes=[f"ps{pi}"], sig=(c == 7))
                    S.op("scalar", lambda e, hf=hf, pi=pi, ys=ys: e.copy(out=ys[:, hf * 512:(hf + 1) * 512], in_=PS[pi][:, :]), reads=[f"ps{pi}"], writes=[yst])
                r0 = ex * CAP + b_ * 128
                S.op("sync", lambda e, r0=r0, ys=ys: e.dma_start(out=ys_d[r0:r0 + 128, :], in_=ys), reads=[yst], writes=[f"ysd{ex}_{b_}"], dma=ysk)

        S.alias("actt0", ["actt"]); S.alias("actt1", ["actt"])
        xe_load(0)
        w2_load(0)
        for c in range(8):
            w1_load(0, c)
        for ex in range(NE):
            if ex + 1 < NE:
                xe_load(ex + 1)
                w2_load(ex + 1)
            if ex == 0:
                moe_T(ex, 0)
            moe_H(ex, 0)
            S.region_begin(FLG[0:1, ex:ex + 1], "FLG")
            xe_load_hi(ex)
            moe_T(ex, 1)
            moe_H(ex, 1)
            moe_Y(ex, 1)
            S.region_end()
            if ex + 1 < NE:
                moe_T(ex + 1, 0)
            moe_Y(ex, 0)
        ys_tokens = [f"ysd{ex}_{b_}" for ex in range(NE) for b_ in range(NBL)]
        names5 = names5 + ["actt0", "actt1"]

        S.alias("fin", names5)
        ACC6 = [carve(4096 * i, 4096) for i in range(2)]
        XO = [carve(8192 + 4096 * i, 4096) for i in range(2)]
        JK = carve(16384, 4096)
        B2S = [carve(20480 + 2048 * i, 2048) for i in range(2)]
        GTT = carve(24576, 2048)
        NYG = 4
        YG = [[carve(28672 + 16384 * s_ + 4096 * j, 4096) for j in range(4)] for s_ in range(NYG)]
        n6 = ["acc0", "acc1", "xo0", "xo1", "jk", "b2s0", "b2s1", "gtt"] + [f"yg{s_}{j}" for s_ in range(NYG) for j in range(4)]
        for nm in n6:
            S.alias(nm, ["fin"])
        for s_ in range(NYG):
            for j in range(4):
                S.op("gpsimd", lambda e, s_=s_, j=j: e.memset(YG[s_][j], 0.0), writes=[f"yg{s_}{j}"])
        for hf in range(2):
            S.op("sync", lambda e, hf=hf: e.dma_start(out=B2S[hf][0:NE, :], in_=b2_h[:, hf * 512:(hf + 1) * 512]), writes=[f"b2s{hf}"], dma=f"b2s{hf}")
        for tile in range(16):
            s6 = tile % 2
            sg6 = tile % NYG
            acc = ACC6[s6]; acct = f"acc{s6}"
            xo = XO[s6]; xot = f"xo{s6}"
            r0 = tile * 128
            S.op("sync", lambda e, r0=r0, xo=xo: e.dma_start(out=xo, in_=x1s[r0:r0 + 128, :]), reads=["x1sd"], writes=[xot], dma=xot)
            for kk in range(4):
                S.op("gpsimd", lambda e, tile=tile, kk=kk, sg6=sg6: e.indirect_dma_start(
                    out=YG[sg6][kk], out_offset=None, in_=ys_d, in_offset=bass.IndirectOffsetOnAxis(ap=IDX[:, tile, kk:kk + 1], axis=0),
                    bounds_check=S.regs["bnd"], oob_is_err=False), reads=ys_tokens + ["IDX"], writes=[f"yg{sg6}{kk}"], dma=f"yg{sg6}{kk}")
            S.op("tensor", lambda e, tile=tile: e.transpose(out=PS[7][0:NE, 0:128], in_=Gt[:, tile, :], identity=ident[:]),
                 reads=["Gt", "ident"], writes=["ps7"])
            S.op("vector", lambda e: e.tensor_copy(out=GTT[0:NE, 0:128], in_=PS[7][0:NE, 0:128]), reads=["ps7"], writes=["gtt"])
            for hf in range(2):
                S.op("tensor", lambda e, hf=hf: e.matmul(PS[4 + hf][:, :], lhsT=GTT[0:NE, 0:128], rhs=B2S[hf][0:NE, :], start=True, stop=True),
                     reads=[f"b2s{hf}", "gtt"], writes=[f"ps{4 + hf}"])
                S.op("vector", lambda e, hf=hf, acc=acc: e.tensor_copy(out=acc[:, hf * 512:(hf + 1) * 512], in_=PS[4 + hf][:, :]),
                     reads=[f"ps{4 + hf}"], writes=[acct])
            for kk in range(4):
                S.op("vector", lambda e, tile=tile, kk=kk, sg6=sg6, acc=acc: e.scalar_tensor_tensor(out=acc, in0=YG[sg6][kk], scalar=GV[:, tile, kk:kk + 1], in1=acc,
                                                                                            op0=ALU.mult, op1=ALU.add),
                     reads=[f"yg{sg6}{kk}", "GV", acct], writes=[acct])
            so, stk = newstat()
            S.op("scalar", lambda e, acc=acc, so=so: e.activation(out=JK, in_=acc, func=AF.Square, accum_out=stat[:, so:so + 1]),
                 reads=[acct], writes=["jk", stk])
            S.op("scalar", lambda e, so=so: e.activation(out=stat[:, so + 1:so + 2], in_=stat[:, so:so + 1], func=AF.Sqrt, scale=1.0 / D, bias=EPS),
                 reads=[stk], writes=[stk])
            S.op("vector", lambda e, so=so: e.reciprocal(out=stat[:, so + 1:so + 2], in_=stat[:, so + 1:so + 2]), reads=[stk], writes=[stk])
            S.op("vector", lambda e, acc=acc, so=so: e.scalar_tensor_tensor(out=acc, in0=acc, scalar=stat[:, so + 1:so + 2], in1=G2b[:],
                                                                          op0=ALU.mult, op1=ALU.mult), reads=[acct, stk, "G2b"], writes=[acct])
            S.op("vector", lambda e, acc=acc, xo=xo: e.tensor_tensor(out=xo, in0=xo, in1=acc, op=ALU.add), reads=[acct, xot], writes=[xot])
            S.op("sync", lambda e, r0=r0, xo=xo: e.dma_start(out=out[r0:r0 + 128, :], in_=xo), reads=[xot], dma="outst")
        return finish()


def _alibi_bias():
    slopes = np.array([2.0 ** (-8.0 * (h + 1) / 16) for h in range(16)], dtype=np.float32)
    qi = np.arange(128)[:, None]
    ci = np.arange(256)[None, :]
    dist = qi + 128 - ci
    valid = (dist >= 0) & (dist < 128)
    b = np.where(valid[:, None, :], -slopes[None, :, None] * dist[:, None, :].astype(np.float32), np.float32(NEG))
    return np.ascontiguousarray(b.astype(np.float32))


def _col(v, k=KC):
    return np.ascontiguousarray(np.asarray(v, np.float32).reshape(k, 128).T)


def prepare_inputs(x, c, w_ada, b_ada, norm_pre_mix, norm_post_mix, norm_pre_ffn, norm_post_ffn,
                   w_in, b_in, conv_w, conv_b, rg_w_a, rg_b_a, rg_w_x, rg_b_x, rg_lambda,
                   attn_sinks, w_o_rnn, w_o_attn, w_out, router_w, router_b,
                   moe_w1, moe_b1, moe_w2, moe_b2):
    f = lambda a: np.asarray(a, np.float32)
    x, c = f(x), f(c)
    L = 0
    shared = {}
    shared["wada"] = np.ascontiguousarray(f(w_ada)[L].reshape(KC, 128, 12, 512).transpose(2, 1, 0, 3))
    shared["bada_col"] = _col(f(b_ada)[L], 48)
    shared["bada_row"] = np.ascontiguousarray(f(b_ada)[L].reshape(6, D))
    gam = np.stack([f(norm_pre_mix)[L], f(norm_post_mix)[L], f(norm_pre_ffn)[L], f(norm_post_ffn)[L]])
    shared["gam_col"] = np.ascontiguousarray(gam.reshape(4, KC, 128).transpose(2, 0, 1))
    shared["gam_row"] = np.ascontiguousarray(gam)
    shared["w_in_h"] = np.ascontiguousarray(f(w_in)[L].reshape(KC, 128, 44, 128).transpose(2, 1, 0, 3))
    shared["b_in_col"] = _col(f(b_in)[L], 44)
    wk = f(w_in)[L][:, 3072:3328].reshape(KC, 128, 2, 2, 64)[:, :, :, ::-1, :].reshape(KC, 128, 2, 128)
    shared["w_ks_h"] = np.ascontiguousarray(wk.transpose(2, 1, 0, 3))
    bk = f(b_in)[L][3072:3328].reshape(2, 2, 64)[:, ::-1, :].reshape(2, 128)
    shared["b_ks_col"] = np.ascontiguousarray(bk.T)
    shared["b_v_row"] = np.ascontiguousarray(f(b_in)[L][3328:3584])
    cw = np.concatenate([f(conv_w)[L], f(conv_b)[L][None, :]], axis=0)
    shared["conv_col"] = np.ascontiguousarray(cw.reshape(5, KC, 128).transpose(2, 1, 0))
    rg = np.zeros((128, 2, KC, 128), np.float32)
    for a, wsrc in enumerate((f(rg_w_a)[L], f(rg_w_x)[L])):
        for cc in range(KC):
            rg[0:64, a, cc, 0:64] = wsrc[2 * cc]
            rg[64:128, a, cc, 64:128] = wsrc[2 * cc + 1]
    shared["rgw"] = rg
    shared["rgb_col"] = np.ascontiguousarray(np.stack([_col(f(rg_b_a)[L]), _col(f(rg_b_x)[L])], axis=1))
    shared["lam_col"] = _col(f(rg_lambda)[L])
    shared["sinks_row"] = np.ascontiguousarray(f(attn_sinks)[L].reshape(4, 4)[:, [0, 2, 1, 3]].reshape(16))
    shared["w_or_h"] = np.ascontiguousarray(f(w_o_rnn)[L].reshape(KC, 128, KC, 128).transpose(2, 1, 0, 3))
    shared["w_oa_h"] = np.ascontiguousarray(f(w_o_attn)[L].reshape(KC, 128, KC, 128).transpose(2, 1, 0, 3))
    shared["w_out_h"] = np.ascontiguousarray(f(w_out)[L].reshape(KC, 128, D).transpose(1, 0, 2))
    shared["router_h"] = np.ascontiguousarray(f(router_w)[L].reshape(KC, 128, NE).transpose(1, 0, 2))
    shared["router_b"] = np.ascontiguousarray(f(router_b)[L])
    shared["w1_h"] = np.ascontiguousarray(f(moe_w1)[L].reshape(NE, KC, 128, 8, 128, 2).transpose(0, 3, 2, 5, 1, 4))
    b1 = f(moe_b1)[L].reshape(NE, 8, 128, 2)
    shared["b1_col"] = np.ascontiguousarray(b1.transpose(2, 0, 3, 1).reshape(128, NE, 16))
    shared["w2_h"] = np.ascontiguousarray(f(moe_w2)[L].reshape(NE, KC, 128, D).transpose(0, 2, 1, 3))
    shared["b2_h"] = np.ascontiguousarray(f(moe_b2)[L])
    shared["abias_h"] = _alibi_bias()
    in_maps = []
    for r in range(NCORES):
        b, j = r // 4, r % 4
        m = dict(shared)
        xe = np.zeros((NST * T, D), np.float32)
        n_real = (j + 1) * T
        xe[NST * T - n_real:] = x[b, :n_real]
        m["xe"] = xe
        m["ccol"] = _col(c[b])
        fl = np.zeros((128, 16), np.float32)
        for st in range(NST):
            valid = 1.0 if st >= NST - 1 - j else 0.0
            first = 1.0 if st == NST - 1 - j else 0.0
            fl[:, st] = valid
            fl[:, 4 + st] = first
        fl[:, 8] = 0.0 if j > 0 else NEG
        m["flags_h"] = fl
        rc = np.zeros((128, 160), np.float32)
        rc[:, 0:32] = (np.arange(32, dtype=np.float32) * CAP)[None, :]
        rc[:, 32:160] = (np.arange(128)[:, None] < np.arange(128)[None, :]).astype(np.float32)
        m["rc_h"] = rc
        in_maps.append(m)
    return in_maps


_NC_CACHE = {}


def kernel(**inputs):
    debug = bool(os.environ.get("KDEBUG"))
    stop = os.environ.get("KSTOP") or None
    in_maps = prepare_inputs(**inputs)
    if stop is not None:
        for m in in_maps:
            m["w1_h"] = m["w1_h"][:1]
            m["w2_h"] = m["w2_h"][:1]
    if (debug, stop) not in _NC_CACHE:
        _NC_CACHE[(debug, stop)] = build_nc(debug, stop)
    nc = _NC_CACHE[(debug, stop)]
    res = run_bass_kernel_spmd(nc, in_maps, core_ids=list(range(NCORES)))
    outs = [np.asarray(r["out"], np.float32) for r in res.results]
    full = np.stack([np.concatenate(outs[0:4], axis=0), np.concatenate(outs[4:8], axis=0)], axis=0)
    if debug:
        kernel.last_results = res.results
    return full.astype(np.float32)
```

```python
import os
import numpy as np
from contextlib import ExitStack
import concourse.bass as bass
import concourse.mybir as mybir
from concourse.bass_utils import run_bass_kernel_spmd

F32, BF16 = mybir.dt.float32, mybir.dt.bfloat16
AF = mybir.ActivationFunctionType
ALU = mybir.AluOpType
AX = mybir.AxisListType

NCORES = 8
D = 1024
T = 2048
NST = 4
KC = 8
NE = 32
EPS = 1e-6
NEG = -30000.0
CAP = 1024
U32 = mybir.dt.uint32
ENGS = ("sync", "scalar", "vector", "gpsimd", "tensor")
SEM_ROT = 3000


class Sched:
    def __init__(self, nc, stack):
        self.nc = nc
        self._stack = stack
        self.ops = {e: [] for e in ENGS}
        self.cur_sem = {}
        self.waited = {e: {} for e in ENGS}
        self.last_w = {}
        self.readers = {}
        self.nsem = 0
        self.dma_sems = {}
        self.pending = {e: [] for e in ENGS}
        self.regs = {}
        self.region = None
        self.nregion = 0
        self._saved_waited = None

    def _new_sem(self, name):
        self.nsem += 1
        return self._stack.enter_context(self.nc.semaphore(f"{name}_{self.nsem}"))

    def region_begin(self, flag_ap, flag_tok):
        self.nregion += 1
        for eng in ENGS:
            assert not self.pending[eng] or eng == "tensor" or True
        self.region = {"id": self.nregion, "flag": flag_ap, "tok": flag_tok, "seen": set()}
        self._saved_waited = {e: dict(d) for e, d in self.waited.items()}

    def region_end(self):
        self.region = None
        self.waited = self._saved_waited
        self._saved_waited = None

    def _eng_completion(self, eng):
        cs = self.cur_sem.get(eng)
        if cs is None or (cs[1] >= SEM_ROT and self.region is None):
            cs = [self._new_sem(f"s_{eng}"), 0]
            self.cur_sem[eng] = cs
        cs[1] += 1
        return (cs[0], cs[1], 1)

    def _dma_completion(self, key):
        ds = self.dma_sems.get(key)
        if ds is None or (ds[1] >= SEM_ROT * 8 and self.region is None):
            ds = [self._new_sem("d"), 0]
            self.dma_sems[key] = ds
        ds[1] += 16
        return (ds[0], ds[1], 16)

    def op(self, eng, fn, reads=(), writes=(), dma=None, sig=True):
        rg = self.region
        if rg is not None and eng not in rg["seen"]:
            rg["seen"].add(eng)
            self.region = None
            flag = rg["flag"]
            self.op(eng, lambda e, eng=eng, flag=flag: e.reg_load(self.regs["flag_" + eng], flag), reads=[rg["tok"]], sig=False)
            self.region = rg
        deps = []
        for t in reads:
            deps.extend(self.last_w.get(t, ()))
        for t in writes:
            deps.extend(self.last_w.get(t, ()))
            deps.extend(self.readers.get(t, ()))
        need = {}
        for (s, v, _) in deps:
            k = id(s)
            if k not in need or need[k][1] < v:
                need[k] = (s, v)
        waits = []
        wd = self.waited[eng]
        for k, (s, v) in need.items():
            if wd.get(k, 0) >= v:
                continue
            wd[k] = v
            waits.append((s, v))
        rid = self.region["id"] if self.region is not None else 0
        if not sig:
            self.ops[eng].append((waits, fn, None, rid))
            self.pending[eng].extend(reads)
            return None
        if dma is not None:
            ds0 = self.dma_sems.get(dma)
            before = (ds0[0], ds0[1]) if ds0 is not None and not (ds0[1] >= SEM_ROT * 8 and self.region is None) else None
        else:
            cs0 = self.cur_sem.get(eng)
            before = (cs0[0], cs0[1]) if cs0 is not None and not (cs0[1] >= SEM_ROT and self.region is None) else None
        comp = self._dma_completion(dma) if dma is not None else self._eng_completion(eng)
        self.ops[eng].append((waits, fn, comp, rid, before))
        if dma is None:
            for t in self.pending[eng]:
                self.readers.setdefault(t, []).append(comp)
            self.pending[eng] = []
        for t in reads:
            self.readers.setdefault(t, []).append(comp)
        for t in writes:
            self.last_w[t] = [comp]
            self.readers[t] = []
        return comp

    def alias(self, new, olds):
        acc = list(self.last_w.get(new, ())) + list(self.readers.get(new, ()))
        for t in olds:
            acc.extend(self.last_w.get(t, ()))
            acc.extend(self.readers.get(t, ()))
        self.last_w[new] = acc
        self.readers[new] = []

    def final_wait(self, eng, keys):
        waits = [(self.dma_sems[k][0], self.dma_sems[k][1]) for k in keys]
        self.ops[eng].append((waits, None, None, 0))

    def emit(self, block):
        def emit_op(e, rec):
            waits, fn, comp = rec[0], rec[1], rec[2]
            for (s_, v) in waits:
                e.wait_ge(s_, v)
            if fn is not None:
                ins = fn(e)
                if comp is not None:
                    ins.then_inc(comp[0], comp[2])

        def mk(engname):
            def body(e):
                if engname == "gpsimd":
                    r = e.alloc_register("bnd")
                    e.reg_mov(r, NE * CAP - 1)
                    self.regs["bnd"] = r
                freg = e.alloc_register("rflag")
                self.regs["flag_" + engname] = freg
                ops = self.ops[engname]
                i = 0
                while i < len(ops):
                    rid = ops[i][3]
                    if rid == 0:
                        emit_op(e, ops[i])
                        i += 1
                        continue
                    j = i
                    while j < len(ops) and ops[j][3] == rid:
                        j += 1
                    run = ops[i:j]
                    with e.If_ne(freg, 0):
                        for rec in run:
                            emit_op(e, rec)
                    with e.Else():
                        tot = {}
                        for rec in run:
                            comp = rec[2]
                            if comp is None:
                                continue
                            k = id(comp[0])
                            if k not in tot:
                                before = rec[4]
                                tot[k] = [comp[0], before[1] if before is not None else 0, 0]
                            tot[k][2] += comp[2]
                        for sem_, base, total in tot.values():
                            if base > 0:
                                e.wait_ge(sem_, base)
                            e.sem_inc(sem_, total)
                    i = j
            return body
        block.sync(mk("sync"))
        block.scalar(mk("scalar"))
        block.vector(mk("vector"))
        block.gpsimd(mk("gpsimd"))
        block.tensor(mk("tensor"))


CH_XR, CH_GR, CH_Q, CH_K, CH_V, CH_GATR, CH_GATA = 0, 8, 16, 24, 26, 28, 36


def build_nc(debug=False, stop=None):
    nc = bass.Bass("TRN2", target_bir_lowering=False)

    def din(name, shape, dt=F32):
        return nc.dram_tensor(name, list(shape), dt, kind="ExternalInput").ap()

    xe = din("xe", [NST * T, D])
    ccol = din("ccol", [128, KC])
    wada = din("wada", [12, 128, KC, 512])
    bada_col = din("bada_col", [128, 48])
    bada_row = din("bada_row", [6, D])
    gam_col = din("gam_col", [128, 4, KC])
    gam_row = din("gam_row", [4, D])
    w_in_h = din("w_in_h", [44, 128, KC, 128])
    b_in_col = din("b_in_col", [128, 44])
    w_ks_h = din("w_ks_h", [2, 128, KC, 128])
    b_ks_col = din("b_ks_col", [128, 2])
    b_v_row = din("b_v_row", [256])
    conv_col = din("conv_col", [128, KC, 5])
    rgw = din("rgw", [128, 2, KC, 128])
    rgb_col = din("rgb_col", [128, 2, KC])
    lam_col = din("lam_col", [128, KC])
    sinks_row = din("sinks_row", [16])
    w_or_h = din("w_or_h", [KC, 128, KC, 128])
    w_oa_h = din("w_oa_h", [KC, 128, KC, 128])
    w_out_h = din("w_out_h", [128, KC, D])
    router_h = din("router_h", [128, KC, NE])
    router_b = din("router_b", [NE])
    NEd = NE if stop is None else 1
    w1_h = din("w1_h", [NEd, 8, 128, 2, KC, 128])
    b1_col = din("b1_col", [128, NE, 16])
    w2_h = din("w2_h", [NEd, 128, KC, D])
    b2_h = din("b2_h", [NE, D])
    abias_h = din("abias_h", [128, 16, 256])
    flags_h = din("flags_h", [128, 16])
    rc_h = din("rc_h", [128, 160])
    out = nc.dram_tensor("out", [T, D], F32, kind="ExternalOutput").ap()
    x1s = nc.dram_tensor("x1s", [T, D], F32).ap()
    xs_d = nc.dram_tensor("xs_d", [NE * CAP, D], BF16).ap()
    ys_d = nc.dram_tensor("ys_d", [NE * CAP, D], F32).ap()
    dbg = {}
    if debug:
        for nm, shp, dt in (("d_yr", [128, KC * T], BF16), ("d_ya", [128, KC * T], BF16),
                            ("d_x1", [T, D], F32), ("d_G", [128, 16 * NE], F32),
                            ("d_idx", [128, 64], U32), ("d_gv", [128, 64], F32), ("d_ada", [128, 32], F32),
                            ("d_g1b", [128, 2 * D], F32)):
            dbg[nm] = nc.dram_tensor(nm, shp, dt, kind="ExternalOutput").ap()

    with ExitStack() as es:
        S = Sched(nc, es)

        def finish(extra=()):
            keys = [k for k in (["outst", "dbg"] + list(extra)) if k in S.dma_sems]
            S.final_wait("sync", keys)
            with nc.Block() as block:
                S.emit(block)
            return nc

        def sbt(name, shape, dt=F32):
            return es.enter_context(nc.sbuf_tensor(name, list(shape), dt))

        ARW = 46720
        AR = sbt("arena", [128, ARW], F32)

        def carve(off, nbytes, dt=F32, pat=None, **kw):
            assert off % 4 == 0 and nbytes % 4 == 0 and off + nbytes <= ARW * 4, (off, nbytes)
            v = AR[:, off // 4:(off + nbytes) // 4]
            if dt != F32:
                v = v.bitcast(dt)
            if pat is not None:
                v = v.rearrange(pat, **kw)
            return v

        PS = [es.enter_context(nc.psum_tensor(f"ps{i}", [128, 512], F32)) for i in range(8)]

        ident = sbt("ident", [128, 128])
        identb = sbt("identb", [128, 128], BF16)
        ones_r = sbt("ones_r", [1, 128])
        flags = sbt("flags", [128, 16])
        ccs = sbt("ccs", [128, KC])
        scs = sbt("scs", [128, KC])
        ada = sbt("ada", [128, 32])
        badac = sbt("badac", [128, 48])
        gamc = sbt("gamc", [128, 4, KC])
        S1 = sbt("S1", [128, KC]); S2 = sbt("S2", [128, KC])
        G1b = sbt("G1b", [128, D]); G2b = sbt("G2b", [128, D])
        binc = sbt("binc", [128, 44])
        binq = sbt("binq", [128, 8])
        bflag = sbt("bflag", [128, NST, KC])
        bks = sbt("bks", [128, 2])
        vb = sbt("vb", [128, 256])
        convc = sbt("convc", [128, KC, 5])
        rgbc = sbt("rgbc", [128, 2, KC])
        lamc = sbt("lamc", [128, KC])
        cL = sbt("cL", [128, KC]); cL2 = sbt("cL2", [128, KC]); spt = sbt("spt", [128, KC]); spe = sbt("spe", [128, KC])
        sinkb = sbt("sinkb", [128, 16])
        hstate = sbt("hstate", [128, KC])
        halo = sbt("halo", [128, KC, 4])
        Gt = sbt("Gt", [128, 16, NE])
        rbb = sbt("rbb", [128, NE])
        routw = sbt("routw", [128, KC, NE])
        b1c = sbt("b1c", [128, NE, 16])
        rcf = sbt("rcf", [128, 160])
        trib = sbt("trib", [128, 128], BF16)
        onesb = sbt("onesb", [128, 128], BF16)
        onesf = sbt("onesf", [128, 128])
        MB = sbt("MB", [128, 16, NE], BF16)
        IDX = sbt("IDX", [128, 16, 4], U32)
        GV = sbt("GV", [128, 16, 4])
        FLG = sbt("FLG", [128, NE], mybir.dt.int32)
        HHALO = sbt("HHALO", [128, KC, 128], BF16)
        stat = sbt("stat", [128, 256])
        statn = [0]

        def newstat(n=16):
            i = statn[0] % 16
            statn[0] += 1
            return i * 16, f"st{i}"

        O_HT, O_YR, O_QT = 0, 32768, 65536
        O_KT, O_V, O_RING, O_R = 98304, 115712, 124416, 140800
        O_SP = 174592
        HT = carve(O_HT, 32768, BF16, "p (k t) -> p k t", k=KC)
        YR = carve(O_YR, 32768, BF16, "p (k t) -> p k t", k=KC)
        QT = carve(O_QT, 32768, BF16, "p (k t) -> p k t", k=KC)
        KT = carve(O_KT, 17408, BF16, "p (k t) -> p k t", k=4)
        VV = carve(O_V, 8704, BF16, "p (n c) -> p n c", n=17)
        RING = [carve(O_RING + 2048 * i, 2048, BF16, "p (k m) -> p k m", k=KC) for i in range(8)]
        WRG = carve(O_SP, 4096, BF16, "p (a k m) -> p a k m", a=2, k=KC)
        XC16 = carve(O_SP + 4096, 4096, BF16)
        JUNK = carve(O_SP + 8192, 4096)
        B = [carve(O_QT + 8192 * i, 8192) for i in range(4)]
        XRB = carve(O_R, 8208)
        B.append(carve(O_R + 8208, 8192))
        XST = [carve(O_R + 16400 + 8192 * i, 8192, F32, "p (j d) -> p j d", j=2) for i in range(2)]

        grow = carve(O_QT, 8192)
        browt = carve(O_QT + 8192, 8192)
        gamr = carve(O_QT + 16384, 8192)

        ring_n = [0]

        def ring_load(src):
            i = ring_n[0] % 8
            ring_n[0] += 1
            S.op("gpsimd", lambda e, i=i, src=src: e.dma_start(out=RING[i], in_=src),
                 writes=[f"ring{i}"], dma=f"ring{i}")
            return RING[i], f"ring{i}"

        def small_load(dst, src, tok):
            S.op("sync", lambda e: e.dma_start(out=dst, in_=src), writes=[tok], dma=tok)

        small_load(flags[:], flags_h, "flags")
        small_load(ccs[:], ccol, "ccs")
        small_load(badac[:], bada_col, "badac")
        small_load(gamc[:], gam_col, "gamc")
        small_load(binc[:], b_in_col, "binc")
        small_load(bks[:], b_ks_col, "bks")
        small_load(vb[:], b_v_row.partition_broadcast(128), "vb")
        small_load(convc[:], conv_col, "convc")
        small_load(rgbc[:], rgb_col, "rgbc")
        small_load(lamc[:], lam_col, "lamc")
        small_load(sinkb[:], sinks_row.partition_broadcast(128), "sinkb")
        small_load(rbb[:], router_b.partition_broadcast(128), "rbb")
        small_load(routw[:], router_h, "routw")
        small_load(b1c[:], b1_col, "b1c")
        small_load(rcf[:], rc_h, "rcf")
        small_load(browt[0:1, 0:D], bada_row[2:3, :], "browt0")
        small_load(browt[0:1, D:2 * D], bada_row[5:6, :], "browt1")
        small_load(gamr[0:1, 0:D], gam_row[1:2, :], "gamr0")
        small_load(gamr[0:1, D:2 * D], gam_row[3:4, :], "gamr1")
        S.op("gpsimd", lambda e: e.dma_start(out=WRG, in_=rgw), writes=["wrg"], dma="wrg")

        S.op("gpsimd", lambda e: e.memset(ident[:], 0.0), writes=["ident"])
        S.op("gpsimd", lambda e: e.affine_select(out=ident[:], in_=ident[:], pattern=[[-1, 128]],
                                                  compare_op=ALU.not_equal, fill=1.0, base=0,
                                                  channel_multiplier=1), reads=["ident"], writes=["ident"])
        S.op("vector", lambda e: e.tensor_copy(out=identb[:], in_=ident[:]), reads=["ident"], writes=["identb"])
        S.op("vector", lambda e: e.memset(ones_r[:], 1.0), writes=["ones_r"])
        S.op("vector", lambda e: e.memset(onesb[:], 1.0), writes=["onesb"])
        S.op("vector", lambda e: e.memset(onesf[:], 1.0), writes=["onesf"])
        S.op("vector", lambda e: e.tensor_copy(out=trib[:], in_=rcf[:, 32:160]), reads=["rcf"], writes=["trib"])
        S.op("vector", lambda e: e.memset(hstate[:], 0.0), writes=["hstate"])
        S.op("vector", lambda e: e.memset(halo[:], 0.0), writes=["halo"])
        S.op("vector", lambda e: e.tensor_scalar(out=binq[:], in0=binc[:, CH_Q:CH_Q + 8], scalar1=0.125, scalar2=None,
                                                 op0=ALU.mult), reads=["binc"], writes=["binq"])
        for st_ in range(NST):
            S.op("vector", lambda e, st_=st_: e.tensor_scalar(out=bflag[:, st_, :], in0=binc[:, CH_XR:CH_XR + 8], scalar1=flags[:, st_:st_ + 1],
                                                              scalar2=None, op0=ALU.mult), reads=["binc", "flags"], writes=["bflag"])
        S.op("vector", lambda e: e.tensor_scalar(out=b1c[:, :, 8:16], in0=b1c[:, :, 8:16], scalar1=1.0, scalar2=None,
                                                 op0=ALU.add), reads=["b1c"], writes=["b1c"])
        S.op("scalar", lambda e: e.activation(out=spe[:], in_=lamc[:], func=AF.Exp, scale=-1.0), reads=["lamc"], writes=["spe"])
        S.op("vector", lambda e: e.tensor_scalar(out=spt[:], in0=spe[:], scalar1=-0.2, scalar2=0.25, op0=ALU.mult, op1=ALU.add),
             reads=["spe"], writes=["spt"])
        for cst in (1.0 / 3.0, 0.5, 1.0):
            S.op("vector", lambda e: e.tensor_tensor(out=spt[:], in0=spt[:], in1=spe[:], op=ALU.mult), reads=["spt", "spe"], writes=["spt"])
            S.op("vector", lambda e, cst=cst: e.tensor_scalar(out=spt[:], in0=spt[:], scalar1=-1.0, scalar2=cst, op0=ALU.mult, op1=ALU.add),
                 reads=["spt"], writes=["spt"])
        S.op("vector", lambda e: e.tensor_tensor(out=spt[:], in0=spt[:], in1=spe[:], op=ALU.mult), reads=["spt", "spe"], writes=["spt"])
        S.op("vector", lambda e: e.tensor_scalar(out=cL[:], in0=spt[:], scalar1=-8.0, scalar2=None, op0=ALU.mult), reads=["spt"], writes=["cL"])
        S.op("vector", lambda e: e.tensor_scalar(out=cL2[:], in0=spt[:], scalar1=-16.0, scalar2=None, op0=ALU.mult), reads=["spt"], writes=["cL2"])

        S.op("scalar", lambda e: e.activation(out=scs[:], in_=ccs[:], func=AF.Silu), reads=["ccs"], writes=["scs"])
        WA = [carve(O_YR + 8192 * i, 8192, BF16, "p (k n) -> p k n", k=KC) for i in range(3)]
        scs16 = sbt("scs16", [128, KC], BF16)
        S.op("vector", lambda e: e.tensor_copy(out=scs16[:], in_=scs[:]), reads=["scs"], writes=["scs16"])
        col_pieces = {0: 0, 1: 4, 2: 8, 3: 12, 6: 16, 7: 20, 8: 24, 9: 28}
        row_pieces = {4: 0, 5: 512, 10: 1024, 11: 1536}
        for pc in range(12):
            wa = WA[pc % 3]
            tk = f"wa{pc % 3}"
            for hk in range(2):
                S.op("gpsimd", lambda e, wa=wa, pc=pc, hk=hk: e.dma_start(out=wa[:, 4 * hk:4 * hk + 4, :], in_=wada[pc][:, 4 * hk:4 * hk + 4, :]),
                     writes=[tk], dma=tk)
            if pc in col_pieces:
                base = col_pieces[pc]
                for sub in range(4):
                    for k in range(KC):
                        S.op("tensor", lambda e, wa=wa, sub=sub, k=k: e.matmul(
                            PS[0][:, sub:sub + 1], lhsT=wa[:, k, sub * 128:(sub + 1) * 128], rhs=scs16[:, k:k + 1],
                            start=(k == 0), stop=(k == KC - 1)), reads=[tk, "scs16"], writes=["ps0"], sig=(k == KC - 1))
                S.op("vector", lambda e, base=base, pc=pc: e.tensor_tensor(
                    out=ada[:, base:base + 4], in0=PS[0][:, 0:4], in1=badac[:, pc * 4:pc * 4 + 4], op=ALU.add),
                    reads=["ps0", "badac"], writes=["ada"])
            else:
                ro = row_pieces[pc]
                for k in range(KC):
                    S.op("tensor", lambda e, wa=wa, k=k: e.matmul(
                        PS[1][0:1, :], lhsT=scs16[:, k:k + 1], rhs=wa[:, k, :], start=(k == 0), stop=(k == KC - 1)),
                        reads=[tk, "scs16"], writes=["ps1"], sig=(k == KC - 1))
                S.op("vector", lambda e, ro=ro: e.tensor_tensor(out=grow[0:1, ro:ro + 512], in0=PS[1][0:1, :],
                                                                in1=browt[0:1, ro:ro + 512], op=ALU.add),
                     reads=["ps1", "browt0", "browt1"], writes=["grow"])
                S.op("vector", lambda e, ro=ro: e.tensor_tensor(out=grow[0:1, ro:ro + 512], in0=grow[0:1, ro:ro + 512],
                                                                in1=gamr[0:1, ro:ro + 512], op=ALU.mult),
                     reads=["grow", "gamr0", "gamr1"], writes=["grow"])
                S.op("tensor", lambda e, ro=ro: e.matmul(PS[2][:, :], lhsT=ones_r[0:1, :], rhs=grow[0:1, ro:ro + 512],
                                                         start=True, stop=True), reads=["grow", "ones_r"], writes=["ps2"])
                dstb = G1b if ro < 1024 else G2b
                S.op("vector", lambda e, ro=ro, dstb=dstb: e.tensor_copy(out=dstb[:, (ro % 1024):(ro % 1024) + 512], in_=PS[2][:, :]),
                     reads=["ps2"], writes=["G1b" if ro < 1024 else "G2b"])
        S.op("vector", lambda e: e.scalar_tensor_tensor(out=S1[:], in0=ada[:, 8:16], scalar=1.0, in1=gamc[:, 0, :], op0=ALU.add, op1=ALU.mult),
             reads=["ada", "gamc"], writes=["S1"])
        S.op("vector", lambda e: e.scalar_tensor_tensor(out=S2[:], in0=ada[:, 24:32], scalar=1.0, in1=gamc[:, 2, :], op0=ALU.add, op1=ALU.mult),
             reads=["ada", "gamc"], writes=["S2"])
        S.alias("YR", ["wa0", "wa1", "wa2"])
        S.alias("ga0", ["grow"]); S.alias("ga1", ["grow"]); S.alias("gi0", ["browt0", "browt1"]); S.alias("gi1", ["browt0", "browt1"]); S.alias("ta0", ["gamr0", "gamr1"]); S.alias("ta1", ["gamr0", "gamr1"])
        if debug:
            S.op("sync", lambda e: e.dma_start(out=dbg["d_ada"], in_=ada[:]), reads=["ada"], dma="dbg")
            S.op("sync", lambda e: e.dma_start(out=dbg["d_g1b"][:, 0:D], in_=G1b[:]), reads=["G1b"], dma="dbg")
            S.op("sync", lambda e: e.dma_start(out=dbg["d_g1b"][:, D:2 * D], in_=G2b[:]), reads=["G2b"], dma="dbg")

        if stop == "p0":
            return finish()

        def norm_to_T(src_rows, row0, ntile2, scale_col, shift_col, dstT, dst_tok, col0, dst_f32=None):
            xs = XST[ntile2 % 2]
            tk = f"xst{ntile2 % 2}"
            S.op("sync", lambda e: e.dma_start(out=xs, in_=src_rows[row0:row0 + 256, :].rearrange("(j p) d -> p j d", p=128)),
                 writes=[tk], dma=tk)
            so, stk = newstat()
            for j in range(2):
                S.op("vector", lambda e, j=j: e.scalar_tensor_tensor(out=JUNK, in0=xs[:, j, :], scalar=1.0, in1=xs[:, j, :], op0=ALU.mult, op1=ALU.mult,
                                                                     accum_out=stat[:, so + j:so + j + 1]),
                     reads=[tk], writes=["junk", stk])
            S.op("scalar", lambda e: e.activation(out=stat[:, so + 2:so + 4], in_=stat[:, so:so + 2], func=AF.Sqrt, scale=1.0 / D, bias=EPS),
                 reads=[stk], writes=[stk])
            S.op("vector", lambda e: e.reciprocal(out=stat[:, so + 2:so + 4], in_=stat[:, so + 2:so + 4]), reads=[stk], writes=[stk])
            for j in range(2):
                S.op("vector", lambda e, j=j: e.tensor_scalar(out=xs[:, j, :], in0=xs[:, j, :], scalar1=stat[:, so + 2 + j:so + 3 + j],
                                                               scalar2=None, op0=ALU.mult),
                     reads=[tk, stk], writes=[tk])
            for half in range(4):
                pb = PS[4 + half]
                pt = f"ps{4 + half}"
                for kk in range(2):
                    k = half * 2 + kk
                    for j in range(2):
                        S.op("tensor", lambda e, k=k, j=j, kk=kk, pb=pb: e.transpose(
                            out=pb[:, (kk * 2 + j) * 128:(kk * 2 + j + 1) * 128], in_=xs[:, j, k * 128:(k + 1) * 128], identity=ident[:]),
                            reads=[tk, "ident"], writes=[pt], sig=(kk == 1 and j == 1))
                for kk in range(2):
                    k = half * 2 + kk
                    S.op("scalar", lambda e, k=k, kk=kk, pb=pb: e.activation(
                        out=dstT[:, k, col0:col0 + 256], in_=pb[:, kk * 256:(kk + 1) * 256], func=AF.Identity,
                        bias=shift_col[:, k:k + 1], scale=scale_col[:, k:k + 1]),
                        reads=[pt, "ada", "S1", "S2"], writes=[dst_tok])

        def proj_T(chunk_src, rhsT, rhs_tok, ncols, col0, evac):
            w, wt = ring_load(chunk_src)
            for i in range((ncols + 511) // 512):
                n = min(512, ncols - i * 512)
                pb = PS[i % 4]
                pt = f"ps{i % 4}"
                for k in range(KC):
                    S.op("tensor", lambda e, k=k, pb=pb, n=n, i=i: e.matmul(
                        pb[:, 0:n], lhsT=w[:, k, :], rhs=rhsT[:, k, col0 + i * 512:col0 + i * 512 + n],
                        start=(k == 0), stop=(k == KC - 1)), reads=[wt, rhs_tok], writes=[pt], sig=(k == KC - 1))
                evac(i, pb, pt, i * 512, n)

        XRBs = [XRB, carve(O_KT, 8208)]
        XCs = [B[4], carve(O_KT + 8208, 8192)]
        XC16s = [XC16, carve(O_KT + 16400, 4096, BF16)]
        GA, GI, TA_, TM = B[0], B[1], B[2], B[3]
        ntile2 = [0]

        def rnn_norm(st):
            for g2 in range(T // 256):
                norm_to_T(xe, st * T + g2 * 256, ntile2[0], S1, ada[:, 0:8], HT, "HT", g2 * 256)
                ntile2[0] += 1
            if st == NST - 2:
                S.op("gpsimd", lambda e: e.tensor_copy(out=HHALO[:], in_=HT[:, :, T - 128:T]), reads=["HT"], writes=["hhalo"])

        def rnn_A(st, c):
            sx = (st * KC + c) % 2
            xrb, xc, xc16 = XRBs[sx], XCs[sx], XC16s[sx]
            xrbt, xct, xc16t = f"xrb{sx}", f"xc{sx}", f"xc16{sx}"
            S.op("vector", lambda e: e.tensor_copy(out=xrb[:, 0:3], in_=halo[:, c, 0:3]), reads=["halo"], writes=[xrbt])

            def ev_xr(i, pb, pt, c0, n):
                if i % 2 == 0:
                    S.op("scalar", lambda e: e.activation(out=xrb[:, 3 + c0:3 + c0 + n], in_=pb[:, 0:n], func=AF.Identity,
                                                          bias=bflag[:, st, c:c + 1], scale=flags[:, st:st + 1]),
                         reads=[pt, "bflag", "flags"], writes=[xrbt])
                else:
                    S.op("vector", lambda e: e.tensor_scalar(out=xrb[:, 3 + c0:3 + c0 + n], in0=pb[:, 0:n], scalar1=binc[:, c:c + 1],
                                                             scalar2=flags[:, st:st + 1], op0=ALU.add, op1=ALU.mult),
                         reads=[pt, "binc", "flags"], writes=[xrbt])
            proj_T(w_in_h[CH_XR + c], HT, "HT", T, 0, ev_xr)
            S.op("vector", lambda e: e.tensor_copy(out=halo[:, c, 0:3], in_=xrb[:, T:T + 3]), reads=[xrbt], writes=["halo"])
            S.op("vector", lambda e: e.tensor_scalar(out=xc, in0=xrb[:, 0:T], scalar1=convc[:, c, 0:1], scalar2=convc[:, c, 4:5],
                                                     op0=ALU.mult, op1=ALU.add), reads=[xrbt, "convc"], writes=[xct])
            for kk in range(1, 4):
                S.op("vector", lambda e, kk=kk: e.scalar_tensor_tensor(out=xc, in0=xrb[:, kk:kk + T], scalar=convc[:, c, kk:kk + 1],
                                                                       in1=xc, op0=ALU.mult, op1=ALU.add),
                     reads=[xrbt, "convc", xct], writes=[xct])
            S.op("vector", lambda e: e.tensor_copy(out=xc16, in_=xc), reads=[xct], writes=[xc16t])

        def rnn_B(st, c):
            own = (st == NST - 1)
            sx = (st * KC + c) % 2
            xc, xc16 = XCs[sx], XC16s[sx]
            xct, xc16t = f"xc{sx}", f"xc16{sx}"
            HH = xc
            TH = T // 2
            for h in range(2):
                cs = slice(h * TH, (h + 1) * TH)
                gat, git, tat, tmt = f"ga{h}", f"gi{h}", f"ta{h}", f"tm{h}"
                for i2 in range(2):
                    i = 2 * h + i2
                    for a in range(2):
                        pb = PS[4 + 2 * i2 + a]
                        pt = f"ps{4 + 2 * i2 + a}"
                        S.op("tensor", lambda e, a=a, i=i, pb=pb: e.matmul(pb[:, :], lhsT=WRG[:, a, c, :], rhs=xc16[:, i * 512:(i + 1) * 512],
                                                                           start=True, stop=True), reads=["wrg", xc16t], writes=[pt])
                        dst = GA if a == 0 else GI
                        S.op("scalar", lambda e, a=a, i=i, pb=pb, dst=dst: e.activation(
                            out=dst[:, i * 512:(i + 1) * 512], in_=pb[:, :], func=AF.Sigmoid, bias=rgbc[:, a, c:c + 1], scale=1.0),
                            reads=[pt, "rgbc"], writes=[gat if a == 0 else git])
                S.op("scalar", lambda e, cs=cs: e.activation(out=TA_[:, cs], in_=GA[:, cs], func=AF.Exp, scale=cL[:, c:c + 1]), reads=[gat, "cL"], writes=[tat])
                S.op("scalar", lambda e, cs=cs: e.activation(out=TM[:, cs], in_=GA[:, cs], func=AF.Exp, scale=cL2[:, c:c + 1]), reads=[gat, "cL2"], writes=[tmt])
                S.op("vector", lambda e, cs=cs: e.tensor_scalar(out=TM[:, cs], in0=TM[:, cs], scalar1=1.0, scalar2=-1.0, op0=ALU.min, op1=ALU.mult),
                     reads=[tmt], writes=[tmt])
                S.op("scalar", lambda e, cs=cs: e.activation(out=TM[:, cs], in_=TM[:, cs], func=AF.Sqrt, scale=1.0, bias=1.0), reads=[tmt], writes=[tmt])
                if h == 0:
                    S.op("vector", lambda e: e.tensor_tensor(out=TM[:, 0:1], in0=TM[:, 0:1], in1=flags[:, 4 + st:5 + st], op=ALU.max),
                         reads=[tmt, "flags"], writes=[tmt])
                S.op("vector", lambda e, cs=cs: e.scalar_tensor_tensor(out=GI[:, cs], in0=xc[:, cs], scalar=flags[:, st:st + 1], in1=GI[:, cs],
                                                                      op0=ALU.mult, op1=ALU.mult), reads=[xct, git, "flags"], writes=[git])
                S.op("vector", lambda e, cs=cs: e.tensor_tensor(out=GI[:, cs], in0=GI[:, cs], in1=TM[:, cs], op=ALU.mult), reads=[git, tmt], writes=[git])
                init = hstate[:, c:c + 1] if h == 0 else HH[:, TH - 1:TH]
                S.op("vector", lambda e, cs=cs, init=init: e.tensor_tensor_scan(out=HH[:, cs], data0=TA_[:, cs], data1=GI[:, cs], initial=init,
                                                                                op0=ALU.mult, op1=ALU.add),
                     reads=[tat, git, "hstate", xct], writes=[xct])
            S.op("vector", lambda e: e.tensor_copy(out=hstate[:, c:c + 1], in_=HH[:, T - 1:T]), reads=[xct], writes=["hstate"])
            if own:
                XG, X2 = B[0], B[1]
                gaA, giA = ["ga0", "ga1"], ["gi0", "gi1"]

                def ev_gr(i, pb, pt, c0, n):
                    S.op("scalar", lambda e: e.activation(out=XG[:, c0:c0 + n], in_=pb[:, 0:n], func=AF.Identity,
                                                          bias=binc[:, CH_GR + c:CH_GR + c + 1], scale=1.0),
                         reads=[pt, "binc"], writes=gaA)
                proj_T(w_in_h[CH_GR + c], HT, "HT", T, 0, ev_gr)
                S.op("vector", lambda e: e.tensor_tensor(out=X2, in0=XG, in1=XG, op=ALU.mult), reads=gaA, writes=giA)
                S.op("vector", lambda e: e.tensor_scalar(out=X2, in0=X2, scalar1=0.044715, scalar2=1.0, op0=ALU.mult, op1=ALU.add),
                     reads=giA, writes=giA)
                S.op("vector", lambda e: e.tensor_tensor(out=X2, in0=X2, in1=XG, op=ALU.mult), reads=giA + gaA, writes=giA)
                S.op("scalar", lambda e: e.activation(out=X2, in_=X2, func=AF.Sigmoid, scale=1.5957691216057308), reads=giA, writes=giA)
                S.op("vector", lambda e: e.tensor_tensor(out=X2, in0=X2, in1=XG, op=ALU.mult), reads=giA + gaA, writes=giA)
                S.op("vector", lambda e: e.tensor_tensor(out=YR[:, c, :], in0=HH, in1=X2, op=ALU.mult), reads=[xct] + giA, writes=["YR"])

        seq = [(st, c) for st in range(NST) for c in range(KC)]
        rnn_norm(0)
        rnn_A(*seq[0])
        for n in range(len(seq)):
            if n + 1 < len(seq):
                st1, c1 = seq[n + 1]
                if c1 == 0:
                    rnn_norm(st1)
                rnn_A(st1, c1)
            rnn_B(*seq[n])
        if debug:
            S.op("sync", lambda e: e.dma_start(out=dbg["d_yr"], in_=YR.rearrange("p k t -> p (k t)")), reads=["YR"], dma="dbg")

        if stop == "p1":
            return finish()

        S.alias("QT", ["ga0", "ga1", "gi0", "gi1", "ta0", "ta1", "tm0", "tm1", "xc0"])
        S.alias("KT", ["xrb1", "xc1", "xc161"]); S.alias("VV", ["xrb1", "xc1", "xc161"])
        for kc in range(4):
            def ev_kh(i, pb, pt, c0, n, kc=kc):
                bcol = binc[:, CH_K + kc:CH_K + kc + 1] if kc < 2 else bks[:, kc - 2:kc - 1]
                S.op("scalar", lambda e: e.activation(out=KT[:, kc, 0:128], in_=pb[:, 0:128], func=AF.Identity, bias=bcol, scale=1.0),
                     reads=[pt, "binc", "bks"], writes=["KT"])
            proj_T(w_in_h[CH_K + kc] if kc < 2 else w_ks_h[kc - 2], HHALO, "hhalo", 128, 0, ev_kh)
        for vc in range(2):
            w_, wt_ = ring_load(w_in_h[CH_V + vc])
            for k in range(KC):
                S.op("tensor", lambda e, k=k, w_=w_, vc=vc: e.matmul(PS[0][:, vc * 128:(vc + 1) * 128], lhsT=HHALO[:, k, :],
                                                                   rhs=w_[:, k, :], start=(k == 0), stop=(k == KC - 1)),
                     reads=[wt_, "hhalo"], writes=["ps0"], sig=(k == KC - 1))
        S.op("vector", lambda e: e.tensor_tensor(out=VV[:, 0, :], in0=PS[0][:, 0:256], in1=vb[:], op=ALU.add),
             reads=["ps0", "vb"], writes=["VV"])
        for qc in range(8):
            def ev_q(i, pb, pt, c0, n, qc=qc):
                S.op("scalar", lambda e: e.activation(out=QT[:, qc, c0:c0 + n], in_=pb[:, 0:n], func=AF.Identity,
                                                      bias=binq[:, qc:qc + 1], scale=0.125), reads=[pt, "binq"], writes=["QT"])
            proj_T(w_in_h[CH_Q + qc], HT, "HT", T, 0, ev_q)
        for kc in range(4):
            def ev_k(i, pb, pt, c0, n, kc=kc):
                bcol = binc[:, CH_K + kc:CH_K + kc + 1] if kc < 2 else bks[:, kc - 2:kc - 1]
                S.op("scalar", lambda e: e.activation(out=KT[:, kc, 128 + c0:128 + c0 + n], in_=pb[:, 0:n], func=AF.Identity, bias=bcol, scale=1.0),
                     reads=[pt, "binc", "bks"], writes=["KT"])
            proj_T(w_in_h[CH_K + kc] if kc < 2 else w_ks_h[kc - 2], HT, "HT", T, 0, ev_k)
        wv = [ring_load(w_in_h[CH_V + vc]) for vc in range(2)]
        for tt in range(16):
            pb = PS[tt % 4]; pt = f"ps{tt % 4}"
            for vc in range(2):
                for k in range(KC):
                    S.op("tensor", lambda e, k=k, vc=vc, tt=tt, pb=pb: e.matmul(pb[:, vc * 128:(vc + 1) * 128], lhsT=HT[:, k, tt * 128:(tt + 1) * 128],
                                                                             rhs=wv[vc][0][:, k, :], start=(k == 0), stop=(k == KC - 1)),
                         reads=[wv[vc][1], "HT"], writes=[pt], sig=(k == KC - 1))
            S.op("vector", lambda e, tt=tt, pb=pb: e.tensor_tensor(out=VV[:, 1 + tt, :], in0=pb[:, 0:256], in1=vb[:], op=ALU.add),
                 reads=[pt, "vb"], writes=["VV"])

        if stop == "p2":
            return finish()

        S.alias("attR", ["xrb0", "xc0", "xst0", "xst1"])
        SS = [carve(O_R + 4096 * i, 4096, F32, "p (h c) -> p h c", h=4) for i in range(2)]
        PN = [carve(O_R + 8192 + 2048 * i, 2048, BF16, "p (h c) -> p h c", h=4) for i in range(2)]
        PTB = [carve(O_R + 12288 + 2048 * i, 2048, BF16, "p (b c) -> p b c", b=2) for i in range(2)]
        ABI = carve(O_R + 16384, 16384, F32, "p (h c) -> p h c", h=16)
        S.op("sync", lambda e: e.dma_start(out=ABI, in_=abias_h), writes=["attR"], dma="abi")
        S.alias("abi", ["attR"]); S.alias("ss0", ["attR"]); S.alias("ss1", ["attR"]); S.alias("pn0", ["attR"]); S.alias("pn1", ["attR"])
        S.alias("ptb0", ["attR"]); S.alias("ptb1", ["attR"])
        def att_stage1(it, qb, g):
            par = it % 2
            ss, pn = SS[par], PN[par]
            sst, pnt = f"ss{par}", f"pn{par}"
            pS = (PS[0], PS[1]) if par == 0 else (PS[2], PS[3])
            pSt = ("ps0", "ps1") if par == 0 else ("ps2", "ps3")
            for hh in (0, 2, 1, 3):
                h = 4 * g + hh
                ch, hp = h // 2, h % 2
                po = 64 * hp
                kch = (g // 2) if (g % 2) == hp else 2 + (g // 2)
                pb = pS[hp]
                S.op("tensor", lambda e, ch=ch, po=po, qb=qb, kch=kch, hh=hh, pb=pb: e.matmul(
                    pb[:, (hh // 2) * 256:(hh // 2) * 256 + 256], lhsT=QT[po:po + 64, ch, qb * 128:(qb + 1) * 128],
                    rhs=KT[po:po + 64, kch, qb * 128:qb * 128 + 256], start=True, stop=True),
                    reads=["QT", "KT"], writes=[pSt[hp]], sig=(hh // 2 == 1))
            for half in range(2):
                S.op("vector", lambda e, half=half, g=g, ss=ss, pS=pS: e.tensor_tensor(
                    out=ss[:, 2 * half:2 * half + 2, :], in0=pS[half][:, :].rearrange("p (h c) -> p h c", h=2),
                    in1=ABI[:, 4 * g + half:4 * g + half + 3:2, :], op=ALU.add),
                    reads=[pSt[half], "abi"], writes=[sst])
            if qb == 0:
                S.op("vector", lambda e, ss=ss: e.tensor_scalar(out=ss[:, :, 0:128], in0=ss[:, :, 0:128], scalar1=flags[:, 8:9], scalar2=None,
                                                                op0=ALU.add), reads=[sst, "flags"], writes=[sst])
            so, stk = newstat()
            mx, nmx, rs, es_ = stat[:, so:so + 4], stat[:, so + 4:so + 8], stat[:, so + 8:so + 12], stat[:, so + 12:so + 16]
            S.op("vector", lambda e, ss=ss, mx=mx: e.tensor_reduce(out=mx, in_=ss, axis=AX.X, op=ALU.max), reads=[sst], writes=[stk])
            S.op("vector", lambda e, mx=mx, g=g: e.tensor_tensor(out=mx, in0=mx, in1=sinkb[:, 4 * g:4 * g + 4], op=ALU.max),
                 reads=[stk, "sinkb"], writes=[stk])
            S.op("vector", lambda e, mx=mx, nmx=nmx: e.tensor_scalar(out=nmx, in0=mx, scalar1=-1.0, scalar2=None, op0=ALU.mult),
                 reads=[stk], writes=[stk])
            att_ctx[it] = (so, stk)

        def att_stage1b(it, qb, g):
            par = it % 2
            ss, pn = SS[par], PN[par]
            sst, pnt = f"ss{par}", f"pn{par}"
            so, stk = att_ctx.pop(it)
            mx, nmx, rs, es_ = stat[:, so:so + 4], stat[:, so + 4:so + 8], stat[:, so + 8:so + 12], stat[:, so + 12:so + 16]
            for hh in range(4):
                S.op("scalar", lambda e, hh=hh, ss=ss, nmx=nmx, rs=rs: e.activation(
                    out=ss[:, hh, :], in_=ss[:, hh, :], func=AF.Exp, bias=nmx[:, hh:hh + 1], scale=1.0, accum_out=rs[:, hh:hh + 1]),
                    reads=[sst, stk], writes=[sst, stk])
            S.op("vector", lambda e, mx=mx, g=g, es_=es_: e.tensor_tensor(out=es_, in0=sinkb[:, 4 * g:4 * g + 4], in1=mx, op=ALU.subtract),
                 reads=[stk, "sinkb"], writes=[stk])
            S.op("scalar", lambda e, es_=es_: e.activation(out=es_, in_=es_, func=AF.Exp), reads=[stk], writes=[stk])
            S.op("vector", lambda e, rs=rs, es_=es_: e.tensor_tensor(out=rs, in0=rs, in1=es_, op=ALU.add), reads=[stk], writes=[stk])
            S.op("vector", lambda e, rs=rs: e.reciprocal(out=rs, in_=rs), reads=[stk], writes=[stk])
            for pos in range(4):
                S.op("vector", lambda e, pos=pos, ss=ss, pn=pn, rs=rs: e.tensor_scalar(
                    out=pn[:, pos, :], in0=ss[:, pos, :], scalar1=rs[:, pos:pos + 1], scalar2=None, op0=ALU.mult),
                    reads=[sst, stk], writes=[pnt])

        def att_stage2(it, qb, g):
            par = it % 2
            pn, ptb = PN[par], PTB[par]
            pnt, ptbt = f"pn{par}", f"ptb{par}"
            pT = PS[4 + par]
            pTt = f"ps{4 + par}"
            pTb = pT[:, 0:512].bitcast(BF16).rearrange("p (b c) -> p b c", b=2)
            for pos in range(4):
                for kb in range(2):
                    S.op("tensor", lambda e, pos=pos, kb=kb, pn=pn, pTb=pTb: e.transpose(
                        out=pTb[:, kb, pos * 128:(pos + 1) * 128], in_=pn[:, pos, kb * 128:(kb + 1) * 128], identity=identb[:]),
                        reads=[pnt, "identb"], writes=[pTt], sig=(pos == 3 and kb == 1))
            S.op("scalar", lambda e, ptb=ptb, pTb=pTb: e.copy(out=ptb, in_=pTb), reads=[pTt], writes=[ptbt])
            pO = PS[6 + par]
            pOt = f"ps{6 + par}"
            for eo in range(2):
                for kb in range(2):
                    S.op("tensor", lambda e, eo=eo, kb=kb, qb=qb, g=g, ptb=ptb, pO=pO: e.matmul(
                        pO[64 * eo:64 * eo + 64, 0:256], lhsT=VV[:, qb + kb, 64 * g:64 * g + 64], rhs=ptb[:, kb, 256 * eo:256 * eo + 256],
                        start=(kb == 0), stop=(kb == 1)), reads=["VV", ptbt], writes=[pOt], sig=(eo == 1 and kb == 1))
            S.op("vector", lambda e, qb=qb, g=g, pO=pO: e.tensor_copy(
                out=QT[:, 2 * g:2 * g + 2, qb * 128:(qb + 1) * 128], in_=pO[:, 0:256].rearrange("p (c t) -> p c t", c=2)),
                reads=[pOt], writes=["yaW"])

        att_ctx = {}
        iters = [(qb, g) for qb in range(16) for g in range(4)]
        for it in range(len(iters) + 2):
            if it < len(iters):
                att_stage1(it, *iters[it])
            if 1 <= it <= len(iters):
                att_stage1b(it - 1, *iters[it - 1])
            if it >= 2:
                att_stage2(it - 2, *iters[it - 2])
        S.alias("QTy", ["QT", "yaW"])
        if debug:
            S.op("sync", lambda e: e.dma_start(out=dbg["d_ya"], in_=QT.rearrange("p k t -> p (k t)")), reads=["QTy"], dma="dbg")

        if stop == "p3":
            return finish()

        S.alias("p4R", ["abi", "ss0", "ss1", "pn0", "pn1", "ptb0", "ptb1"])
        S.alias("p4K", ["KT", "VV"])
        MG = [carve(O_R + 8192 * i, 8192, BF16, "p (k t) -> p k t", k=KC) for i in range(2)]
        XT4 = carve(O_R + 16384, 4096)
        MIXT = carve(O_R + 20480, 4096)
        H2F = carve(O_R + 24576, 4096, F32, "p (k t) -> p k t", k=KC)
        SG = [carve(O_R + 28672 + 2048 * i, 2048) for i in range(2)]
        WOUT = carve(O_KT, 16384, BF16, "p (k n) -> p k n", k=KC)
        for q4 in range(4):
            S.op("gpsimd", lambda e, q4=q4: e.dma_start(out=WOUT[:, 2 * q4:2 * q4 + 2, :], in_=w_out_h[:, 2 * q4:2 * q4 + 2, :]),
                 writes=["p4K"], dma="wout")
        S.alias("wout", ["p4K"])
        for nm in ("mg0", "mg1", "xt4", "mixt", "h2f", "sg0", "sg1"):
            S.alias(nm, ["p4R"])
        S2B = carve(O_V, 4096)
        SH2B = carve(O_V + 4096, 4096)
        H2TOK = [carve(O_SP + 2048 * i, 2048, BF16) for i in range(2)]
        S.alias("h2t0", ["wrg"]); S.alias("h2t1", ["wrg"])
        S.alias("s2b", ["KT", "VV"])
        for which, dstb in ((0, S2B), (1, SH2B)):
            for k in range(KC):
                src = S2[:, k:k + 1] if which == 0 else ada[:, 16 + k:17 + k]
                S.op("vector", lambda e, src=src: e.tensor_scalar(out=SG[0][:, 0:128], in0=ident[:], scalar1=src, scalar2=None, op0=ALU.mult),
                     reads=["ident", "S2", "ada", "sg0"], writes=["sg0"])
                S.op("tensor", lambda e: e.matmul(PS[5][:, 0:128], lhsT=onesf[:], rhs=SG[0][:, 0:128], start=True, stop=True),
                     reads=["sg0", "onesf"], writes=["ps5"])
                S.op("vector", lambda e, k=k, dstb=dstb: e.tensor_copy(out=dstb[:, k * 128:(k + 1) * 128], in_=PS[5][:, 0:128]),
                     reads=["ps5"], writes=["s2b"])
        def p4_merge(tt):
            mg = MG[tt % 2]; mgt = f"mg{tt % 2}"
            c0 = tt * 512
            for f in range(KC):
                wr, wrt = ring_load(w_in_h[CH_GATR + f])
                wa_, wat = ring_load(w_in_h[CH_GATA + f])
                wor, wort = ring_load(w_or_h[f])
                woa, woat = ring_load(w_oa_h[f])
                specs = ((wr, wrt, HT, "HT"), (wa_, wat, HT, "HT"), (wor, wort, YR, "YR"), (woa, woat, QT, "QTy"))
                for j, (w, wt, rT, rtok) in enumerate(specs):
                    for k in range(KC):
                        S.op("tensor", lambda e, j=j, k=k, w=w, rT=rT, c0=c0: e.matmul(PS[j][:, :], lhsT=w[:, k, :], rhs=rT[:, k, c0:c0 + 512],
                                                                            start=(k == 0), stop=(k == KC - 1)),
                             reads=[wt, rtok], writes=[f"ps{j}"], sig=(k == KC - 1))
                for j in range(2):
                    bci = (CH_GATR if j == 0 else CH_GATA) + f
                    S.op("scalar", lambda e, j=j, bci=bci: e.activation(out=SG[j], in_=PS[j][:, :], func=AF.Sigmoid, bias=binc[:, bci:bci + 1], scale=1.0),
                         reads=[f"ps{j}", "binc"], writes=[f"sg{j}"])
                S.op("vector", lambda e: e.tensor_tensor(out=SG[0], in0=SG[0], in1=PS[2][:, :], op=ALU.mult), reads=["sg0", "ps2"], writes=["sg0"])
                S.op("vector", lambda e: e.tensor_tensor(out=SG[1], in0=SG[1], in1=PS[3][:, :], op=ALU.mult), reads=["sg1", "ps3"], writes=["sg1"])
                S.op("vector", lambda e, f=f, mg=mg: e.tensor_tensor(out=mg[:, f, :], in0=SG[0], in1=SG[1], op=ALU.add),
                     reads=["sg0", "sg1"], writes=[mgt])
        XT4s = [XT4, carve(O_SP + 4096, 4096)]
        S.alias("xt40", ["xt4"]); S.alias("xt41", ["xc160"])

        def p4_main(tile):
            tt, t4 = tile // 4, tile % 4
            mg = MG[tt % 2]; mgt = f"mg{tt % 2}"
            XT4 = XT4s[tile % 2]; xt4t = f"xt4{tile % 2}"
            if True:
                r0 = tile * 128
                S.op("sync", lambda e, r0=r0: e.dma_start(out=XT4, in_=xe[(NST - 1) * T + r0:(NST - 1) * T + r0 + 128, :]), writes=[xt4t], dma=xt4t)
                for hf in range(2):
                    for k in range(KC):
                        S.op("tensor", lambda e, hf=hf, k=k, t4=t4, mg=mg: e.matmul(PS[4 + hf][:, :], lhsT=mg[:, k, t4 * 128:(t4 + 1) * 128],
                                                                                rhs=WOUT[:, k, hf * 512:(hf + 1) * 512], start=(k == 0), stop=(k == KC - 1)),
                             reads=[mgt, "wout"], writes=[f"ps{4 + hf}"], sig=(k == KC - 1))
                so, stk = newstat()
                for hf in range(2):
                    S.op("scalar", lambda e, hf=hf, so=so: e.activation(out=JUNK[:, 0:512], in_=PS[4 + hf][:, :], func=AF.Square,
                                                                        accum_out=stat[:, so + hf:so + hf + 1]), reads=[f"ps{4 + hf}"], writes=["junk", stk])
                S.op("vector", lambda e, so=so: e.tensor_tensor(out=stat[:, so + 2:so + 3], in0=stat[:, so:so + 1], in1=stat[:, so + 1:so + 2], op=ALU.add),
                     reads=[stk], writes=[stk])
                S.op("scalar", lambda e, so=so: e.activation(out=stat[:, so + 3:so + 4], in_=stat[:, so + 2:so + 3], func=AF.Sqrt, scale=1.0 / D, bias=EPS),
                     reads=[stk], writes=[stk])
                S.op("vector", lambda e, so=so: e.reciprocal(out=stat[:, so + 3:so + 4], in_=stat[:, so + 3:so + 4]), reads=[stk], writes=[stk])
                for hf in range(2):
                    S.op("vector", lambda e, hf=hf, so=so: e.scalar_tensor_tensor(out=MIXT[:, hf * 512:(hf + 1) * 512], in0=PS[4 + hf][:, :],
                                                                                  scalar=stat[:, so + 3:so + 4], in1=G1b[:, hf * 512:(hf + 1) * 512],
                                                                                  op0=ALU.mult, op1=ALU.mult),
                         reads=[f"ps{4 + hf}", stk, "G1b"], writes=["mixt"])
                S.op("vector", lambda e: e.tensor_tensor(out=MIXT, in0=MIXT, in1=XT4, op=ALU.add), reads=["mixt", xt4t], writes=["mixt"])
                S.op("sync", lambda e, r0=r0: e.dma_start(out=x1s[r0:r0 + 128, :], in_=MIXT), reads=["mixt"], writes=["x1sd"], dma="x1s")
                if debug:
                    S.op("sync", lambda e, r0=r0: e.dma_start(out=dbg["d_x1"][r0:r0 + 128, :], in_=MIXT), reads=["mixt"], dma="dbg")
                S.op("vector", lambda e, so=so: e.scalar_tensor_tensor(out=JUNK, in0=MIXT, scalar=1.0, in1=MIXT, op0=ALU.mult, op1=ALU.mult,
                                                                       accum_out=stat[:, so + 4:so + 5]),
                     reads=["mixt"], writes=["junk", stk])
                S.op("scalar", lambda e, so=so: e.activation(out=stat[:, so + 5:so + 6], in_=stat[:, so + 4:so + 5], func=AF.Sqrt, scale=1.0 / D, bias=EPS),
                     reads=[stk], writes=[stk])
                S.op("vector", lambda e, so=so: e.reciprocal(out=stat[:, so + 5:so + 6], in_=stat[:, so + 5:so + 6]), reads=[stk], writes=[stk])
                S.op("vector", lambda e, so=so: e.tensor_scalar(out=XT4, in0=MIXT, scalar1=stat[:, so + 5:so + 6], scalar2=None, op0=ALU.mult),
                     reads=["mixt", stk, xt4t], writes=[xt4t])
                for hf in range(2):
                    for kk in range(4):
                        k = hf * 4 + kk
                        S.op("tensor", lambda e, k=k, kk=kk, hf=hf: e.transpose(out=PS[6 + hf][:, kk * 128:(kk + 1) * 128], in_=XT4[:, k * 128:(k + 1) * 128],
                                                                                identity=ident[:]), reads=[xt4t, "ident"], writes=[f"ps{6 + hf}"], sig=(kk == 3))
                    for kk in range(4):
                        k = hf * 4 + kk
                        S.op("vector", lambda e, k=k, kk=kk, hf=hf: e.tensor_scalar(out=H2F[:, k, :], in0=PS[6 + hf][:, kk * 128:(kk + 1) * 128],
                                                                                   scalar1=S2[:, k:k + 1], scalar2=ada[:, 16 + k:17 + k],
                                                                                   op0=ALU.mult, op1=ALU.add),
                             reads=[f"ps{6 + hf}", "S2", "ada"], writes=["h2f"])
                h2t = H2TOK[tile % 2]; h2tt = f"h2t{tile % 2}"
                S.op("vector", lambda e: e.tensor_tensor(out=JUNK, in0=XT4, in1=S2B, op=ALU.mult), reads=[xt4t, "s2b", "junk"], writes=["junk"])
                S.op("vector", lambda e, h2t=h2t: e.tensor_tensor(out=h2t, in0=JUNK, in1=SH2B, op=ALU.add), reads=["junk", "s2b"], writes=[h2tt])
                for k in range(KC):
                    S.op("tensor", lambda e, k=k: e.matmul(PS[7][:, 0:NE], lhsT=H2F[:, k, :], rhs=routw[:, k, :], start=(k == 0), stop=(k == KC - 1)),
                         reads=["h2f", "routw"], writes=["ps7"], sig=(k == KC - 1))
                go, gtk = newstat()
                lg = Gt[:, tile, :]
                S.op("vector", lambda e, lg=lg: e.tensor_tensor(out=lg, in0=PS[7][:, 0:NE], in1=rbb[:], op=ALU.add), reads=["ps7", "rbb"], writes=["Gt"])
                S.op("vector", lambda e, lg=lg, go=go: e.max(out=stat[:, go:go + 8], in_=lg), reads=["Gt"], writes=[gtk])
                S.op("vector", lambda e, go=go: e.tensor_scalar(out=stat[:, go + 8:go + 9], in0=stat[:, go:go + 1], scalar1=-1.0, scalar2=None, op0=ALU.mult),
                     reads=[gtk], writes=[gtk])
                S.op("vector", lambda e, lg=lg, go=go: e.tensor_scalar(out=SG[0][:, 0:NE], in0=lg, scalar1=stat[:, go + 3:go + 4], scalar2=None, op0=ALU.is_ge),
                     reads=["Gt", gtk], writes=["sg0"])
                S.op("scalar", lambda e, lg=lg, go=go: e.activation(out=lg, in_=lg, func=AF.Exp, bias=stat[:, go + 8:go + 9], scale=1.0),
                     reads=["Gt", gtk], writes=["Gt"])
                S.op("vector", lambda e, lg=lg: e.tensor_tensor(out=lg, in0=lg, in1=SG[0][:, 0:NE], op=ALU.mult), reads=["Gt", "sg0"], writes=["Gt"])
                S.op("vector", lambda e, lg=lg, go=go: e.tensor_reduce(out=stat[:, go + 9:go + 10], in_=lg, axis=AX.X, op=ALU.add), reads=["Gt"], writes=[gtk])
                S.op("vector", lambda e, go=go: e.reciprocal(out=stat[:, go + 9:go + 10], in_=stat[:, go + 9:go + 10]), reads=[gtk], writes=[gtk])
                S.op("vector", lambda e, lg=lg, go=go: e.tensor_scalar(out=lg, in0=lg, scalar1=stat[:, go + 9:go + 10], scalar2=None, op0=ALU.mult),
                     reads=["Gt", gtk], writes=["Gt"])

        def p4_route(tile):
            lg = Gt[:, tile, :]
            h2t = H2TOK[tile % 2]; h2tt = f"h2t{tile % 2}"
            S.op("vector", lambda e, lg=lg, tile=tile: e.tensor_scalar(out=MB[:, tile, :], in0=lg, scalar1=0.0, scalar2=None, op0=ALU.is_gt),
                 reads=["Gt"], writes=["MB"])
            for ip in range(tile):
                S.op("tensor", lambda e, ip=ip: e.matmul(PS[7][:, 32:64], lhsT=onesb[:], rhs=MB[:, ip, :], start=(ip == 0), stop=False),
                     reads=["MB", "onesb"], writes=["ps7"], sig=False)
            S.op("tensor", lambda e, tile=tile: e.matmul(PS[7][:, 32:64], lhsT=trib[:], rhs=MB[:, tile, :], start=(tile == 0), stop=True),
                 reads=["MB", "trib"], writes=["ps7"])
            ro, rtk = newstat()
            SLF, SEL = SG[1][:, 0:NE], SG[1][:, 64:64 + NE]
            S.op("vector", lambda e: e.tensor_scalar(out=SEL, in0=PS[7][:, 32:64], scalar1=CAP - 0.5, scalar2=1.0e9, op0=ALU.is_ge, op1=ALU.mult),
                 reads=["ps7", "sg1"], writes=["sg1"])
            S.op("vector", lambda e: e.tensor_tensor(out=SLF, in0=PS[7][:, 32:64], in1=rcf[:, 0:NE], op=ALU.add), reads=["ps7", "rcf", "sg1"], writes=["sg1"])
            S.op("vector", lambda e: e.tensor_tensor(out=SLF, in0=SLF, in1=SEL, op=ALU.add), reads=["sg1"], writes=["sg1"])
            S.op("vector", lambda e, lg=lg, ro=ro: e.max(out=stat[:, ro:ro + 8], in_=lg), reads=["Gt"], writes=[rtk])
            for kk in range(4):
                S.op("vector", lambda e, lg=lg, ro=ro, kk=kk: e.tensor_scalar(out=SEL, in0=lg, scalar1=stat[:, ro + kk:ro + kk + 1], scalar2=None, op0=ALU.is_equal),
                     reads=["Gt", rtk, "sg1"], writes=["sg1"])
                S.op("vector", lambda e: e.tensor_tensor(out=SEL, in0=SEL, in1=SLF, op=ALU.mult), reads=["sg1"], writes=["sg1"])
                S.op("vector", lambda e, ro=ro, kk=kk: e.tensor_reduce(out=stat[:, ro + 8 + kk:ro + 9 + kk], in_=SEL, axis=AX.X, op=ALU.add),
                     reads=["sg1"], writes=[rtk])
            S.op("vector", lambda e, ro=ro, tile=tile: e.tensor_copy(out=IDX[:, tile, :], in_=stat[:, ro + 8:ro + 12]), reads=[rtk], writes=["IDX"])
            S.op("vector", lambda e, ro=ro: e.tensor_scalar(out=stat[:, ro + 12:ro + 16], in0=stat[:, ro + 8:ro + 12], scalar1=NE * CAP - 0.5, scalar2=None, op0=ALU.is_lt),
                 reads=[rtk], writes=[rtk])
            S.op("vector", lambda e, ro=ro, tile=tile: e.tensor_tensor(out=GV[:, tile, :], in0=stat[:, ro:ro + 4], in1=stat[:, ro + 12:ro + 16], op=ALU.mult),
                 reads=[rtk], writes=["GV"])
            for kk in range(4):
                S.op("gpsimd", lambda e, tile=tile, kk=kk, h2t=h2t: e.indirect_dma_start(
                    out=xs_d, out_offset=bass.IndirectOffsetOnAxis(ap=IDX[:, tile, kk:kk + 1], axis=0), in_=h2t, in_offset=None,
                    bounds_check=S.regs["bnd"], oob_is_err=False), reads=["IDX", h2tt], writes=[f"xs{tile}_{kk}"], dma=f"scat{kk}")
        p4_merge(0)
        for tile in range(16):
            if tile % 4 == 0 and tile // 4 + 1 < 4:
                p4_merge(tile // 4 + 1)
            p4_main(tile)
            if tile >= 1:
                p4_route(tile - 1)
        p4_route(15)
        for ip in range(16):
            S.op("tensor", lambda e, ip=ip: e.matmul(PS[7][:, 64:96], lhsT=onesb[:], rhs=MB[:, ip, :], start=(ip == 0), stop=(ip == 15)),
                 reads=["MB", "onesb"], writes=["ps7"], sig=(ip == 15))
        FORCE = os.environ.get("KFORCE")
        S.op("vector", lambda e: e.tensor_scalar(out=FLG[:], in0=PS[7][:, 64:96], scalar1=(-1.0 if FORCE else 512.0), scalar2=None, op0=ALU.is_gt),
             reads=["ps7"], writes=["FLG"])
        xs_tokens = [f"xs{tile}_{kk}" for tile in range(16) for kk in range(4)]
        if debug:
            S.op("sync", lambda e: e.dma_start(out=dbg["d_G"], in_=Gt.rearrange("p a b -> p (a b)")), reads=["Gt"], dma="dbg")
            S.op("sync", lambda e: e.dma_start(out=dbg["d_idx"], in_=IDX.rearrange("p a b -> p (a b)")), reads=["IDX"], dma="dbg")
            S.op("sync", lambda e: e.dma_start(out=dbg["d_gv"], in_=GV.rearrange("p a b -> p (a b)")), reads=["GV"], dma="dbg")

        if stop == "p4":
            return finish(["x1s"])

        allold = ["HT", "YR", "QTy", "wout", "mg0", "mg1", "xt4", "mixt", "h2f", "sg0", "sg1", "junk", "wrg", "xc160", "s2b", "h2t0", "h2t1"] \
            + [f"ring{i}" for i in range(8)]
        NBL = CAP // 128
        XE = [carve(16384 * i, 16384, BF16, "p (b d) -> p b d", b=NBL) for i in range(2)]
        XET = [carve(32768 + 16384 * i, 16384, BF16, "p (k t) -> p k t", k=KC) for i in range(2)]
        ACTT = carve(65536, 16384, BF16, "p (k t) -> p k t", k=KC)
        W2B = [carve(81920 + 16384 * i, 16384, BF16, "p (k n) -> p k n", k=KC) for i in range(2)]
        W1R = [carve(114688 + 4096 * i, 4096, BF16, "p (a k m) -> p a k m", a=2, k=KC) for i in range(8)]
        TMP = [[carve(147456 + 8192 * s_ + 2048 * j, 2048) for j in range(4)] for s_ in range(2)]
        YS = [carve(163840 + 4096 * i, 4096) for i in range(2)]
        W1X = [carve(172032 + 4096 * i, 4096, BF16, "p (a k m) -> p a k m", a=2, k=KC) for i in range(2)]
        names5 = ["xe0", "xe1", "xet0", "xet1", "actt", "w2b0", "w2b1", "ys0", "ys1", "w1x0", "w1x1"] + [f"w1r{i}" for i in range(8)] \
            + [f"tmp{s_}{j}" for s_ in range(2) for j in range(4)]
        for nm in names5:
            S.alias(nm, allold)

        def w1_load(ex_, c_):
            S.op("gpsimd", lambda e: e.dma_start(out=W1R[c_], in_=w1_h[ex_, c_]), writes=[f"w1r{c_}"], dma=f"w1r{c_}")

        def xe_load(ex_):
            S.op("sync", lambda e, ex_=ex_: e.dma_start(out=XE[ex_ % 2], in_=xs_d[ex_ * CAP:(ex_ + 1) * CAP, :].rearrange("(b p) d -> p b d", p=128)),
                 reads=xs_tokens, writes=[f"xe{ex_ % 2}"], dma=f"xe{ex_ % 2}")

        def w2_load(ex_):
            for q4 in range(4):
                S.op("gpsimd", lambda e, ex_=ex_, q4=q4: e.dma_start(out=W2B[ex_ % 2][:, 2 * q4:2 * q4 + 2, :], in_=w2_h[ex_, :, 2 * q4:2 * q4 + 2, :]),
                     writes=[f"w2b{ex_ % 2}"], dma=f"w2b{ex_ % 2}")

        cnt5 = {"it": 0, "ys": 0}

        def moe_T(ex, half):
            xeb, xetb = XE[ex % 2], XET[ex % 2]
            xetk, xettk = f"xe{ex % 2}", f"xet{ex % 2}"
            for k in range(KC):
                pbk = PS[6 + (k % 2)]
                pbt = f"ps{6 + (k % 2)}"
                pv = pbk[:, 0:256].bitcast(BF16)
                for b4 in range(4):
                    b_ = half * 4 + b4
                    S.op("tensor", lambda e, k=k, b_=b_, b4=b4, pv=pv: e.transpose(out=pv[:, b4 * 128:(b4 + 1) * 128], in_=xeb[:, b_, k * 128:(k + 1) * 128],
                                                                                 identity=identb[:]), reads=[xetk, "identb"], writes=[pbt], sig=(b4 == 3))
                if k % 2 == 0:
                    S.op("scalar", lambda e, k=k, pv=pv: e.copy(out=xetb[:, k, half * 512:(half + 1) * 512], in_=pv), reads=[pbt], writes=[xettk])
                else:
                    S.op("vector", lambda e, k=k, pv=pv: e.tensor_copy(out=xetb[:, k, half * 512:(half + 1) * 512], in_=pv), reads=[pbt], writes=[xettk])

        def moe_H(ex, tt):
            xetb = XET[ex % 2]
            xettk = f"xet{ex % 2}"
            if tt == 1:
                for c in range(2):
                    S.op("gpsimd", lambda e, c=c: e.dma_start(out=W1X[c], in_=w1_h[ex, c]), writes=[f"w1x{c}"], dma=f"w1x{c}")
            for c in range(8):
                if tt == 0:
                    w1 = W1R[c]; w1t = f"w1r{c}"
                else:
                    w1 = W1X[c % 2]; w1t = f"w1x{c % 2}"
                sset = cnt5["it"] % 2
                cnt5["it"] += 1
                tg, tsg, tu, tgs = TMP[sset]
                for a in range(2):
                    pb = PS[2 * sset + a]
                    for k in range(KC):
                        S.op("tensor", lambda e, a=a, k=k, w1=w1, pb=pb: e.matmul(pb[:, :], lhsT=w1[:, a, k, :], rhs=xetb[:, k, tt * 512:(tt + 1) * 512],
                                                                               start=(k == 0), stop=(k == KC - 1)),
                             reads=[w1t, xettk], writes=[f"ps{2 * sset + a}"], sig=(k == KC - 1))
                pg, pl = PS[2 * sset], PS[2 * sset + 1]
                S.op("vector", lambda e, c=c, tg=tg, pg=pg: e.tensor_scalar(out=tg, in0=pg[:, :], scalar1=b1c[:, ex, c:c + 1], scalar2=7.0,
                                                                           op0=ALU.add, op1=ALU.min), reads=[f"ps{2 * sset}", "b1c"], writes=[f"tmp{sset}0"])
                S.op("scalar", lambda e, tg=tg, tsg=tsg: e.activation(out=tsg, in_=tg, func=AF.Sigmoid, scale=1.702), reads=[f"tmp{sset}0"], writes=[f"tmp{sset}1"])
                S.op("vector", lambda e, c=c, tu=tu, pl=pl: e.tensor_scalar(out=tu, in0=pl[:, :], scalar1=b1c[:, ex, 8 + c:9 + c], scalar2=8.0,
                                                                           op0=ALU.add, op1=ALU.min), reads=[f"ps{2 * sset + 1}", "b1c"], writes=[f"tmp{sset}2"])
                S.op("vector", lambda e, tu=tu, tg=tg, tgs=tgs: e.scalar_tensor_tensor(out=tgs, in0=tu, scalar=-6.0, in1=tg, op0=ALU.max, op1=ALU.mult),
                     reads=[f"tmp{sset}2", f"tmp{sset}0"], writes=[f"tmp{sset}3"])
                S.op("vector", lambda e, c=c, tsg=tsg, tgs=tgs: e.tensor_tensor(out=ACTT[:, c, tt * 512:(tt + 1) * 512], in0=tgs, in1=tsg, op=ALU.mult),
                     reads=[f"tmp{sset}3", f"tmp{sset}1"], writes=[f"actt{tt}"])
                if tt == 1 and c + 2 < 8:
                    S.op("gpsimd", lambda e, c=c: e.dma_start(out=W1X[c % 2], in_=w1_h[ex, c + 2]), writes=[f"w1x{c % 2}"], dma=f"w1x{c % 2}")
                if tt == 0 and ex + 1 < NE:
                    w1_load(ex + 1, c)

        def moe_Y(ex, half):
            w2 = W2B[ex % 2]; w2t = f"w2b{ex % 2}"
            for b4 in range(4):
                b_ = half * 4 + b4
                ys = YS[cnt5["ys"] % 2]; yst = f"ys{cnt5['ys'] % 2}"
                ysk = f"ysst{cnt5['ys'] % 2}"
                cnt5["ys"] += 1
                for hf in range(2):
                    pi = 4 + hf
                    for c in range(8):
                        S.op("tensor", lambda e, c=c, b_=b_, hf=hf, pi=pi: e.matmul(PS[pi][:, :], lhsT=ACTT[:, c, b_ * 128:(b_ + 1) * 128],
                                                                                  rhs=w2[:, c, hf * 512:(hf + 1) * 512], start=(c == 0), stop=(c == 7)),
                             reads=[f"actt{half}", w2t], writes=[f"ps{pi}"], sig=(c == 7))
                    S.op("scalar", lambda e, hf=hf, pi=pi, ys=ys: e.copy(out=ys[:, hf * 512:(hf + 1) * 512], in_=PS[pi][:, :]), reads=[f"ps{pi}"], writes=[yst])
                r0 = ex * CAP + b_ * 128
                S.op("sync", lambda e, r0=r0, ys=ys: e.dma_start(out=ys_d[r0:r0 + 128, :], in_=ys), reads=[yst], writes=[f"ysd{ex}_{b_}"], dma=ysk)

        S.alias("actt0", ["actt"]); S.alias("actt1", ["actt"])
        xe_load(0)
        w2_load(0)
        for c in range(8):
            w1_load(0, c)
        for ex in range(NE):
            if ex + 1 < NE:
                xe_load(ex + 1)
                w2_load(ex + 1)
            if ex == 0:
                moe_T(ex, 0)
            moe_H(ex, 0)
            S.region_begin(FLG[0:1, ex:ex + 1], "FLG")
            moe_T(ex, 1)
            moe_H(ex, 1)
            moe_Y(ex, 1)
            S.region_end()
            if ex + 1 < NE:
                moe_T(ex + 1, 0)
            moe_Y(ex, 0)
        ys_tokens = [f"ysd{ex}_{b_}" for ex in range(NE) for b_ in range(NBL)]
        names5 = names5 + ["actt0", "actt1"]

        S.alias("fin", names5)
        ACC6 = [carve(4096 * i, 4096) for i in range(2)]
        XO = [carve(8192 + 4096 * i, 4096) for i in range(2)]
        JK = carve(16384, 4096)
        B2S = [carve(20480 + 2048 * i, 2048) for i in range(2)]
        GTT = carve(24576, 2048)
        NYG = 4
        YG = [[carve(28672 + 16384 * s_ + 4096 * j, 4096) for j in range(4)] for s_ in range(NYG)]
        n6 = ["acc0", "acc1", "xo0", "xo1", "jk", "b2s0", "b2s1", "gtt"] + [f"yg{s_}{j}" for s_ in range(NYG) for j in range(4)]
        for nm in n6:
            S.alias(nm, ["fin"])
        for s_ in range(NYG):
            for j in range(4):
                S.op("gpsimd", lambda e, s_=s_, j=j: e.memset(YG[s_][j], 0.0), writes=[f"yg{s_}{j}"])
        for hf in range(2):
            S.op("sync", lambda e, hf=hf: e.dma_start(out=B2S[hf][0:NE, :], in_=b2_h[:, hf * 512:(hf + 1) * 512]), writes=[f"b2s{hf}"], dma=f"b2s{hf}")
        for tile in range(16):
            s6 = tile % 2
            sg6 = tile % NYG
            acc = ACC6[s6]; acct = f"acc{s6}"
            xo = XO[s6]; xot = f"xo{s6}"
            r0 = tile * 128
            S.op("sync", lambda e, r0=r0, xo=xo: e.dma_start(out=xo, in_=x1s[r0:r0 + 128, :]), reads=["x1sd"], writes=[xot], dma=xot)
            for kk in range(4):
                S.op("gpsimd", lambda e, tile=tile, kk=kk, sg6=sg6: e.indirect_dma_start(
                    out=YG[sg6][kk], out_offset=None, in_=ys_d, in_offset=bass.IndirectOffsetOnAxis(ap=IDX[:, tile, kk:kk + 1], axis=0),
                    bounds_check=S.regs["bnd"], oob_is_err=False), reads=ys_tokens + ["IDX"], writes=[f"yg{sg6}{kk}"], dma=f"yg{sg6}{kk}")
            S.op("tensor", lambda e, tile=tile: e.transpose(out=PS[7][0:NE, 0:128], in_=Gt[:, tile, :], identity=ident[:]),
                 reads=["Gt", "ident"], writes=["ps7"])
            S.op("vector", lambda e: e.tensor_copy(out=GTT[0:NE, 0:128], in_=PS[7][0:NE, 0:128]), reads=["ps7"], writes=["gtt"])
            for hf in range(2):
                S.op("tensor", lambda e, hf=hf: e.matmul(PS[4 + hf][:, :], lhsT=GTT[0:NE, 0:128], rhs=B2S[hf][0:NE, :], start=True, stop=True),
                     reads=[f"b2s{hf}", "gtt"], writes=[f"ps{4 + hf}"])
                S.op("vector", lambda e, hf=hf, acc=acc: e.tensor_copy(out=acc[:, hf * 512:(hf + 1) * 512], in_=PS[4 + hf][:, :]),
                     reads=[f"ps{4 + hf}"], writes=[acct])
            for kk in range(4):
                S.op("vector", lambda e, tile=tile, kk=kk, sg6=sg6, acc=acc: e.scalar_tensor_tensor(out=acc, in0=YG[sg6][kk], scalar=GV[:, tile, kk:kk + 1], in1=acc,
                                                                                            op0=ALU.mult, op1=ALU.add),
                     reads=[f"yg{sg6}{kk}", "GV", acct], writes=[acct])
            so, stk = newstat()
            S.op("scalar", lambda e, acc=acc, so=so: e.activation(out=JK, in_=acc, func=AF.Square, accum_out=stat[:, so:so + 1]),
                 reads=[acct], writes=["jk", stk])
            S.op("scalar", lambda e, so=so: e.activation(out=stat[:, so + 1:so + 2], in_=stat[:, so:so + 1], func=AF.Sqrt, scale=1.0 / D, bias=EPS),
                 reads=[stk], writes=[stk])
            S.op("vector", lambda e, so=so: e.reciprocal(out=stat[:, so + 1:so + 2], in_=stat[:, so + 1:so + 2]), reads=[stk], writes=[stk])
            S.op("vector", lambda e, acc=acc, so=so: e.scalar_tensor_tensor(out=acc, in0=acc, scalar=stat[:, so + 1:so + 2], in1=G2b[:],
                                                                          op0=ALU.mult, op1=ALU.mult), reads=[acct, stk, "G2b"], writes=[acct])
            S.op("vector", lambda e, acc=acc, xo=xo: e.tensor_tensor(out=xo, in0=xo, in1=acc, op=ALU.add), reads=[acct, xot], writes=[xot])
            S.op("sync", lambda e, r0=r0, xo=xo: e.dma_start(out=out[r0:r0 + 128, :], in_=xo), reads=[xot], dma="outst")
        return finish()


def _alibi_bias():
    slopes = np.array([2.0 ** (-8.0 * (h + 1) / 16) for h in range(16)], dtype=np.float32)
    qi = np.arange(128)[:, None]
    ci = np.arange(256)[None, :]
    dist = qi + 128 - ci
    valid = (dist >= 0) & (dist < 128)
    b = np.where(valid[:, None, :], -slopes[None, :, None] * dist[:, None, :].astype(np.float32), np.float32(NEG))
    return np.ascontiguousarray(b.astype(np.float32))


def _col(v, k=KC):
    return np.ascontiguousarray(np.asarray(v, np.float32).reshape(k, 128).T)


def prepare_inputs(x, c, w_ada, b_ada, norm_pre_mix, norm_post_mix, norm_pre_ffn, norm_post_ffn,
                   w_in, b_in, conv_w, conv_b, rg_w_a, rg_b_a, rg_w_x, rg_b_x, rg_lambda,
                   attn_sinks, w_o_rnn, w_o_attn, w_out, router_w, router_b,
                   moe_w1, moe_b1, moe_w2, moe_b2):
    f = lambda a: np.asarray(a, np.float32)
    x, c = f(x), f(c)
    L = 0
    shared = {}
    shared["wada"] = np.ascontiguousarray(f(w_ada)[L].reshape(KC, 128, 12, 512).transpose(2, 1, 0, 3))
    shared["bada_col"] = _col(f(b_ada)[L], 48)
    shared["bada_row"] = np.ascontiguousarray(f(b_ada)[L].reshape(6, D))
    gam = np.stack([f(norm_pre_mix)[L], f(norm_post_mix)[L], f(norm_pre_ffn)[L], f(norm_post_ffn)[L]])
    shared["gam_col"] = np.ascontiguousarray(gam.reshape(4, KC, 128).transpose(2, 0, 1))
    shared["gam_row"] = np.ascontiguousarray(gam)
    shared["w_in_h"] = np.ascontiguousarray(f(w_in)[L].reshape(KC, 128, 44, 128).transpose(2, 1, 0, 3))
    shared["b_in_col"] = _col(f(b_in)[L], 44)
    wk = f(w_in)[L][:, 3072:3328].reshape(KC, 128, 2, 2, 64)[:, :, :, ::-1, :].reshape(KC, 128, 2, 128)
    shared["w_ks_h"] = np.ascontiguousarray(wk.transpose(2, 1, 0, 3))
    bk = f(b_in)[L][3072:3328].reshape(2, 2, 64)[:, ::-1, :].reshape(2, 128)
    shared["b_ks_col"] = np.ascontiguousarray(bk.T)
    shared["b_v_row"] = np.ascontiguousarray(f(b_in)[L][3328:3584])
    cw = np.concatenate([f(conv_w)[L], f(conv_b)[L][None, :]], axis=0)
    shared["conv_col"] = np.ascontiguousarray(cw.reshape(5, KC, 128).transpose(2, 1, 0))
    rg = np.zeros((128, 2, KC, 128), np.float32)
    for a, wsrc in enumerate((f(rg_w_a)[L], f(rg_w_x)[L])):
        for cc in range(KC):
            rg[0:64, a, cc, 0:64] = wsrc[2 * cc]
            rg[64:128, a, cc, 64:128] = wsrc[2 * cc + 1]
    shared["rgw"] = rg
    shared["rgb_col"] = np.ascontiguousarray(np.stack([_col(f(rg_b_a)[L]), _col(f(rg_b_x)[L])], axis=1))
    shared["lam_col"] = _col(f(rg_lambda)[L])
    shared["sinks_row"] = np.ascontiguousarray(f(attn_sinks)[L].reshape(4, 4)[:, [0, 2, 1, 3]].reshape(16))
    shared["w_or_h"] = np.ascontiguousarray(f(w_o_rnn)[L].reshape(KC, 128, KC, 128).transpose(2, 1, 0, 3))
    shared["w_oa_h"] = np.ascontiguousarray(f(w_o_attn)[L].reshape(KC, 128, KC, 128).transpose(2, 1, 0, 3))
    shared["w_out_h"] = np.ascontiguousarray(f(w_out)[L].reshape(KC, 128, D).transpose(1, 0, 2))
    shared["router_h"] = np.ascontiguousarray(f(router_w)[L].reshape(KC, 128, NE).transpose(1, 0, 2))
    shared["router_b"] = np.ascontiguousarray(f(router_b)[L])
    shared["w1_h"] = np.ascontiguousarray(f(moe_w1)[L].reshape(NE, KC, 128, 8, 128, 2).transpose(0, 3, 2, 5, 1, 4))
    b1 = f(moe_b1)[L].reshape(NE, 8, 128, 2)
    shared["b1_col"] = np.ascontiguousarray(b1.transpose(2, 0, 3, 1).reshape(128, NE, 16))
    shared["w2_h"] = np.ascontiguousarray(f(moe_w2)[L].reshape(NE, KC, 128, D).transpose(0, 2, 1, 3))
    shared["b2_h"] = np.ascontiguousarray(f(moe_b2)[L])
    shared["abias_h"] = _alibi_bias()
    in_maps = []
    for r in range(NCORES):
        b, j = r // 4, r % 4
        m = dict(shared)
        xe = np.zeros((NST * T, D), np.float32)
        n_real = (j + 1) * T
        xe[NST * T - n_real:] = x[b, :n_real]
        m["xe"] = xe
        m["ccol"] = _col(c[b])
        fl = np.zeros((128, 16), np.float32)
        for st in range(NST):
            valid = 1.0 if st >= NST - 1 - j else 0.0
            first = 1.0 if st == NST - 1 - j else 0.0
            fl[:, st] = valid
            fl[:, 4 + st] = first
        fl[:, 8] = 0.0 if j > 0 else NEG
        m["flags_h"] = fl
        rc = np.zeros((128, 160), np.float32)
        rc[:, 0:32] = (np.arange(32, dtype=np.float32) * CAP)[None, :]
        rc[:, 32:160] = (np.arange(128)[:, None] < np.arange(128)[None, :]).astype(np.float32)
        m["rc_h"] = rc
        in_maps.append(m)
    return in_maps


_NC_CACHE = {}


def kernel(**inputs):
    debug = bool(os.environ.get("KDEBUG"))
    stop = os.environ.get("KSTOP") or None
    in_maps = prepare_inputs(**inputs)
    if stop is not None:
        for m in in_maps:
            m["w1_h"] = m["w1_h"][:1]
            m["w2_h"] = m["w2_h"][:1]
    if (debug, stop) not in _NC_CACHE:
        _NC_CACHE[(debug, stop)] = build_nc(debug, stop)
    nc = _NC_CACHE[(debug, stop)]
    res = run_bass_kernel_spmd(nc, in_maps, core_ids=list(range(NCORES)))
    outs = [np.asarray(r["out"], np.float32) for r in res.results]
    full = np.stack([np.concatenate(outs[0:4], axis=0), np.concatenate(outs[4:8], axis=0)], axis=0)
    if debug:
        kernel.last_results = res.results
    return full.astype(np.float32)
```
